# Optimizing a Trainium2 kernel written in Bass

```python
import math
import jax
import jax.numpy as jnp
from jax import lax
import numpy as np

D_MODEL = 1024
BATCH = 8
SEQ = 4096
DEPTH = 2

GRID_W = 64
CTX_LEN = 256
RMS_EPS = 1e-6
N_ADA = 6
DIFF_HEADS = 8
DIFF_HEAD_DIM = 64
DIFF_V_DIM = 2 * DIFF_HEAD_DIM
DIFF_QK_W = DIFF_HEADS * 2 * DIFF_HEAD_DIM
DIFF_V_W = DIFF_HEADS * DIFF_V_DIM
Q_BLOCK = 128
ROPE_BASE = 10000.0
ROT_AXIS_DIM = DIFF_HEAD_DIM // 2
CONV_W = D_MODEL // 2
CONV_K = 3
GLA_HEADS = 4
GLA_DK = 64
GLA_DV = 128
GLA_K_W = GLA_HEADS * GLA_DK
GLA_V_W = GLA_HEADS * GLA_DV
GLA_RANK = 16
GLA_TAU = 16.0
GLA_CHUNK = 64
N_BRANCH = 3
N_EXPERTS = 32
TOP_K = 4
D_EXPERT = D_MODEL
SWIGLU_LIMIT = 7.0
SWIGLU_ALPHA = 1.702
MOE_BLOCK = 128
SPLITS = (DIFF_QK_W, DIFF_QK_W, DIFF_V_W,
          CONV_W, CONV_W, CONV_W,
          GLA_K_W, GLA_K_W, GLA_V_W, GLA_V_W,
          2 * GLA_RANK,
          N_BRANCH * D_MODEL)
W_IN_COLS = int(sum(SPLITS))
SPLIT_POINTS = tuple(int(p) for p in np.cumsum(SPLITS)[:-1])

kernel_name = 'hybrid_diffattn_conv_gla_moe_dit'


def rms_norm(x, g):
    xf = x.astype(jnp.float32)
    y = xf * lax.rsqrt(jnp.mean(xf * xf, axis=-1, keepdims=True) + RMS_EPS)
    return (y * g.astype(jnp.float32)).astype(x.dtype)


def modulate(h, shift, scale):
    return h * (1.0 + scale) + shift


def rope_2d(x, row, col):
    inv_freq = ROPE_BASE ** (-jnp.arange(0, ROT_AXIS_DIM, 2, dtype=jnp.float32) / ROT_AXIS_DIM)

    def rotate(xp, pos):
        ang = pos.astype(jnp.float32)[:, None] * inv_freq[None, :]
        ang = jnp.concatenate([ang, ang], axis=-1)[:, None, None, :]
        cos = jnp.cos(ang).astype(xp.dtype)
        sin = jnp.sin(ang).astype(xp.dtype)
        x1, x2 = jnp.split(xp, 2, axis=-1)
        return xp * cos + jnp.concatenate([-x2, x1], axis=-1) * sin

    x_row, x_col = jnp.split(x, 2, axis=-1)
    return jnp.concatenate([rotate(x_row, row), rotate(x_col, col)], axis=-1)


def diff_attend(q, k, v, lam):
    s = jnp.einsum('bqhid,bkhid->bhiqk', q, k).astype(jnp.float32) * (DIFF_HEAD_DIM ** -0.5)
    p = jax.nn.softmax(s, axis=-1)
    a = p[:, :, 0] - lam * p[:, :, 1]
    return jnp.einsum('bhqk,bkhe->bqhe', a.astype(v.dtype), v)


def short_conv(u, w):
    up = jnp.pad(u, ((0, 0), (1, 1), (0, 0)))
    return up[:, :-2] * w[0] + up[:, 1:-1] * w[1] + up[:, 2:] * w[2]


def gla_chunked(q, k, v, g, s0):
    b, h, t, dk = q.shape
    dv = v.shape[-1]
    n = t // GLA_CHUNK
    q, k, g = (a.reshape(b, h, n, GLA_CHUNK, dk) for a in (q, k, g))
    v = v.reshape(b, h, n, GLA_CHUNK, dv)
    g_cum = jnp.cumsum(g, axis=3)
    g_tot = g_cum[:, :, :, -1]
    q_dec = q * jnp.exp(g_cum)
    k_inv = k * jnp.exp(-g_cum)
    k_end = k * jnp.exp(g_tot[:, :, :, None] - g_cum)
    lower_tri = jnp.tril(jnp.ones((GLA_CHUNK, GLA_CHUNK), dtype=bool))
    a = jnp.where(lower_tri, jnp.einsum('bhnld,bhnmd->bhnlm', q_dec, k_inv), 0.0)
    o_intra = jnp.einsum('bhnlm,bhnme->bhnle', a, v)
    s_local = jnp.einsum('bhnld,bhnle->bhnde', k_end, v)

    def step(s, inp):
        dec, s_loc = inp
        return jnp.exp(dec)[..., None] * s + s_loc, s

    s_fin, s_prev = lax.scan(step, s0, (jnp.moveaxis(g_tot, 2, 0), jnp.moveaxis(s_local, 2, 0)))
    o_inter = jnp.einsum('bhnld,bhnde->bhnle', q_dec, jnp.moveaxis(s_prev, 0, 2))
    return (o_intra + o_inter).reshape(b, h, t, dv), s_fin


def gla_bidir(q, k, v, g_f, g_b, s_f0, s_b0):
    o_f, s_f = gla_chunked(q, k, v, g_f, s_f0)
    flip = lambda a: jnp.flip(a, axis=2)
    o_b, s_b = gla_chunked(flip(q), flip(k), flip(v), flip(g_b), s_b0)
    return o_f + flip(o_b), s_f, s_b


def clamped_swiglu(hh):
    x_glu, x_lin = hh[..., ::2], hh[..., 1::2]
    x_glu = jnp.minimum(x_glu, SWIGLU_LIMIT)
    x_lin = jnp.clip(x_lin, -SWIGLU_LIMIT, SWIGLU_LIMIT)
    return x_glu * jax.nn.sigmoid(SWIGLU_ALPHA * x_glu) * (x_lin + 1.0)


def moe_ffn(h, router_w, router_b, w1, b1, w2, b2):
    t, d = h.shape
    logits = (h @ router_w + router_b).astype(jnp.float32)
    top_val, top_idx = lax.top_k(logits, TOP_K)
    gate = jax.nn.softmax(top_val, axis=-1)
    e_flat = top_idx.reshape(-1)
    g_flat = gate.reshape(-1)
    tk = t * TOP_K
    order = jnp.argsort(e_flat)
    e_sorted = e_flat[order]
    counts = jnp.bincount(e_flat, length=N_EXPERTS)
    padded = (counts + MOE_BLOCK - 1) // MOE_BLOCK * MOE_BLOCK
    pad_end = jnp.cumsum(padded)
    pad_start = pad_end - padded
    start = jnp.cumsum(counts) - counts
    dest = pad_start[e_sorted] + (jnp.arange(tk) - start[e_sorted])
    n_blocks = -(-tk // MOE_BLOCK) + N_EXPERTS
    n_rows = n_blocks * MOE_BLOCK
    row_tok = jnp.full((n_rows,), t, dtype=jnp.int32).at[dest].set((order // TOP_K).astype(jnp.int32))
    row_gate = jnp.zeros((n_rows,), jnp.float32).at[dest].set(g_flat[order])
    block_exp = jnp.minimum(jnp.searchsorted(pad_end, jnp.arange(n_blocks) * MOE_BLOCK, side='right'),
                            N_EXPERTS - 1)
    h_pad = jnp.concatenate([h, jnp.zeros((1, d), h.dtype)], axis=0)
    xs = h_pad[row_tok].reshape(n_blocks, MOE_BLOCK, d)

    def expert_block(args):
        xb, e = args
        return clamped_swiglu(xb @ w1[e] + b1[e]) @ w2[e] + b2[e]

    ys = lax.map(expert_block, (xs, block_exp)).reshape(n_rows, d)
    out = jnp.zeros((t + 1, d), ys.dtype).at[row_tok].add(ys * row_gate[:, None].astype(ys.dtype))
    return out[:t]


def token_mixer(h, hc, row, col, w_in, diff_lambda, lam_init, diff_subln_g, diff_w_out,
                conv_w, conv_w_out, gla_w_a2, gla_b_a, gla_norm_g, gla_w_out, w_o, ctx_out):
    b, s, _ = h.shape
    (d_q, d_k, d_v, cv_b, cv_c, cv_x, gl_q, gl_k, gl_v, gl_r, gl_a, gates) = jnp.split(
        h @ w_in, SPLIT_POINTS, axis=-1)
    (d_q_c, d_k_c, d_v_c, cv_b_c, cv_c_c, cv_x_c, gl_q_c, gl_k_c, gl_v_c, gl_r_c, gl_a_c, gates_c) = jnp.split(
        hc @ w_in, SPLIT_POINTS, axis=-1)

    lam_v = diff_lambda.astype(jnp.float32)
    lam = jnp.exp(jnp.sum(lam_v[0] * lam_v[1])) - jnp.exp(jnp.sum(lam_v[2] * lam_v[3])) + lam_init
    qk_heads = lambda a: a.reshape(a.shape[0], a.shape[1], DIFF_HEADS, 2, DIFF_HEAD_DIM)
    v_heads = lambda a: a.reshape(a.shape[0], a.shape[1], DIFF_HEADS, DIFF_V_DIM)
    q_lat = rope_2d(qk_heads(d_q), row, col)
    k_all = jnp.concatenate([rope_2d(qk_heads(d_k), row, col), qk_heads(d_k_c)], axis=1)
    v_all = jnp.concatenate([v_heads(d_v), v_heads(d_v_c)], axis=1)
    n_qb = s // Q_BLOCK
    q_blocks = jnp.moveaxis(q_lat.reshape(b, n_qb, Q_BLOCK, DIFF_HEADS, 2, DIFF_HEAD_DIM), 1, 0)
    o_blocks = lax.map(lambda qi: diff_attend(qi, k_all, v_all, lam), q_blocks)
    o_lat = jnp.moveaxis(o_blocks, 0, 1).reshape(b, s, DIFF_HEADS, DIFF_V_DIM)

    def diff_out(o_heads):
        o_heads = rms_norm(o_heads, diff_subln_g) * (1.0 - lam_init)
        return o_heads.reshape(o_heads.shape[0], o_heads.shape[1], DIFF_V_W) @ diff_w_out

    def conv_out(gb, gc, u):
        return (gb * short_conv(gc * u, conv_w)) @ conv_w_out

    def gla_inputs(gq, gk, gv, ga):
        t = gq.shape[1]
        heads = lambda a, dh: jnp.moveaxis(a.reshape(b, t, GLA_HEADS, dh), 2, 1).astype(jnp.float32)
        a_f, a_b = jnp.split(ga, 2, axis=-1)
        g_f = jax.nn.log_sigmoid((a_f @ gla_w_a2[0] + gla_b_a[0]).astype(jnp.float32)) / GLA_TAU
        g_b = jax.nn.log_sigmoid((a_b @ gla_w_a2[1] + gla_b_a[1]).astype(jnp.float32)) / GLA_TAU
        return (heads(gq, GLA_DK) * (GLA_DK ** -0.5), heads(gk, GLA_DK), heads(gv, GLA_DV),
                heads(g_f, GLA_DK), heads(g_b, GLA_DK))

    s0 = jnp.zeros((b, GLA_HEADS, GLA_DK, GLA_DV), jnp.float32)
    qc_, kc_, vc_, gfc_, gbc_ = gla_inputs(gl_q_c, gl_k_c, gl_v_c, gl_a_c)
    o_gla_c, s_f, s_b = gla_bidir(qc_, kc_, vc_, gfc_, gbc_, s0, s0)
    q_, k_, v_, gf_, gb_ = gla_inputs(gl_q, gl_k, gl_v, gl_a)
    o_gla, _, _ = gla_bidir(q_, k_, v_, gf_, gb_, s_f, s_b)

    def gla_out(o_heads, r):
        o_heads = rms_norm(jnp.moveaxis(o_heads, 1, 2), gla_norm_g).astype(r.dtype)
        return (o_heads.reshape(r.shape[0], r.shape[1], GLA_V_W) * jax.nn.silu(r)) @ gla_w_out

    def merge(gt, y_diff, y_conv, y_gla):
        g = jax.nn.sigmoid(gt).reshape(gt.shape[0], gt.shape[1], N_BRANCH, D_MODEL)
        return (g[:, :, 0] * y_diff + g[:, :, 1] * y_conv + g[:, :, 2] * y_gla) @ w_o

    m = merge(gates, diff_out(o_lat), conv_out(cv_b, cv_c, cv_x), gla_out(o_gla, gl_r))
    if not ctx_out:
        return m, None
    o_ctx = diff_attend(qk_heads(d_q_c), qk_heads(d_k_c), v_heads(d_v_c), lam)
    mc = merge(gates_c, diff_out(o_ctx), conv_out(cv_b_c, cv_c_c, cv_x_c), gla_out(o_gla_c, gl_r_c))
    return m, mc


def setup_inputs(seed: int = 0) -> dict:
    key = jax.random.key(seed)
    ks = jax.random.split(key, 32)
    L = DEPTH

    def nrm(k, shape, scale):
        return jax.random.normal(k, shape, jnp.float32) * scale

    return {
        'x': nrm(ks[0], (BATCH, SEQ, D_MODEL), 1.0),
        'c': nrm(ks[1], (BATCH, D_MODEL), 1.0),
        'ctx': nrm(ks[2], (BATCH, CTX_LEN, D_MODEL), 1.0),
        'c_ctx': nrm(ks[3], (D_MODEL,), 1.0),
        'ada_w': nrm(ks[4], (L, D_MODEL, N_ADA * D_MODEL), 0.5 * D_MODEL ** -0.5),
        'ada_b': nrm(ks[5], (L, N_ADA * D_MODEL), 0.01),
        'norm1_g': 1.0 + nrm(ks[6], (L, D_MODEL), 0.05),
        'norm2_g': 1.0 + nrm(ks[7], (L, D_MODEL), 0.05),
        'w_in': nrm(ks[8], (L, D_MODEL, W_IN_COLS), D_MODEL ** -0.5),
        'diff_lambda': nrm(ks[9], (L, 4, DIFF_HEAD_DIM), 0.1),
        'diff_subln_g': 1.0 + nrm(ks[10], (L, DIFF_V_DIM), 0.05),
        'diff_w_out': nrm(ks[11], (L, DIFF_V_W, D_MODEL), DIFF_V_W ** -0.5),
        'conv_w': nrm(ks[12], (L, CONV_K, CONV_W), CONV_K ** -0.5),
        'conv_w_out': nrm(ks[13], (L, CONV_W, D_MODEL), CONV_W ** -0.5),
        'gla_w_a2': nrm(ks[14], (L, 2, GLA_RANK, GLA_K_W), GLA_RANK ** -0.5),
        'gla_b_a': nrm(ks[15], (L, 2, GLA_K_W), 0.1),
        'gla_norm_g': 1.0 + nrm(ks[16], (L, GLA_DV), 0.05),
        'gla_w_out': nrm(ks[17], (L, GLA_V_W, D_MODEL), GLA_V_W ** -0.5),
        'w_o': nrm(ks[18], (L, D_MODEL, D_MODEL), D_MODEL ** -0.5),
        'router_w': nrm(ks[19], (L, D_MODEL, N_EXPERTS), D_MODEL ** -0.5),
        'router_b': nrm(ks[20], (L, N_EXPERTS), 0.01),
        'moe_w1': nrm(ks[21], (L, N_EXPERTS, D_MODEL, 2 * D_EXPERT), D_MODEL ** -0.5),
        'moe_b1': nrm(ks[22], (L, N_EXPERTS, 2 * D_EXPERT), 0.01),
        'moe_w2': nrm(ks[23], (L, N_EXPERTS, D_EXPERT, D_MODEL), D_EXPERT ** -0.5),
        'moe_b2': nrm(ks[24], (L, N_EXPERTS, D_MODEL), 0.01),
        'final_norm_g': 1.0 + nrm(ks[25], (D_MODEL,), 0.05),
    }


def reference(x, c, ctx, c_ctx, ada_w, ada_b, norm1_g, norm2_g, w_in, diff_lambda, diff_subln_g,
              diff_w_out, conv_w, conv_w_out, gla_w_a2, gla_b_a, gla_norm_g, gla_w_out, w_o,
              router_w, router_b, moe_w1, moe_b1, moe_w2, moe_b2, final_norm_g):
    b, s, d = x.shape
    rows = s // GRID_W
    row = jnp.repeat(jnp.arange(rows, dtype=jnp.int32), GRID_W)
    col = jnp.tile(jnp.arange(GRID_W, dtype=jnp.int32), rows)
    c_act = jax.nn.silu(c)
    c_ctx_act = jax.nn.silu(c_ctx)
    for layer in range(DEPTH):
        last = layer == DEPTH - 1
        lam_init = 0.8 - 0.6 * math.exp(-0.3 * layer)
        mod = c_act @ ada_w[layer] + ada_b[layer]
        mod_c = c_ctx_act @ ada_w[layer] + ada_b[layer]
        sh1, sc1, g1, sh2, sc2, g2 = jnp.split(mod[:, None, :], N_ADA, axis=-1)
        sh1c, sc1c, g1c, sh2c, sc2c, g2c = jnp.split(mod_c, N_ADA, axis=-1)
        h = modulate(rms_norm(x, norm1_g[layer]), sh1, sc1)
        hc = modulate(rms_norm(ctx, norm1_g[layer]), sh1c, sc1c)
        m, mc = token_mixer(h, hc, row, col, w_in[layer], diff_lambda[layer], lam_init,
                            diff_subln_g[layer], diff_w_out[layer], conv_w[layer], conv_w_out[layer],
                            gla_w_a2[layer], gla_b_a[layer], gla_norm_g[layer], gla_w_out[layer],
                            w_o[layer], not last)
        x = x + g1 * m
        h2 = modulate(rms_norm(x, norm2_g[layer]), sh2, sc2)
        moe_args = (router_w[layer], router_b[layer], moe_w1[layer], moe_b1[layer],
                    moe_w2[layer], moe_b2[layer])
        if last:
            f = moe_ffn(h2.reshape(b * s, d), *moe_args)
            x = x + g2 * f.reshape(b, s, d)
        else:
            ctx = ctx + g1c * mc
            h2c = modulate(rms_norm(ctx, norm2_g[layer]), sh2c, sc2c)
            tokens = jnp.concatenate([h2.reshape(b * s, d), h2c.reshape(-1, d)], axis=0)
            f = moe_ffn(tokens, *moe_args)
            x = x + g2 * f[:b * s].reshape(b, s, d)
            ctx = ctx + g2c * f[b * s:].reshape(ctx.shape)
    return rms_norm(x, final_norm_g)
```

```python
import math
from contextlib import ExitStack
import numpy as np
import concourse.bass as bass
import concourse.mybir as mybir
from concourse.bass_utils import run_bass_kernel_spmd

F32 = mybir.dt.float32
BF16 = mybir.dt.bfloat16
AF = mybir.ActivationFunctionType
ALU = mybir.AluOpType
AX = mybir.AxisListType

D = 1024
S = 4096
C = 256
T = S + C
NT = T // 128
DEPTH = 2
NE = 32
WIN = 9248
EPS = 1e-6
OQ, OK_, OV = 0, 1024, 2048
OCB, OCC, OCX = 3072, 3584, 4096
OGQ, OGK, OGV, OGR, OGA = 4608, 4864, 5120, 5632, 6144
OGT = 6176
TILES = [(0, 256)] + [(256 + 512 * i, 512) for i in range(8)]


DBG = {}


class Dep:
    def __init__(self):
        self.w = None
        self.r = {}
        self.ds = None


class TL(Dep):
    def __init__(self, h):
        super().__init__()
        self.h = h

    def __getitem__(self, k):
        return self.h[k]


class DSem:
    def __init__(self, sem):
        self.sem = sem
        self.cnt = 0


class Prog:
    def __init__(self, nc, ndsem=96):
        self.nc = nc
        self.E = {"pe": nc.tensor, "act": nc.scalar, "dve": nc.vector, "pool": nc.gpsimd, "sp": nc.sync}
        self.sem = {k: nc.alloc_semaphore("s_" + k) for k in self.E}
        self.cnt = {k: 0 for k in self.E}
        self.seen = {k: {} for k in self.E}
        self.dpool = [DSem(nc.alloc_semaphore("d%d" % i)) for i in range(ndsem)]
        self.dnext = 0
        self.persist = 0
        self.nsb = 0
        self.pe_cols = [0]
        self.dly = {}
        self.nfence = 0

    def sb(self, es, shape, dt, name=None):
        self.nsb += 1
        h = es.enter_context(self.nc.sbuf_tensor("t%d" % self.nsb, list(shape), dt))
        return TL(h)

    def ps(self, es, shape, dt=F32):
        self.nsb += 1
        h = es.enter_context(self.nc.psum_tensor("p%d" % self.nsb, list(shape), dt))
        return TL(h)

    def _ds(self, t):
        if t.ds is None:
            assert self.dnext < len(self.dpool), "out of dma semaphores"
            t.ds = self.dpool[self.dnext]
            self.dnext += 1
        return t.ds

    def _wait(self, eng, ev, raw=False):
        if ev is None:
            return
        if ev[0] == "e":
            _, src, val = ev
            if src == eng and (eng == "pe" or not raw):
                return
            key = src
            sem = self.sem[src]
            if src == "pe":
                need = self.pe_cols[val] + 256
                k2 = val
                while k2 < self.cnt["pe"] and self.pe_cols[k2] < need:
                    k2 += 1
                if self.pe_cols[k2] >= need:
                    val = k2
                else:
                    val = self.cnt["pe"]
                    if self.seen[eng].get(key, 0) < val:
                        self.E[eng].wait_ge(sem, val)
                        self.seen[eng][key] = val
                    if self.seen[eng].get("pe_safe", 0) < val:
                        self._delay(eng)
                        self.seen[eng]["pe_safe"] = val
                    return
                if self.seen[eng].get("pe_safe", 0) < val:
                    self.seen[eng]["pe_safe"] = val
        else:
            ds = ev[1]
            key = id(ds)
            sem = ds.sem
            val = ds.cnt
        if self.seen[eng].get(key, 0) >= val:
            return
        self.E[eng].wait_ge(sem, val)
        self.seen[eng][key] = val

    def _delay(self, eng):
        if eng not in self.dly:
            return
        self.nfence += 1
        d = self.dly[eng]
        if eng == "act":
            self.nc.scalar.copy(d[:, 0:256], d[:, 256:512])
        else:
            self.E[eng].memset(d[:, 0:256], 0.0)

    def _deps(self, eng, reads, writes):
        for t in reads:
            self._wait(eng, t.w, raw=True)
        for t in writes:
            self._wait(eng, t.w)
            for ev in list(t.r.values()):
                self._wait(eng, ev)

    def _mark(self, ev, key, reads, writes):
        for t in reads:
            t.r[key] = ev
        for t in writes:
            t.w = ev
            t.r = {}

    def op(self, eng, ins_fn, reads=(), writes=(), pe_n=None):
        self._deps(eng, reads, writes)
        ins = ins_fn()
        self.cnt[eng] += 1
        if eng == "pe":
            if pe_n is None:
                try:
                    pe_n = int(ins.ins.outs[0].free_size()) if False else 128
                except Exception:
                    pe_n = 128
            self.pe_cols.append(self.pe_cols[-1] + pe_n)
        ins.then_inc(self.sem[eng], 1)
        self._mark(("e", eng, self.cnt[eng]), eng, reads, writes)
        return ins

    def dma(self, q, out, in_, holder, reads=(), writes=(), **kw):
        self._deps(q, reads, writes)
        ds = self._ds(holder)
        ins = self.E[q].dma_start(out=out, in_=in_, **kw)
        ins.then_inc(ds.sem, 16)
        ds.cnt += 16
        self._mark(("d", ds), id(ds), reads, writes)

    def barrier(self):
        for ds in self.dpool[: self.dnext]:
            if ds.cnt > 0:
                self._wait("sp", ("d", ds))
        for e in self.E:
            if e != "sp":
                self._wait("sp", ("e", e, self.cnt[e]))
        self.E["sp"].sem_inc(self.sem["sp"], 1)
        self.cnt["sp"] += 1
        for e in self.E:
            if e == "sp":
                continue
            for o in self.E:
                if o != e:
                    self._wait(e, ("e", o, self.cnt[o]))
        self.dnext = self.persist

    def rep(self, name):
        print("SBUF remaining after", name, self.nc.sbuf_bytes_remaining, flush=True)

    def persist_dsems(self):
        self.persist = self.dnext

    def mm(self, out, lhsT, rhs, start, stop, reads, writes):
        n = 1
        for d_ in rhs.shape[1:]:
            n *= int(d_)
        return self.op("pe", lambda: self.nc.tensor.matmul(out, lhsT, rhs, start=start, stop=stop), reads, writes, pe_n=n)

    def tr(self, out, in_, ident, reads, writes):
        return self.op("pe", lambda: self.nc.tensor.transpose(out, in_, ident), reads, writes, pe_n=64)

    def act(self, out, in_, func, reads, writes, **kw):
        return self.op("act", lambda: self.nc.scalar.activation(out=out, in_=in_, func=func, **kw), reads, writes)

    def ts(self, eng, out, in0, s1, s2, op0, op1, reads, writes):
        eng = self.cmap(eng)
        e = self.E[eng]
        if op1 is None:
            return self.op(eng, lambda: e.tensor_scalar(out, in0, s1, None, op0), reads, writes)
        return self.op(eng, lambda: e.tensor_scalar(out, in0, s1, s2, op0, op1), reads, writes)

    def tt(self, eng, out, in0, in1, op, reads, writes):
        eng = self.cmap(eng)
        e = self.E[eng]
        return self.op(eng, lambda: e.tensor_tensor(out, in0, in1, op), reads, writes)

    def stt(self, eng, out, in0, scalar, in1, op0, op1, reads, writes):
        eng = self.cmap(eng)
        e = self.E[eng]
        return self.op(eng, lambda: e.scalar_tensor_tensor(out, in0, scalar, in1, op0, op1), reads, writes)

    def cp(self, eng, out, in_, reads, writes):
        eng = self.cmap(eng)
        if eng == "act":
            return self.op("act", lambda: self.nc.scalar.copy(out, in_), reads, writes)
        e = self.E[eng]
        return self.op(eng, lambda: e.tensor_copy(out, in_), reads, writes)

    def cmap(self, eng):
        return "dve" if (eng == "pool" and not DBG.get("pool_compute", False)) else eng

    def memset(self, eng, t, ap, val):
        eng = self.cmap(eng)
        e = self.E[eng]
        return self.op(eng, lambda: e.memset(ap, val), (), (t,))


class Ring:
    def __init__(self, tiles):
        self.t = tiles
        self.i = 0

    def next(self):
        t = self.t[self.i % len(self.t)]
        self.i += 1
        return t


def build(n_layers=DEPTH, debug_out=(), stop_after=None):
    nc = bass.Bass("TRN2", target_bir_lowering=False)
    P = Prog(nc)

    def din(name, shape, dt=F32):
        return nc.dram_tensor(name, list(shape), dt, kind="ExternalInput").ap()

    SHAPES = dict(x=[S, D], c=[D], ctx=[C, D], c_ctx=[D], ada_w=[DEPTH, D, 6 * D], ada_b=[DEPTH, 6 * D],
                  norm1_g=[DEPTH, D], norm2_g=[DEPTH, D], w_in=[DEPTH, D, WIN], diff_lambda=[DEPTH, 256],
                  diff_subln_g=[DEPTH, 128], diff_w_out=[DEPTH, D, D], conv_w=[DEPTH, 3, 512],
                  conv_w_out=[DEPTH, 512, D], gla_w_a2=[DEPTH, 2, 16, 256], gla_b_a=[DEPTH, 512],
                  gla_norm_g=[DEPTH, 128], gla_w_out=[DEPTH, 512, D], w_o=[DEPTH, D, D], router_w=[DEPTH, D, NE],
                  router_b=[DEPTH, NE], moe_w1=[DEPTH, NE, D, 2 * D], moe_b1=[DEPTH, NE, 2 * D],
                  moe_w2=[DEPTH, NE, D, D], moe_b2=[DEPTH, NE, D], final_norm_g=[D],
                  k_cos=[S, 32], k_sin=[S, 32], k_ident=[128, 128], k_tri=[6, 128, 128])

    class LazyIn(dict):
        def __missing__(self, k):
            self[k] = din(k, SHAPES[k])
            return self[k]

    I = LazyIn()
    if stop_after is None:
        for k in SHAPES:
            I[k]
    yout = nc.dram_tensor("y", [S, D], F32, kind="ExternalOutput").ap()

    def scr(name, shape, dt=F32):
        kind = "ExternalOutput" if name in debug_out else "Internal"
        return nc.dram_tensor(name, list(shape), dt, kind=kind).ap()

    X = {}
    X["XR"] = scr("XR", [T, D])
    X["MODS"] = scr("MODS", [2, 128, 6 * D])
    X["QT"] = scr("QT", [8, 128, T], BF16)
    X["KT"] = scr("KT", [8, 128, T], BF16)
    X["V"] = scr("V", [8, 128, NT * 132], BF16)
    X["CBT"] = scr("CBT", [4, 128, T])
    X["CCT"] = scr("CCT", [4, 128, T])
    X["CXT"] = scr("CXT", [4, 128, T])
    X["GQT"] = scr("GQT", [2, 128, T])
    X["GKT"] = scr("GKT", [2, 128, T])
    X["GK"] = scr("GK", [T, 256])
    X["GV"] = scr("GV", [T, 512], BF16)
    X["GR"] = scr("GR", [T, 512])
    X["GAF"] = scr("GAF", [16, T])
    X["GAB"] = scr("GAB", [16, T])
    X["SIGT"] = scr("SIGT", [24, 128, T], BF16)
    X["DIFFT"] = scr("DIFFT", [8, 128, T], BF16)
    X["YCT"] = scr("YCT", [4, 128, T], BF16)
    X["YGT"] = scr("YGT", [4, 128, T], BF16)
    X["H2T"] = scr("H2T", [8, 128, T], BF16)
    X["GATES"] = scr("GATES", [T, NE])
    if "HT" in debug_out:
        X["HT"] = scr("HT", [8, 128, T], BF16)

    with ExitStack() as gs:
        ident_f = P.sb(gs, [128, 128], F32)
        ident_b = P.sb(gs, [128, 128], BF16)
        ones_f = P.sb(gs, [128, 128], F32)
        lam = P.sb(gs, [128, 4], F32)
        subg = P.sb(gs, [128, 128], F32)
        glag = P.sb(gs, [128, 128], F32)
        P.dma("sp", ident_f[:], I["k_ident"], ident_f, writes=[ident_f])
        P.cp("dve", ident_b[:], ident_f[:], [ident_f], [ident_b])
        P.memset("dve", ones_f, ones_f[:], 1.0)
        for e_ in ("act", "dve"):
            P.dly[e_] = P.sb(gs, [128, 512], F32)
            P.memset("dve", P.dly[e_], P.dly[e_][:], 0.0)
        epsc = P.sb(gs, [128, 1], F32)
        P.memset("dve", epsc, epsc[:], EPS)
        P.persist_dsems()
        P.barrier()

        for L in range(n_layers):
            last = L == n_layers - 1 and n_layers == DEPTH
            lam_init = 0.8 - 0.6 * math.exp(-0.3 * L)
            phases = [phase0, phase1_2, phase3, phase4, phase5, phase6, phase7]
            for ph in phases:
                kk = dict(ident_f=ident_f, ident_b=ident_b, ones_f=ones_f, lam=lam, subg=subg, glag=glag, epsc=epsc)
                if ph is phase7:
                    ph(P, I, X, L, last, lam_init, kk, yout)
                else:
                    ph(P, I, X, L, last, lam_init, kk)
                P.barrier()
                if stop_after == (L, ph.__name__):
                    break
            else:
                continue
            break
        P.barrier()
    nc._used_inputs = set(I.keys())
    return nc


def phase0(P, I, X, L, last, lam_init, K):
    nc = P.nc
    with ExitStack() as es:
        cs = P.sb(es, [128, 8, 2], F32)
        crep = [P.sb(es, [128, 8, 128], BF16) for _ in range(2)]
        mod = [P.sb(es, [128, 6 * D], F32) for _ in range(2)]
        grep = [P.sb(es, [128, D], F32) for _ in range(2)]
        wring = Ring([P.sb(es, [128, 8, 512], BF16) for _ in range(2)])
        pss = Ring([P.ps(es, [128, 512]) for _ in range(4)])
        dl = P.sb(es, [128, 256], F32)
        tmp = P.sb(es, [128, 256], F32)

        with nc.allow_non_contiguous_dma(reason="tiny transposed vector load"):
            P.dma("sp", cs[:, :, 0], I["c"].rearrange("(kc p) -> p kc", p=128), cs, writes=[cs])
            P.dma("sp", cs[:, :, 1], I["c_ctx"].rearrange("(kc p) -> p kc", p=128), cs, writes=[cs])
        P.act(cs[:], cs[:], AF.Silu, [cs], [cs])
        for w in range(2):
            for kc in range(8):
                P.ts("dve", crep[w][:, kc, :], K["ones_f"][:], cs[:, kc, w:w + 1], None, ALU.mult, None,
                     [cs, K["ones_f"]], [crep[w]])
            P.dma("sp", mod[w][:], I["ada_b"][L].partition_broadcast(128), mod[w], writes=[mod[w]])
        P.dma("sp", grep[0][:], I["norm1_g"][L].partition_broadcast(128), grep[0], writes=[grep[0]])
        P.dma("sp", grep[1][:], I["norm2_g"][L].partition_broadcast(128), grep[1], writes=[grep[1]])
        aw = I["ada_w"][L].rearrange("(kc p) n -> p kc n", p=128)
        for cb in range(12):
            wt = wring.next()
            P.dma("pool", wt[:], aw[:, :, cb * 512:(cb + 1) * 512], wt, writes=[wt])
            for w in range(2):
                ps = pss.next()
                for kc in range(8):
                    P.mm(ps[:], crep[w][:, kc, :], wt[:, kc, :], kc == 0, kc == 7, [crep[w], wt], [ps])
                sl = mod[w][:, cb * 512:(cb + 1) * 512]
                P.tt("dve", sl, ps[:], sl, ALU.add, [ps, mod[w]], [mod[w]])
        for w in range(2):
            for seg, g in ((1, grep[0]), (4, grep[1])):
                sl = mod[w][:, seg * D:(seg + 1) * D]
                P.stt("dve", sl, sl, 1.0, g[:], ALU.add, ALU.mult, [mod[w], g], [mod[w]])
            P.dma("sp", X["MODS"][w], mod[w][:], mod[w], reads=[mod[w]])
        lamt = K["lam"]
        P.dma("sp", dl[:], I["diff_lambda"][L].partition_broadcast(128), dl, writes=[dl])
        P.tt("dve", tmp[:, 0:64], dl[:, 0:64], dl[:, 64:128], ALU.mult, [dl], [tmp])
        P.tt("dve", tmp[:, 64:128], dl[:, 128:192], dl[:, 192:256], ALU.mult, [dl], [tmp])
        P.op("dve", lambda: nc.vector.tensor_reduce(lamt[:, 1:3], tmp[:, 0:128].rearrange("p (a b) -> p a b", a=2),
                                                    AX.X, ALU.add), [tmp], [lamt])
        P.act(lamt[:, 1:3], lamt[:, 1:3], AF.Exp, [lamt], [lamt])
        P.tt("dve", lamt[:, 0:1], lamt[:, 1:2], lamt[:, 2:3], ALU.subtract, [lamt], [lamt])
        P.ts("dve", lamt[:, 0:1], lamt[:, 0:1], float(lam_init), None, ALU.add, None, [lamt], [lamt])
        P.dma("sp", K["subg"][:], I["diff_subln_g"][L].partition_broadcast(128), K["subg"], writes=[K["subg"]])
        P.ts("dve", K["subg"][:], K["subg"][:], float(1.0 - lam_init), None, ALU.mult, None, [K["subg"]], [K["subg"]])
        P.dma("sp", K["glag"][:], I["gla_norm_g"][L].partition_broadcast(128), K["glag"], writes=[K["glag"]])


def rstd(P, st, out_ap, in_ap, n, epsc):
    P.act(out_ap, in_ap, AF.Ln, [st, epsc], [st], scale=1.0 / n, bias=epsc[:, 0:1])
    P.act(out_ap, out_ap, AF.Exp, [st], [st], scale=-0.5)


def xsrc(I, X, L, r0):
    if L > 0:
        return X["XR"][r0:r0 + 128, :]
    if r0 < C:
        return I["ctx"][r0:r0 + 128, :]
    return I["x"][r0 - C:r0 - C + 128, :]


def sumsq(P, scr, xt, st):
    P.act(scr[:], xt[:], AF.Square, [xt], [scr])
    P.op("dve", lambda: P.nc.vector.tensor_reduce(st[:, 0:1], scr[:], AX.X, ALU.add), [scr], [st])


def norm_mod(P, xt, Gt, SHt, hb, scr_b, st, epsc, eng2="pool", xo=None):
    nc = P.nc
    sumsq(P, scr_b, xt, st)
    rstd(P, st, st[:, 1:2], st[:, 0:1], D, epsc)
    xo = xt if xo is None else xo
    P.stt("dve", xo[:], xt[:], st[:, 1:2], Gt[:], ALU.mult, ALU.mult, [xt, st, Gt], [xo])
    P.tt(eng2, hb[:], xo[:], SHt[:], ALU.add, [xo, SHt], [hb])


def phase1_2(P, I, X, L, last, lam_init, K):
    nc = P.nc
    with ExitStack() as es:
        hT = P.sb(es, [128, 8, T], BF16)
        with ExitStack() as e1:
            msl = [[P.sb(e1, [128, D], F32) for _ in range(2)] for _ in range(2)]
            for w in range(2):
                for j, seg in enumerate((0, 1)):
                    P.dma("sp", msl[w][j][:], X["MODS"][w][:, seg * D:(seg + 1) * D], msl[w][j], writes=[msl[w][j]])
            xr = Ring([P.sb(e1, [128, D], F32) for _ in range(3)])
            hbr = Ring([P.sb(e1, [128, D], BF16) for _ in range(2)])
            scr_b = P.sb(e1, [128, D], F32)
            str_ = Ring([P.sb(e1, [128, 2], F32) for _ in range(2)])
            ptr = Ring([P.ps(e1, [128, 8, 128], BF16) for _ in range(2)])
            for i in range(NT):
                w = 1 if i < 2 else 0
                xt = xr.next()
                P.dma("sp", xt[:], xsrc(I, X, L, i * 128), xt, writes=[xt])
                hb = hbr.next()
                st = str_.next()
                norm_mod(P, xt, msl[w][1], msl[w][0], hb, scr_b, st, K["epsc"])
                pt = ptr.next()
                for kc in range(8):
                    P.tr(pt[:, kc, :], hb[:, kc * 128:(kc + 1) * 128], K["ident_b"][:], [hb, K["ident_b"]], [pt])
                P.cp("act", hT[:, :, i * 128:(i + 1) * 128], pt[:], [pt], [hT])
            P.barrier()
        if "HT" in X:
            P.dma("sp", X["HT"].rearrange("c p t -> p c t"), hT[:], hT, reads=[hT])
            return
        phase2(P, I, X, L, K, hT)


def phase2(P, I, X, L, K, hT):
    nc = P.nc
    wv = I["w_in"][L].rearrange("(kc p) n -> p kc n", p=128)
    with ExitStack() as es:
        wring = Ring([P.sb(es, [128, 8, 512], BF16) for _ in range(3)])
        psr = Ring([P.ps(es, [128, 512]) for _ in range(4)])
        ptr = Ring([P.ps(es, [128, 4, 128], BF16) for _ in range(2)])
        cos = P.sb(es, [128, 32, 32], F32)
        sin = P.sb(es, [128, 32, 32], F32)
        P.dma("sp", cos[:], I["k_cos"].rearrange("(t p) f -> p t f", p=128), cos, writes=[cos])
        P.dma("sp", sin[:], I["k_sin"].rearrange("(t p) f -> p t f", p=128), sin, writes=[sin])
        ra = Ring([P.sb(es, [128, 512], F32) for _ in range(2)])
        rb = Ring([P.sb(es, [128, 512], F32) for _ in range(2)])
        rob = Ring([P.sb(es, [128, 512], BF16) for _ in range(2)])
        stq = Ring([P.sb(es, [128, 4, 512], BF16) for _ in range(2)])
        stf = Ring([P.sb(es, [128, 4, 512], F32) for _ in range(2)])

        def load_w(c0, ncols):
            wt = wring.next()
            P.dma("pool", wt[:, :, 0:ncols], wv[:, :, c0:c0 + ncols], wt, writes=[wt])
            return wt

        def tok_major(wt, cw0, ncols, i):
            ps = psr.next()
            for kc in range(8):
                P.mm(ps[:, 0:ncols], hT[:, kc, i * 128:(i + 1) * 128], wt[:, kc, cw0:cw0 + ncols],
                     kc == 0, kc == 7, [hT, wt], [ps])
            return ps

        def feat_major(wt, cw0, m, t0, n):
            ps = psr.next()
            for kc in range(8):
                P.mm(ps[0:m, 0:n], wt[:, kc, cw0:cw0 + m], hT[:, kc, t0:t0 + n], kc == 0, kc == 7, [hT, wt], [ps])
            return ps

        for which, dst, c_base in (("q", X["QT"], OQ), ("k", X["KT"], OK_)):
            for half in range(2):
                wt = load_w(c_base + half * 512, 512)
                for (t0, n) in TILES:
                    sq = stq.next()
                    for s in range(n // 128):
                        i = (t0 + s * 128) // 128
                        ps = tok_major(wt, 0, 512, i)
                        ob = rob.next()
                        if i < 2:
                            P.cp("act", ob[:], ps[:], [ps], [ob])
                        else:
                            li = i - 2
                            a = ra.next()
                            b = rb.next()
                            for ax in range(2):
                                def v4(ap):
                                    return ap.rearrange("p (h r) -> p h r", r=64)[:, :, ax * 32:(ax + 1) * 32].rearrange("p h (s f) -> p h s f", s=2)
                                x4, a4, b4, o4 = v4(ps[:]), v4(a[:]), v4(b[:]), v4(ob[:])
                                cs4 = cos[:, li, ax * 16:(ax + 1) * 16].unsqueeze(1).unsqueeze(1).broadcast_to([128, 8, 2, 16])
                                sn3 = sin[:, li, ax * 16:(ax + 1) * 16].unsqueeze(1).broadcast_to([128, 8, 16])
                                P.tt("dve", a4, x4, cs4, ALU.mult, [ps, cos], [a])
                                P.tt("dve", b4[:, :, 0, :], x4[:, :, 1, :], sn3, ALU.mult, [ps, sin], [b])
                                P.tt("dve", b4[:, :, 1, :], x4[:, :, 0, :], sn3, ALU.mult, [ps, sin], [b])
                                P.tt("pool", o4[:, :, 0, :], a4[:, :, 0, :], b4[:, :, 0, :], ALU.subtract, [a, b], [ob])
                                P.tt("pool", o4[:, :, 1, :], a4[:, :, 1, :], b4[:, :, 1, :], ALU.add, [a, b], [ob])
                        pt = ptr.next()
                        for hh in range(4):
                            P.tr(pt[:, hh, :], ob[:, hh * 128:(hh + 1) * 128], K["ident_b"][:], [ob, K["ident_b"]], [pt])
                        P.cp("act", sq[:, :, s * 128:(s + 1) * 128], pt[:], [pt], [sq])
                    P.dma("sp", dst[half * 4:(half + 1) * 4, :, t0:t0 + n].rearrange("h p t -> p h t"),
                          sq[:, :, 0:n], sq, reads=[sq])
        with ExitStack() as ev_:
            vst = P.sb(ev_, [128, 4, NT, 132], BF16)
            P.memset("dve", vst, vst[:].rearrange("p h t e -> p (h t e)"), 1.0)
            for half in range(2):
                wt = load_w(OV + half * 512, 512)
                for i in range(NT):
                    ps = tok_major(wt, 0, 512, i)
                    P.cp("act", vst[:, :, i, 0:128], ps[:].rearrange("p (h e) -> p h e", h=4), [ps], [vst])
                for hh in range(4):
                    P.dma("sp", X["V"][half * 4 + hh], vst[:, hh, :, :].rearrange("p t e -> p (t e)"), vst, reads=[vst])
        for dst, c0 in ((X["CBT"], OCB), (X["CCT"], OCC), (X["CXT"], OCX)):
            wt = load_w(c0, 512)
            for (t0, n) in TILES:
                sf = stf.next()
                for ch in range(4):
                    ps = feat_major(wt, ch * 128, 128, t0, n)
                    P.cp("act", sf[:, ch, 0:n], ps[:, 0:n], [ps], [sf])
                P.dma("sp", dst[:, :, t0:t0 + n].rearrange("c p t -> p c t"), sf[:, :, 0:n], sf, reads=[sf])
        wt = load_w(OGQ, 512)
        for (t0, n) in TILES:
            sf = stf.next()
            for ch in range(4):
                ps = feat_major(wt, ch * 128, 128, t0, n)
                P.cp("act", sf[:, ch, 0:n], ps[:, 0:n], [ps], [sf])
            P.dma("sp", X["GQT"][:, :, t0:t0 + n].rearrange("c p t -> p c t"), sf[:, 0:2, 0:n], sf, reads=[sf])
            P.dma("sp", X["GKT"][:, :, t0:t0 + n].rearrange("c p t -> p c t"), sf[:, 2:4, 0:n], sf, reads=[sf])
            sf = stf.next()
            for s in range(n // 128):
                ps = tok_major(wt, 256, 256, (t0 + s * 128) // 128)
                P.cp("act", sf[:, s, 0:256], ps[:, 0:256], [ps], [sf])
            P.dma("sp", X["GK"][t0:t0 + n, :].rearrange("(s p) c -> p s c", p=128), sf[:, 0:n // 128, 0:256], sf, reads=[sf])
        wt = load_w(OGV, 512)
        for (t0, n) in TILES:
            sq = stq.next()
            for s in range(n // 128):
                ps = tok_major(wt, 0, 512, (t0 + s * 128) // 128)
                P.cp("act", sq[:, s, :], ps[:], [ps], [sq])
            P.dma("sp", X["GV"][t0:t0 + n, :].rearrange("(s p) c -> p s c", p=128), sq[:, 0:n // 128, :], sq, reads=[sq])
        wt = load_w(OGR, 512)
        for (t0, n) in TILES:
            sf = stf.next()
            for s in range(n // 128):
                ps = tok_major(wt, 0, 512, (t0 + s * 128) // 128)
                P.act(sf[:, s, :], ps[:], AF.Silu, [ps], [sf])
            P.dma("sp", X["GR"][t0:t0 + n, :].rearrange("(s p) c -> p s c", p=128), sf[:, 0:n // 128, :], sf, reads=[sf])
        wt = load_w(OGA, 32)
        for (t0, n) in TILES:
            sf = stf.next()
            ps = feat_major(wt, 0, 32, t0, n)
            P.cp("act", sf[0:32, 0, 0:n], ps[0:32, 0:n], [ps], [sf])
            P.dma("sp", X["GAF"][:, t0:t0 + n], sf[0:16, 0, 0:n], sf, reads=[sf])
            P.dma("sp", X["GAB"][:, t0:t0 + n], sf[16:32, 0, 0:n], sf, reads=[sf])
        for gblk in range(6):
            wt = load_w(OGT + gblk * 512, 512)
            for (t0, n) in TILES:
                sq = stq.next()
                for ch in range(4):
                    ps = feat_major(wt, ch * 128, 128, t0, n)
                    P.act(sq[:, ch, 0:n], ps[:, 0:n], AF.Sigmoid, [ps], [sq])
                P.dma("sp", X["SIGT"][gblk * 4:(gblk + 1) * 4, :, t0:t0 + n].rearrange("c p t -> p c t"),
                      sq[:, :, 0:n], sq, reads=[sq])


class V(Dep):
    def __init__(self, ap):
        super().__init__()
        self.ap = ap


def phase3(P, I, X, L, last, lam_init, K):
    nc = P.nc
    with ExitStack() as es:
        ktr = Ring([P.sb(es, [128, T], BF16) for _ in range(2)])
        qtr = Ring([P.sb(es, [128, T], BF16) for _ in range(2)])
        vtr = Ring([P.sb(es, [128, NT, 132], BF16) for _ in range(2)])
        pss = Ring([P.ps(es, [128, 1024]) for _ in range(2)])
        accT = P.ps(es, [128, 1536])
        ptT = Ring([P.ps(es, [128, 2, 128], BF16) for _ in range(1)])
        offs = [0, 160, 320, 512, 672, 832, 1024, 1184]
        accv = [[V(accT[:, offs[m * 4 + s]:offs[m * 4 + s] + 129]) for s in range(4)] for m in range(2)]
        ptr_ = Ring([P.sb(es, [128, 1024], BF16) for _ in range(3)])
        evr = Ring([P.sb(es, [128, 2, 132], F32) for _ in range(3)])
        t1r = Ring([P.sb(es, [128, 128], F32) for _ in range(2)])
        o_r = Ring([P.sb(es, [128, 128], F32) for _ in range(2)])
        jk = P.sb(es, [128, 128], F32)
        obr = Ring([P.sb(es, [128, 128], BF16) for _ in range(2)])
        str_ = Ring([P.sb(es, [128, 4], F32) for _ in range(3)])
        dstr = Ring([P.sb(es, [128, 512], BF16) for _ in range(2)])
        lam = K["lam"]
        qtiles = [(256 + 512 * i, 512, list(range(NT))) for i in range(8)]
        if not last:
            qtiles = [(0, 256, [0, 1])] + qtiles
        if DBG.get("p3_qtiles") is not None:
            qtiles = [qtiles[i] for i in DBG["p3_qtiles"]]
        for h in range(DBG.get("p3_heads", 8)):
            kt, qt, vt = ktr.next(), qtr.next(), vtr.next()
            P.dma("sp", kt[:], X["KT"][h], kt, writes=[kt])
            P.dma("sp", qt[:], X["QT"][h], qt, writes=[qt])
            P.dma("sp", vt[:].rearrange("p t e -> p (t e)"), X["V"][h], vt, writes=[vt])
            for (q0, n, ktl) in qtiles:
                nsub = n // 128
                for ki, kk in enumerate(ktl):
                    ps = pss.next()
                    for m in range(2):
                        P.mm(ps[:, m * 512:m * 512 + n], kt[m * 64:(m + 1) * 64, kk * 128:(kk + 1) * 128],
                             qt[m * 64:(m + 1) * 64, q0:q0 + n], True, True, [kt, qt], [ps])
                    pt = ptr_.next()
                    if True:
                        for m in range(2):
                            P.act(pt[:, m * 512:m * 512 + n], ps[:, m * 512:m * 512 + n], AF.Exp, [ps], [pt], scale=0.125)
                    if ki == 0:
                        started = set()
                    for m in range(2):
                        for s in range(nsub):
                            av = accv[m][s]
                            bank = offs[m * 4 + s] // 512
                            st_flag = ki == 0 and bank not in started
                            started.add(bank)
                            P.op("pe", lambda: nc.tensor.matmul(av.ap, pt[:, m * 512 + s * 128:m * 512 + (s + 1) * 128],
                                                                vt[:, kk, 0:129], start=st_flag, stop=(ki == len(ktl) - 1),
                                                                skip_group_check=True), [pt, vt], [av], pe_n=129)
                dst = dstr.next()
                for s in range(nsub):
                    ev = evr.next()
                    st = str_.next()
                    for m in range(2):
                        rd = [accv[m][s]] + ([accv[1][nsub - 1]] if DBG.get("h1") else [])
                        P.cp("dve", ev[:, m, 0:129], accv[m][s].ap, rd, [ev])
                    P.op("dve", lambda: nc.vector.reciprocal(st[:, 0:2], ev[:, :, 128]), [ev], [st])
                    P.tt("dve", st[:, 1:2], st[:, 1:2], lam[:, 0:1], ALU.mult, [st, lam], [st])
                    t1 = t1r.next()
                    o = o_r.next()
                    P.ts("pool", t1[:], ev[:, 1, 0:128], st[:, 1:2], None, ALU.mult, None, [ev, st], [t1])
                    P.stt("dve", o[:], ev[:, 0, 0:128], st[:, 0:1], t1[:], ALU.mult, ALU.subtract, [ev, st, t1], [o])
                    P.tt("pool", jk[:], o[:], o[:], ALU.mult, [o], [jk])
                    P.op("dve", lambda: nc.vector.tensor_reduce(st[:, 2:3], jk[:], AX.X, ALU.add), [jk], [st])
                    rstd(P, st, st[:, 2:3], st[:, 2:3], 128, K["epsc"])
                    ob = obr.next()
                    P.stt("dve", ob[:], o[:], st[:, 2:3], K["subg"][:], ALU.mult, ALU.mult, [o, st, K["subg"]], [ob])
                    pT = ptT.next()
                    P.tr(pT[:, 0, :], ob[:], K["ident_b"][:], [ob, K["ident_b"]], [pT])
                    P.cp("dve", dst[:, s * 128:(s + 1) * 128], pT[:, 0, :], [pT], [dst])
                P.dma("sp", X["DIFFT"][h, :, q0:q0 + n], dst[:, 0:n], dst, reads=[dst])


def phase4(P, I, X, L, last, lam_init, K):
    nc = P.nc
    with ExitStack() as es:
        cw = P.sb(es, [128, 4, 3], F32)
        with nc.allow_non_contiguous_dma(reason="tiny conv taps"):
            for k_ in range(3):
                P.dma("sp", cw[:, :, k_], I["conv_w"][L, k_].rearrange("(c p) -> p c", p=128), cw, writes=[cw])
        zero = P.sb(es, [128, 8], F32)
        P.memset("dve", zero, zero[:], 0.0)
        cb = P.sb(es, [128, T], F32)
        c_ = P.sb(es, [128, T], F32)
        cx = P.sb(es, [128, T], F32)
        up = P.sb(es, [128, T], F32)
        un = P.sb(es, [128, T], F32)
        y = P.sb(es, [128, T], F32)
        yb = P.sb(es, [128, T], BF16)
        for cc in range(4):
            P.dma("sp", cb[:], X["CBT"][cc], cb, writes=[cb])
            P.dma("sp", c_[:], X["CCT"][cc], c_, writes=[c_])
            P.dma("sp", cx[:], X["CXT"][cc], cx, writes=[cx])
            P.tt("dve", c_[:], c_[:], cx[:], ALU.mult, [c_, cx], [c_])
            P.dma("sp", up[:, 1:T], c_[:, 0:T - 1], up, reads=[c_], writes=[up])
            P.dma("sp", un[:, 0:T - 1], c_[:, 1:T], un, reads=[c_], writes=[un])
            for col in (0, C):
                P.dma("sp", up[:, col:col + 1], zero[:, 0:1], up, reads=[zero], writes=[up])
            for col in (C - 1, T - 1):
                P.dma("sp", un[:, col:col + 1], zero[:, 0:1], un, reads=[zero], writes=[un])
            P.ts("dve", y[:], c_[:], cw[:, cc, 1:2], None, ALU.mult, None, [c_, cw], [y])
            P.stt("dve", y[:], up[:], cw[:, cc, 0:1], y[:], ALU.mult, ALU.add, [up, cw, y], [y])
            P.stt("dve", y[:], un[:], cw[:, cc, 2:3], y[:], ALU.mult, ALU.add, [un, cw, y], [y])
            P.tt("dve", yb[:], y[:], cb[:], ALU.mult, [y, cb], [yb])
            P.dma("sp", X["YCT"][cc], yb[:], yb, reads=[yb])


def phase5(P, I, X, L, last, lam_init, K):
    nc = P.nc
    NCH = NT
    with ExitStack() as es:
        tri = P.sb(es, [128, 6, 128], F32)
        P.dma("sp", tri[:], I["k_tri"].rearrange("s m l -> m s l"), tri, writes=[tri])
        wa = P.sb(es, [16, 2, 256], F32)
        P.dma("sp", wa[:], I["gla_w_a2"][L].rearrange("d k n -> k d n"), wa, writes=[wa])
        ba = P.sb(es, [128, 512], F32)
        P.dma("sp", ba[:], I["gla_b_a"][L].partition_broadcast(128), ba, writes=[ba])
        neg16 = P.sb(es, [128, 2], F32)
        P.memset("dve", neg16, neg16[:], -1.0 / 16.0)
        Sf = P.sb(es, [128, 2, 128], F32)
        Sfb = P.sb(es, [128, 2, 128], BF16)
        Sb = P.sb(es, [128, 2, 128], F32)
        SLB = P.sb(es, [128, NCH, 2, 128], F32)
        DECB = P.sb(es, [128, NCH, 2], F32)
        SBP = P.sb(es, [128, NCH, 2, 128], BF16)
        for t_ in (Sf, Sb):
            P.memset("dve", t_, t_[:], 0.0)
        P.memset("dve", Sfb, Sfb[:], 0.0)
        psA = P.ps(es, [128, 512])
        psB = P.ps(es, [128, 512])
        psC = P.ps(es, [128, 1024])
        psO = P.ps(es, [128, 512])
        psS = P.ps(es, [128, 512])
        psT = P.ps(es, [128, 4, 128], BF16)
        gqr = Ring([P.sb(es, [128, 2, 512], F32) for _ in range(2)])
        gkr = Ring([P.sb(es, [128, 2, 512], F32) for _ in range(2)])
        gktr = Ring([P.sb(es, [128, 256], F32) for _ in range(2)])
        gvr = Ring([P.sb(es, [128, 512], BF16) for _ in range(2)])
        grr = Ring([P.sb(es, [128, 512], F32) for _ in range(2)])
        gar = [Ring([P.sb(es, [16, 512], F32) for _ in range(2)]) for _ in range(2)]
        zbr = Ring([P.sb(es, [128, 256], F32) for _ in range(2)])
        spr = Ring([P.sb(es, [128, 256], F32) for _ in range(2)])
        e1r = Ring([P.sb(es, [128, 256], F32) for _ in range(2)])
        e2r = Ring([P.sb(es, [128, 256], F32) for _ in range(2)])
        e3r = Ring([P.sb(es, [128, 256], F32) for _ in range(2)])
        decr = Ring([P.sb(es, [128, 2], F32) for _ in range(2)])
        qdr = [Ring([P.sb(es, [128, 2, 128], BF16) for _ in range(2)]) for _ in range(2)]
        kir = [Ring([P.sb(es, [128, 2, 128], BF16) for _ in range(2)]) for _ in range(2)]
        ker = Ring([P.sb(es, [128, 256], BF16) for _ in range(2)])
        atr = [Ring([P.sb(es, [128, 4, 128], BF16) for _ in range(2)]) for _ in range(2)]
        osr = Ring([P.sb(es, [128, 512], F32) for _ in range(2)])
        sqj = P.sb(es, [128, 512], F32)
        onr = Ring([P.sb(es, [128, 512], F32) for _ in range(2)])
        obr = Ring([P.sb(es, [128, 512], BF16) for _ in range(2)])
        ygr = Ring([P.sb(es, [128, 4, 512], BF16) for _ in range(2)])

        def tile_of(c):
            if c < 2:
                return 0, 256, c * 128
            j = (c - 2) // 4
            return 256 + 512 * j, 512, ((c - 2) % 4) * 128
        str_ = Ring([P.sb(es, [128, 8], F32) for _ in range(2)])
        GAsrc = (X["GAF"], X["GAB"])

        def softplus_neg(c, d, ga, off):
            P.mm(psA[:, 0:256], ga[0:16, off:off + 128], wa[0:16, d, :], True, True, [ga, wa], [psA])
            zb = zbr.next()
            P.tt("dve", zb[:], psA[:, 0:256], ba[:, d * 256:(d + 1) * 256], ALU.add, [psA, ba], [zb])
            P.act(zb[:], zb[:], AF.Exp, [zb], [zb], scale=-1.0)
            sp = spr.next()
            P.act(sp[:], zb[:], AF.Ln, [zb], [sp], bias=1.0)
            return sp

        def tot_dec(sp, dec_ap, dec_t):
            for hp in range(2):
                P.mm(psA[:, 256 + hp:257 + hp], sp[:, hp * 128:(hp + 1) * 128], neg16[:, 0:1], True, True, [sp, neg16], [psA])
            P.act(dec_ap, psA[:, 256:258], AF.Exp, [psA], [dec_t])

        def ke_of(sp, d, gkt):
            P.mm(psB[:, 256:512], tri[:, 2 + d, :], sp[:], True, True, [tri, sp], [psB])
            e3 = e3r.next()
            P.act(e3[:], psB[:, 256:512], AF.Exp, [psB], [e3])
            ke = ker.next()
            P.tt("pool", ke[:], gkt[:], e3[:], ALU.mult, [gkt, e3], [ke])
            return ke

        def sloc(ke, gv):
            for hp in range(2):
                P.mm(psS[:, hp * 256:(hp + 1) * 256], ke[:, hp * 128:(hp + 1) * 128], gv[:, hp * 256:(hp + 1) * 256],
                     True, True, [ke, gv], [psS])

        for c in range(NCH):
            gkt, gv = gktr.next(), gvr.next()
            t0_, tn_, off_ = tile_of(c)
            if off_ == 0:
                gab_t = gar[1].next()
                P.dma("sp", gab_t[:, 0:tn_], X["GAB"][:, t0_:t0_ + tn_], gab_t, writes=[gab_t])
            P.dma("sp", gkt[:], X["GK"][c * 128:(c + 1) * 128, :], gkt, writes=[gkt])
            P.dma("sp", gv[:], X["GV"][c * 128:(c + 1) * 128, :], gv, writes=[gv])
            stg = DBG.get("p5_stage", 99)
            sp = softplus_neg(c, 1, gab_t, off_)
            if stg < 2:
                continue
            tot_dec(sp, DECB[:, c, :], DECB)
            if stg < 3:
                continue
            ke = ke_of(sp, 1, gkt)
            if stg < 4:
                continue
            sloc(ke, gv)
            for hp in range(2):
                for hh in range(2):
                    P.cp("act", SLB[hh * 64:(hh + 1) * 64, c, hp, :],
                         psS[hh * 64:(hh + 1) * 64, hp * 256 + hh * 128:hp * 256 + (hh + 1) * 128], [psS], [SLB])
        if DBG.get("p5_stage", 99) < 5:
            return
        for c in [1, 0] + list(range(NCH - 1, 1, -1)):
            P.cp("pool", SBP[:, c, :, :], Sb[:], [Sb], [SBP])
            for hp in range(2):
                P.stt("dve", Sb[:, hp, :], Sb[:, hp, :], DECB[:, c, hp:hp + 1], SLB[:, c, hp, :], ALU.mult, ALU.add,
                      [Sb, DECB, SLB], [Sb])
        if DBG.get("p5_stage", 99) < 6:
            return
        stg = DBG.get("p5_stage", 99)
        for c in range(NCH):
            gkt, gv, gr = gktr.next(), gvr.next(), grr.next()
            cs = slice(c * 128, (c + 1) * 128)
            t0_, tn_, off_ = tile_of(c)
            osl = slice(off_, off_ + 128)
            if off_ == 0:
                gq_t, gk_t, yg = gqr.next(), gkr.next(), ygr.next()
                ga = [gar[0].next(), gar[1].next()]
                ts_ = slice(t0_, t0_ + tn_)
                P.dma("sp", gq_t[:, :, 0:tn_], X["GQT"][:, :, ts_].rearrange("c p t -> p c t"), gq_t, writes=[gq_t])
                P.dma("sp", gk_t[:, :, 0:tn_], X["GKT"][:, :, ts_].rearrange("c p t -> p c t"), gk_t, writes=[gk_t])
                for d in range(2):
                    P.dma("sp", ga[d][:, 0:tn_], GAsrc[d][:, ts_], ga[d], writes=[ga[d]])
            P.dma("sp", gkt[:], X["GK"][cs, :], gkt, writes=[gkt])
            P.dma("sp", gv[:], X["GV"][cs, :], gv, writes=[gv])
            P.dma("sp", gr[:], X["GR"][cs, :], gr, writes=[gr])
            qd, ki, atm = [None, None], [None, None], [None, None]
            dec = decr.next()
            ke = None
            for d in range(2):
                sp = softplus_neg(c, d, ga[d], off_)
                for hp in range(2):
                    P.mm(psB[:, hp * 128:(hp + 1) * 128], sp[:, hp * 128:(hp + 1) * 128], tri[:, d, :], True, True,
                         [sp, tri], [psB])
                e1, e2 = e1r.next(), e2r.next()
                P.act(e1[:], psB[:, 0:256], AF.Exp, [psB], [e1])
                P.act(e2[:], psB[:, 0:256], AF.Exp, [psB], [e2], scale=-1.0)
                qd[d], ki[d] = qdr[d].next(), kir[d].next()
                P.stt("dve", qd[d][:], gq_t[:, :, osl], 0.125, e1[:].rearrange("p (a b) -> p a b", a=2),
                      ALU.mult, ALU.mult, [gq_t, e1], [qd[d]])
                P.tt("pool", ki[d][:], gk_t[:, :, osl], e2[:].rearrange("p (a b) -> p a b", a=2), ALU.mult,
                     [gk_t, e2], [ki[d]])
                if stg < 7:
                    continue
                if d == 0:
                    tot_dec(sp, dec[:], dec)
                    ke = ke_of(sp, 0, gkt)
                if stg < 7.3:
                    continue
                for h in range(4):
                    hp, b0 = h // 2, (h % 2) * 64
                    co = (h % 2) * 512 + (d * 2 + hp) * 128
                    P.mm(psC[:, co:co + 128], ki[d][b0:b0 + 64, hp, :], qd[d][b0:b0 + 64, hp, :],
                         True, True, [ki[d], qd[d]], [psC])
                if stg < 7.6:
                    continue
                atm[d] = atr[d].next()
                P.tt("dve", atm[d][:].rearrange("p (hp par) l -> p hp par l", par=2),
                     psC[:].rearrange("p (par d hp l) -> p d hp par l", par=2, d=2, hp=2)[:, d],
                     tri[:, 4 + d, :].unsqueeze(1).unsqueeze(1).broadcast_to([128, 2, 2, 128]), ALU.mult, [psC, tri], [atm[d]])
            if stg < 8:
                continue
            for h in range(4):
                hp, b0 = h // 2, (h % 2) * 64
                oo = psO[:, h * 128:(h + 1) * 128]
                vv = gv[:, h * 128:(h + 1) * 128]
                P.mm(oo, atm[0][:, h, :], vv, True, False, [atm[0], gv], [psO])
                P.mm(oo, qd[0][b0:b0 + 64, hp, :], Sfb[b0:b0 + 64, hp, :], False, False, [qd[0], Sfb], [psO])
                P.mm(oo, atm[1][:, h, :], vv, False, False, [atm[1], gv], [psO])
                P.mm(oo, qd[1][b0:b0 + 64, hp, :], SBP[b0:b0 + 64, c, hp, :], False, True, [qd[1], SBP], [psO])
            sloc(ke, gv)
            for hp in range(2):
                for hh in range(2):
                    rs = slice(hh * 64, (hh + 1) * 64)
                    P.stt("dve", Sf[rs, hp, :], Sf[rs, hp, :], dec[rs, hp:hp + 1],
                          psS[rs, hp * 256 + hh * 128:hp * 256 + (hh + 1) * 128], ALU.mult, ALU.add, [Sf, dec, psS], [Sf])
            P.cp("pool", Sfb[:], Sf[:], [Sf], [Sfb])
            if stg < 9:
                continue
            osb, on, ob, st = osr.next(), onr.next(), obr.next(), str_.next()
            P.cp("act", osb[:], psO[:], [psO], [osb])
            P.tt("pool", sqj[:], osb[:], osb[:], ALU.mult, [osb], [sqj])
            P.op("dve", lambda: nc.vector.tensor_reduce(st[:, 0:4], sqj[:].rearrange("p (h e) -> p h e", h=4), AX.X, ALU.add),
                 [sqj], [st])
            rstd(P, st, st[:, 0:4], st[:, 0:4], 128, K["epsc"])
            o3 = osb[:].rearrange("p (h e) -> p h e", h=4)
            n3 = on[:].rearrange("p (h e) -> p h e", h=4)
            P.tt("dve", n3, o3, st[:, 0:4].unsqueeze(2).broadcast_to([128, 4, 128]), ALU.mult, [osb, st], [on])
            P.tt("pool", n3, n3, K["glag"][:].unsqueeze(1).broadcast_to([128, 4, 128]), ALU.mult, [on, K["glag"]], [on])
            P.tt("dve", ob[:], on[:], gr[:], ALU.mult, [on, gr], [ob])
            for h in range(4):
                P.tr(psT[:, h, :], ob[:, h * 128:(h + 1) * 128], K["ident_b"][:], [ob, K["ident_b"]], [psT])
            P.cp("act", yg[:, :, osl], psT[:], [psT], [yg])
            if off_ + 128 == tn_:
                P.dma("sp", X["YGT"][:, :, t0_:t0_ + tn_].rearrange("c p t -> p c t"), yg[:, :, 0:tn_], yg, reads=[yg])


def phase6(P, I, X, L, last, lam_init, K):
    nc = P.nc
    with ExitStack() as es:
        WD = P.sb(es, [128, 8, D], BF16)
        WC = P.sb(es, [128, 4, D], BF16)
        WG = P.sb(es, [128, 4, D], BF16)
        WO = P.sb(es, [128, 8, D], BF16)
        for wt, nm in ((WD, "diff_w_out"), (WC, "conv_w_out"), (WG, "gla_w_out"), (WO, "w_o")):
            P.dma("pool", wt[:], I[nm][L].rearrange("(kc p) n -> p kc n", p=128), wt, writes=[wt])
        RW = P.sb(es, [128, 8, NE], F32)
        P.dma("sp", RW[:], I["router_w"][L].rearrange("(kc p) e -> p kc e", p=128), RW, writes=[RW])
        RB = P.sb(es, [128, NE], F32)
        P.dma("sp", RB[:], I["router_b"][L].partition_broadcast(128), RB, writes=[RB])
        nw = 1 if last else 2
        msl = [[P.sb(es, [128, D], F32) for _ in range(3)] for _ in range(nw)]
        for w in range(nw):
            for j, seg in enumerate((2, 3, 4)):
                P.dma("sp", msl[w][j][:], X["MODS"][w][:, seg * D:(seg + 1) * D], msl[w][j], writes=[msl[w][j]])
        dTr = Ring([P.sb(es, [128, 8, 512], BF16) for _ in range(2)])
        ycr = Ring([P.sb(es, [128, 4, 512], BF16) for _ in range(2)])
        ygr = Ring([P.sb(es, [128, 4, 512], BF16) for _ in range(2)])
        sgr = Ring([P.sb(es, [128, 3, 512], BF16) for _ in range(3)])
        mgr = Ring([P.sb(es, [128, 8, 512], BF16) for _ in range(1)])
        m1r = Ring([P.sb(es, [128, 512], F32) for _ in range(2)])
        m2r = Ring([P.sb(es, [128, 512], F32) for _ in range(2)])
        m3r = Ring([P.sb(es, [128, 512], F32) for _ in range(2)])
        xr = Ring([P.sb(es, [128, D], F32) for _ in range(2)])
        xnr = Ring([P.sb(es, [128, D], F32) for _ in range(1)])
        tmr = Ring([P.sb(es, [128, 512], F32) for _ in range(2)])
        hbr = Ring([P.sb(es, [128, D], F32) for _ in range(1)])
        scr_b = P.sb(es, [128, D], F32)
        str_ = Ring([P.sb(es, [128, 2], F32) for _ in range(2)])
        h32r = Ring([P.sb(es, [128, 8, 128], F32) for _ in range(1)])
        h2st = Ring([P.sb(es, [128, 8, 512], BF16) for _ in range(1)])
        gtst = Ring([P.sb(es, [128, 4, NE], F32) for _ in range(2)])
        lgr = Ring([P.sb(es, [128, NE], F32) for _ in range(2)])
        er = Ring([P.sb(es, [128, NE], F32) for _ in range(2)])
        mkr = Ring([P.sb(es, [128, NE], F32) for _ in range(2)])
        m8r = Ring([P.sb(es, [128, 16], F32) for _ in range(2)])
        psbr = Ring([P.ps(es, [128, 512]) for _ in range(3)])
        psor = Ring([P.ps(es, [128, 512]) for _ in range(2)])
        ptr = P.ps(es, [128, 8, 128], F32)
        pslg = P.ps(es, [128, 512])
        sig4 = X["SIGT"].rearrange("(b c) p t -> c p b t", b=3)
        tiles = TILES[1:] if last else TILES
        for (t0, n) in tiles:
            w = 1 if t0 == 0 else 0
            g1, sh2, G2 = msl[w]
            dT, yc, yg = dTr.next(), ycr.next(), ygr.next()
            P.dma("sp", dT[:, :, 0:n], X["DIFFT"][:, :, t0:t0 + n].rearrange("c p t -> p c t"), dT, writes=[dT])
            P.dma("sp", yc[:, :, 0:n], X["YCT"][:, :, t0:t0 + n].rearrange("c p t -> p c t"), yc, writes=[yc])
            P.dma("sp", yg[:, :, 0:n], X["YGT"][:, :, t0:t0 + n].rearrange("c p t -> p c t"), yg, writes=[yg])
            mg = mgr.next()
            for c in range(8):
                sg = sgr.next()
                P.dma("sp", sg[:, :, 0:n], sig4[c][:, :, t0:t0 + n], sg, writes=[sg])
                cs = slice(c * 128, (c + 1) * 128)
                pd, pc, pg = psbr.next(), psbr.next(), psbr.next()
                for kc in range(8):
                    P.mm(pd[:, 0:n], WD[:, kc, cs], dT[:, kc, 0:n], kc == 0, kc == 7, [WD, dT], [pd])
                for kc in range(4):
                    P.mm(pc[:, 0:n], WC[:, kc, cs], yc[:, kc, 0:n], kc == 0, kc == 3, [WC, yc], [pc])
                for kc in range(4):
                    P.mm(pg[:, 0:n], WG[:, kc, cs], yg[:, kc, 0:n], kc == 0, kc == 3, [WG, yg], [pg])
                m1, m2, m3 = m1r.next(), m2r.next(), m3r.next()
                P.tt("dve", m1[:, 0:n], pd[:, 0:n], sg[:, 0, 0:n], ALU.mult, [pd, sg], [m1])
                P.tt("dve", m2[:, 0:n], pc[:, 0:n], sg[:, 1, 0:n], ALU.mult, [pc, sg], [m2])
                P.tt("dve", m3[:, 0:n], pg[:, 0:n], sg[:, 2, 0:n], ALU.mult, [pg, sg], [m3])
                P.tt("pool", m1[:, 0:n], m1[:, 0:n], m2[:, 0:n], ALU.add, [m1, m2], [m1])
                P.tt("pool", mg[:, c, 0:n], m1[:, 0:n], m3[:, 0:n], ALU.add, [m1, m3], [mg])
            h2s = h2st.next()
            gts = gtst.next()
            stg6 = DBG.get("p6_stage", 99)
            if stg6 < 2:
                continue
            for s in range(n // 128):
                r0 = t0 + s * 128
                xt, xn = xr.next(), xnr.next()
                P.dma("sp", xt[:], xsrc(I, X, L, r0), xt, writes=[xt])
                for hf in range(2):
                    po = psor.next()
                    hs = slice(hf * 512, (hf + 1) * 512)
                    for kc in range(8):
                        P.mm(po[:], mg[:, kc, s * 128:(s + 1) * 128], WO[:, kc, hs], kc == 0, kc == 7, [mg, WO], [po])
                    tm = tmr.next()
                    P.tt("dve", tm[:], po[:], g1[:, hs], ALU.mult, [po, g1], [tm])
                    P.tt("pool", xt[:, hs], xt[:, hs], tm[:], ALU.add, [xt, tm], [xt])
                P.dma("sp", X["XR"][r0:r0 + 128, :], xt[:], xt, reads=[xt])
                if stg6 < 3:
                    continue
                hb, st = hbr.next(), str_.next()
                norm_mod(P, xt, G2, sh2, hb, scr_b, st, K["epsc"], xo=xn)
                if stg6 < 3.3:
                    continue
                for kc in range(8):
                    P.mm(ptr[:, kc, :], hb[:, kc * 128:(kc + 1) * 128], K["ident_f"][:], True, True, [hb, K["ident_f"]], [ptr])
                if stg6 < 3.6:
                    continue
                h32 = h32r.next()
                P.cp("act", h32[:], ptr[:], [ptr], [h32])
                if stg6 < 3.8:
                    continue
                P.cp("act", h2s[:, :, s * 128:(s + 1) * 128], ptr[:], [ptr], [h2s])
                if stg6 < 4:
                    continue
                for kc in range(8):
                    P.mm(pslg[:, 0:NE], h32[:, kc, :], RW[:, kc, :], kc == 0, kc == 7, [h32, RW], [pslg])
                lg, e_, mk, m8 = lgr.next(), er.next(), mkr.next(), m8r.next()
                P.tt("dve", lg[:], pslg[:, 0:NE], RB[:], ALU.add, [pslg, RB], [lg])
                P.op("dve", lambda: nc.vector.max(m8[:, 0:8], lg[:]), [lg], [m8])
                P.ts("dve", m8[:, 8:9], m8[:, 0:1], -1.0, None, ALU.mult, None, [m8], [m8])
                P.ts("dve", mk[:], lg[:], m8[:, 3:4], None, ALU.is_ge, None, [lg, m8], [mk])
                P.act(e_[:], lg[:], AF.Exp, [lg, m8], [e_], bias=m8[:, 8:9])
                P.tt("dve", e_[:], e_[:], mk[:], ALU.mult, [e_, mk], [e_])
                P.op("dve", lambda: nc.vector.tensor_reduce(m8[:, 9:10], e_[:], AX.X, ALU.add), [e_], [m8])
                P.op("dve", lambda: nc.vector.reciprocal(m8[:, 10:11], m8[:, 9:10]), [m8], [m8])
                P.ts("dve", gts[:, s, :], e_[:], m8[:, 10:11], None, ALU.mult, None, [e_, m8], [gts])
            if stg6 < 4:
                continue
            P.dma("sp", X["H2T"][:, :, t0:t0 + n].rearrange("c p t -> p c t"), h2s[:, :, 0:n], h2s, reads=[h2s])
            P.dma("sp", X["GATES"][t0:t0 + n, :].rearrange("(s p) e -> p s e", p=128), gts[:, 0:n // 128, :], gts, reads=[gts])


def phase7(P, I, X, L, last, lam_init, K, yout):
    nc = P.nc
    with ExitStack() as es:
        nw = 1 if last else 2
        g2 = [P.sb(es, [128, D], F32) for _ in range(nw)]
        for w in range(nw):
            P.dma("sp", g2[w][:], X["MODS"][w][:, 5 * D:6 * D], g2[w], writes=[g2[w]])
        B1T = P.sb(es, [128, NE, 2, 8], F32)
        with nc.allow_non_contiguous_dma(reason="bias de-interleave"):
            for e in range(NE):
                for s_ in range(2):
                    P.dma("sp", B1T[:, e, s_, :], I["moe_b1"][L, e].rearrange("(j p s) -> s p j", p=128, s=2)[s_], B1T, writes=[B1T])
        B2 = P.sb(es, [NE, D], F32)
        P.dma("sp", B2[:], I["moe_b2"][L], B2, writes=[B2])
        FG = None
        if last:
            FG = P.sb(es, [128, D], F32)
            P.dma("sp", FG[:], I["final_norm_g"].partition_broadcast(128), FG, writes=[FG])
        acc = P.sb(es, [128, 8, D], F32)
        h2 = Ring([P.sb(es, [128, 8, 1024], BF16) for _ in range(1)])
        gtr = Ring([P.sb(es, [128, 8, NE], F32) for _ in range(1)])
        gT = Ring([P.sb(es, [NE, 128], F32) for _ in range(2)])
        w1r = Ring([P.sb(es, [128, 8, 512], BF16) for _ in range(5)])
        w2r = Ring([P.sb(es, [128, 8, 512], BF16) for _ in range(4)])
        actr = Ring([P.sb(es, [128, 8, 512], BF16) for _ in range(1)])
        glr = Ring([P.sb(es, [128, 512], F32) for _ in range(2)])
        sgr = Ring([P.sb(es, [128, 512], F32) for _ in range(2)])
        lnr = Ring([P.sb(es, [128, 512], F32) for _ in range(2)])
        xr = Ring([P.sb(es, [128, D], F32) for _ in range(2)])
        scr_b = P.sb(es, [128, D], F32)
        str_ = Ring([P.sb(es, [128, 2], F32) for _ in range(2)])
        psg = Ring([P.ps(es, [128, 512]) for _ in range(2)])
        psl = Ring([P.ps(es, [128, 512]) for _ in range(2)])
        pso = Ring([P.ps(es, [128, 512]) for _ in range(3)])
        pst = P.ps(es, [128, 512])
        if last:
            tiles = [(C + 1024 * i, 1024) for i in range(4)]
        else:
            tiles = [(0, C)] + [(C + 1024 * i, 1024) for i in range(4)]
        w1v = I["moe_w1"][L].rearrange("e (kc p) n -> e p kc n", p=128)
        w2v = I["moe_w2"][L].rearrange("e (kc p) n -> e p kc n", p=128)
        for (t0, n) in tiles:
            w = 1 if t0 == 0 else 0
            nsub = n // 128
            h2t, gt = h2.next(), gtr.next()
            P.dma("sp", h2t[:, :, 0:n], X["H2T"][:, :, t0:t0 + n].rearrange("c p t -> p c t"), h2t, writes=[h2t])
            P.dma("sp", gt[:, 0:nsub, :], X["GATES"][t0:t0 + n, :].rearrange("(s p) e -> p s e", p=128), gt, writes=[gt])
            for s in range(nsub):
                P.mm(pst[0:NE, 0:128], gt[:, s, :], K["ident_f"][:], True, True, [gt, K["ident_f"]], [pst])
                g_t = gT.next()
                P.cp("act", g_t[:], pst[0:NE, 0:128], [pst], [g_t])
                for hf in range(2):
                    po = pso.next()
                    P.mm(po[:], g_t[:], B2[:, hf * 512:(hf + 1) * 512], True, True, [g_t, B2], [po])
                    P.cp("act", acc[:, s, hf * 512:(hf + 1) * 512], po[:], [po], [acc])
            W1, W2 = {}, {}

            def load_w1(e, q):
                t_ = w1r.next()
                P.dma("pool", t_[:], w1v[e][:, :, q * 512:(q + 1) * 512], t_, writes=[t_])
                W1[(e, q)] = t_

            def load_w2(e, hf):
                t_ = w2r.next()
                P.dma("pool", t_[:], w2v[e][:, :, hf * 512:(hf + 1) * 512], t_, writes=[t_])
                W2[(e, hf)] = t_

            for q in range(4):
                load_w1(0, q)
            for hf in range(2):
                load_w2(0, hf)
            subtiles = [(j0, min(512, n - j0)) for j0 in range(0, n, 512)]
            for e in range(NE):
                for si, (j0, nj) in enumerate(subtiles):
                    pre = si == len(subtiles) - 1 and e + 1 < NE
                    at = actr.next()
                    for q in range(4):
                        w1s = W1[(e, q)]
                        for gch in range(2):
                            fc = q * 2 + gch
                            pg, pl = psg.next(), psl.next()
                            for sidx, pp in ((0, pg), (1, pl)):
                                for kc in range(8):
                                    P.mm(pp[:, 0:nj], w1s[:, kc, gch * 256 + sidx:gch * 256 + 256:2],
                                         h2t[:, kc, j0:j0 + nj], kc == 0, kc == 7, [w1s, h2t], [pp])
                            gl, sg, ln = glr.next(), sgr.next(), lnr.next()
                            P.ts("dve", gl[:, 0:nj], pg[:, 0:nj], B1T[:, e, 0, fc:fc + 1], 7.0, ALU.add, ALU.min, [pg, B1T], [gl])
                            P.act(sg[:, 0:nj], gl[:, 0:nj], AF.Sigmoid, [gl], [sg], scale=1.702)
                            P.ts("dve", ln[:, 0:nj], pl[:, 0:nj], B1T[:, e, 1, fc:fc + 1], 7.0, ALU.add, ALU.min, [pl, B1T], [ln])
                            P.ts("dve", ln[:, 0:nj], ln[:, 0:nj], -7.0, 1.0, ALU.max, ALU.add, [ln], [ln])
                            P.tt("pool", gl[:, 0:nj], gl[:, 0:nj], sg[:, 0:nj], ALU.mult, [gl, sg], [gl])
                            P.tt("pool", at[:, fc, 0:nj], gl[:, 0:nj], ln[:, 0:nj], ALU.mult, [gl, ln], [at])
                        if pre:
                            load_w1(e + 1, q)
                    if pre:
                        load_w2(e + 1, 0)
                        load_w2(e + 1, 1)
                    for s in range(nj // 128):
                        sidx = j0 // 128 + s
                        for hf in range(2):
                            po = pso.next()
                            w2s = W2[(e, hf)]
                            for fc in range(8):
                                P.mm(po[:], at[:, fc, s * 128:(s + 1) * 128], w2s[:, fc, :], fc == 0, fc == 7, [at, w2s], [po])
                            asl = acc[:, sidx, hf * 512:(hf + 1) * 512]
                            P.stt("dve", asl, po[:], gt[:, sidx, e:e + 1], asl, ALU.mult, ALU.add, [po, gt, acc], [acc])
            for s in range(nsub):
                r0 = t0 + s * 128
                xt = xr.next()
                P.dma("sp", xt[:], X["XR"][r0:r0 + 128, :], xt, writes=[xt])
                P.tt("dve", acc[:, s, :], acc[:, s, :], g2[w][:], ALU.mult, [acc, g2[w]], [acc])
                P.tt("pool", xt[:], xt[:], acc[:, s, :], ALU.add, [xt, acc], [xt])
                if not last:
                    P.dma("sp", X["XR"][r0:r0 + 128, :], xt[:], xt, reads=[xt])
                else:
                    st = str_.next()
                    sumsq(P, scr_b, xt, st)
                    rstd(P, st, st[:, 1:2], st[:, 0:1], D, K["epsc"])
                    P.stt("dve", xt[:], xt[:], st[:, 1:2], FG[:], ALU.mult, ALU.mult, [xt, st, FG], [xt])
                    P.dma("sp", yout[r0 - C:r0 - C + 128, :], xt[:], xt, reads=[xt])


def host_consts():
    inv_freq = (10000.0 ** (-np.arange(0, 32, 2, dtype=np.float32) / 32.0)).astype(np.float32)
    pos = np.arange(S)
    row = (pos // 64).astype(np.float32)
    col = (pos % 64).astype(np.float32)
    ang = np.concatenate([row[:, None] * inv_freq[None, :], col[:, None] * inv_freq[None, :]], axis=1).astype(np.float32)
    m = np.arange(128)
    tri = np.zeros((6, 128, 128), np.float32)
    tri[0] = (m[:, None] <= m[None, :]) * (-1.0 / 16.0)
    tri[1] = (m[:, None] >= m[None, :]) * (-1.0 / 16.0)
    tri[2] = (m[:, None] > m[None, :]) * (-1.0 / 16.0)
    tri[3] = (m[:, None] < m[None, :]) * (-1.0 / 16.0)
    tri[4] = (m[:, None] <= m[None, :]) * 1.0
    tri[5] = (m[:, None] >= m[None, :]) * 1.0
    return dict(k_cos=np.cos(ang).astype(np.float32), k_sin=np.sin(ang).astype(np.float32),
                k_ident=np.eye(128, dtype=np.float32), k_tri=tri)


def make_in_maps(inputs, cores, used=None):
    kc = host_consts()
    f = lambda a: np.ascontiguousarray(np.asarray(a, dtype=np.float32))
    shared = {k: f(inputs[k]) for k in ("c_ctx", "ada_w", "ada_b", "norm1_g", "norm2_g", "w_in", "diff_subln_g",
                                        "diff_w_out", "conv_w", "conv_w_out", "gla_w_a2", "gla_norm_g", "gla_w_out",
                                        "w_o", "router_w", "router_b", "moe_w1", "moe_b1", "moe_w2", "moe_b2",
                                        "final_norm_g")}
    shared["diff_lambda"] = f(inputs["diff_lambda"]).reshape(DEPTH, 256)
    shared["gla_b_a"] = f(inputs["gla_b_a"]).reshape(DEPTH, 512)
    shared.update(kc)
    maps = []
    for b in cores:
        m = dict(shared)
        m["x"] = f(inputs["x"][b])
        m["c"] = f(inputs["c"][b])
        m["ctx"] = f(inputs["ctx"][b])
        if used is not None:
            m = {k: v for k, v in m.items() if k in used}
        maps.append(m)
    return maps


def kernel(**inputs):
    nc = build()
    maps = make_in_maps(inputs, list(range(8)))
    res = run_bass_kernel_spmd(nc, maps, core_ids=list(range(8)))
    return np.stack([np.asarray(r["y"], dtype=np.float32) for r in res.results], axis=0)
```

```python
import math
from contextlib import ExitStack
import numpy as np
import concourse.bass as bass
import concourse.mybir as mybir
from concourse.bass_utils import run_bass_kernel_spmd

F32 = mybir.dt.float32
BF16 = mybir.dt.bfloat16
AF = mybir.ActivationFunctionType
ALU = mybir.AluOpType
AX = mybir.AxisListType

D = 1024
S = 4096
C = 256
T = S + C
NT = T // 128
DEPTH = 2
NE = 32
WIN = 9248
EPS = 1e-6
OQ, OK_, OV = 0, 1024, 2048
OCB, OCC, OCX = 3072, 3584, 4096
OGQ, OGK, OGV, OGR, OGA = 4608, 4864, 5120, 5632, 6144
OGT = 6176
TILES = [(0, 256)] + [(256 + 512 * i, 512) for i in range(8)]


DBG = {}


class Dep:
    def __init__(self):
        self.w = None
        self.r = {}
        self.ds = None


class TL(Dep):
    def __init__(self, h):
        super().__init__()
        self.h = h

    def __getitem__(self, k):
        return self.h[k]


class DSem:
    def __init__(self, sem):
        self.sem = sem
        self.cnt = 0


class Prog:
    def __init__(self, nc, ndsem=96):
        self.nc = nc
        self.E = {"pe": nc.tensor, "act": nc.scalar, "dve": nc.vector, "pool": nc.gpsimd, "sp": nc.sync}
        self.sem = {k: nc.alloc_semaphore("s_" + k) for k in self.E}
        self.cnt = {k: 0 for k in self.E}
        self.seen = {k: {} for k in self.E}
        self.dpool = [DSem(nc.alloc_semaphore("d%d" % i)) for i in range(ndsem)]
        self.dnext = 0
        self.persist = 0
        self.nsb = 0
        self.pe_cols = [0]
        self.dly = {}
        self.nfence = 0

    def sb(self, es, shape, dt, name=None):
        self.nsb += 1
        h = es.enter_context(self.nc.sbuf_tensor("t%d" % self.nsb, list(shape), dt))
        return TL(h)

    def ps(self, es, shape, dt=F32):
        self.nsb += 1
        h = es.enter_context(self.nc.psum_tensor("p%d" % self.nsb, list(shape), dt))
        return TL(h)

    def _ds(self, t):
        if t.ds is None:
            assert self.dnext < len(self.dpool), "out of dma semaphores"
            t.ds = self.dpool[self.dnext]
            self.dnext += 1
        return t.ds

    def _wait(self, eng, ev, raw=False):
        if ev is None:
            return
        if ev[0] == "e":
            _, src, val = ev
            if src == eng and (eng == "pe" or not raw):
                return
            key = src
            sem = self.sem[src]
            if src == "pe":
                need = self.pe_cols[val] + 256
                k2 = val
                while k2 < self.cnt["pe"] and self.pe_cols[k2] < need:
                    k2 += 1
                if self.pe_cols[k2] >= need:
                    val = k2
                else:
                    val = self.cnt["pe"]
                    if self.seen[eng].get(key, 0) < val:
                        self.E[eng].wait_ge(sem, val)
                        self.seen[eng][key] = val
                    if self.seen[eng].get("pe_safe", 0) < val:
                        self._delay(eng)
                        self.seen[eng]["pe_safe"] = val
                    return
                if self.seen[eng].get("pe_safe", 0) < val:
                    self.seen[eng]["pe_safe"] = val
        else:
            ds = ev[1]
            key = id(ds)
            sem = ds.sem
            val = ds.cnt
        if self.seen[eng].get(key, 0) >= val:
            return
        self.E[eng].wait_ge(sem, val)
        self.seen[eng][key] = val

    def _delay(self, eng):
        if eng not in self.dly:
            return
        self.nfence += 1
        d = self.dly[eng]
        if eng == "act":
            self.nc.scalar.copy(d[:, 0:256], d[:, 256:512])
        else:
            self.E[eng].memset(d[:, 0:256], 0.0)

    def _deps(self, eng, reads, writes):
        for t in reads:
            self._wait(eng, t.w, raw=True)
        for t in writes:
            self._wait(eng, t.w)
            for ev in list(t.r.values()):
                self._wait(eng, ev)

    def _mark(self, ev, key, reads, writes):
        for t in reads:
            t.r[key] = ev
        for t in writes:
            t.w = ev
            t.r = {}

    def op(self, eng, ins_fn, reads=(), writes=(), pe_n=None):
        self._deps(eng, reads, writes)
        ins = ins_fn()
        self.cnt[eng] += 1
        if eng == "pe":
            if pe_n is None:
                try:
                    pe_n = int(ins.ins.outs[0].free_size()) if False else 128
                except Exception:
                    pe_n = 128
            self.pe_cols.append(self.pe_cols[-1] + pe_n)
        ins.then_inc(self.sem[eng], 1)
        self._mark(("e", eng, self.cnt[eng]), eng, reads, writes)
        return ins

    def dma(self, q, out, in_, holder, reads=(), writes=(), **kw):
        self._deps(q, reads, writes)
        ds = self._ds(holder)
        ins = self.E[q].dma_start(out=out, in_=in_, **kw)
        ins.then_inc(ds.sem, 16)
        ds.cnt += 16
        self._mark(("d", ds), id(ds), reads, writes)

    def barrier(self):
        for ds in self.dpool[: self.dnext]:
            if ds.cnt > 0:
                self._wait("sp", ("d", ds))
        for e in self.E:
            if e != "sp":
                self._wait("sp", ("e", e, self.cnt[e]))
        self.E["sp"].sem_inc(self.sem["sp"], 1)
        self.cnt["sp"] += 1
        for e in self.E:
            if e == "sp":
                continue
            for o in self.E:
                if o != e:
                    self._wait(e, ("e", o, self.cnt[o]))
        self.dnext = self.persist

    def rep(self, name):
        print("SBUF remaining after", name, self.nc.sbuf_bytes_remaining, flush=True)

    def persist_dsems(self):
        self.persist = self.dnext

    def mm(self, out, lhsT, rhs, start, stop, reads, writes):
        n = 1
        for d_ in rhs.shape[1:]:
            n *= int(d_)
        return self.op("pe", lambda: self.nc.tensor.matmul(out, lhsT, rhs, start=start, stop=stop), reads, writes, pe_n=n)

    def tr(self, out, in_, ident, reads, writes):
        return self.op("pe", lambda: self.nc.tensor.transpose(out, in_, ident), reads, writes, pe_n=64)

    def act(self, out, in_, func, reads, writes, **kw):
        return self.op("act", lambda: self.nc.scalar.activation(out=out, in_=in_, func=func, **kw), reads, writes)

    def ts(self, eng, out, in0, s1, s2, op0, op1, reads, writes):
        eng = self.cmap(eng)
        e = self.E[eng]
        if op1 is None:
            return self.op(eng, lambda: e.tensor_scalar(out, in0, s1, None, op0), reads, writes)
        return self.op(eng, lambda: e.tensor_scalar(out, in0, s1, s2, op0, op1), reads, writes)

    def tt(self, eng, out, in0, in1, op, reads, writes):
        eng = self.cmap(eng)
        e = self.E[eng]
        return self.op(eng, lambda: e.tensor_tensor(out, in0, in1, op), reads, writes)

    def stt(self, eng, out, in0, scalar, in1, op0, op1, reads, writes):
        eng = self.cmap(eng)
        e = self.E[eng]
        return self.op(eng, lambda: e.scalar_tensor_tensor(out, in0, scalar, in1, op0, op1), reads, writes)

    def cp(self, eng, out, in_, reads, writes):
        eng = self.cmap(eng)
        if eng == "act":
            return self.op("act", lambda: self.nc.scalar.copy(out, in_), reads, writes)
        e = self.E[eng]
        return self.op(eng, lambda: e.tensor_copy(out, in_), reads, writes)

    def cmap(self, eng):
        return "dve" if (eng == "pool" and not DBG.get("pool_compute", False)) else eng

    def memset(self, eng, t, ap, val):
        eng = self.cmap(eng)
        e = self.E[eng]
        return self.op(eng, lambda: e.memset(ap, val), (), (t,))


class Ring:
    def __init__(self, tiles):
        self.t = tiles
        self.i = 0

    def next(self):
        t = self.t[self.i % len(self.t)]
        self.i += 1
        return t


def build(n_layers=DEPTH, debug_out=(), stop_after=None):
    nc = bass.Bass("TRN2", target_bir_lowering=False)
    P = Prog(nc)

    def din(name, shape, dt=F32):
        return nc.dram_tensor(name, list(shape), dt, kind="ExternalInput").ap()

    SHAPES = dict(x=[S, D], c=[D], ctx=[C, D], c_ctx=[D], ada_w=[DEPTH, D, 6 * D], ada_b=[DEPTH, 6 * D],
                  norm1_g=[DEPTH, D], norm2_g=[DEPTH, D], w_in=[DEPTH, D, WIN], diff_lambda=[DEPTH, 256],
                  diff_subln_g=[DEPTH, 128], diff_w_out=[DEPTH, D, D], conv_w=[DEPTH, 3, 512],
                  conv_w_out=[DEPTH, 512, D], gla_w_a2=[DEPTH, 2, 16, 256], gla_b_a=[DEPTH, 512],
                  gla_norm_g=[DEPTH, 128], gla_w_out=[DEPTH, 512, D], w_o=[DEPTH, D, D], router_w=[DEPTH, D, NE],
                  router_b=[DEPTH, NE], moe_w1=[DEPTH, NE, D, 2 * D], moe_b1=[DEPTH, NE, 2 * D],
                  moe_w2=[DEPTH, NE, D, D], moe_b2=[DEPTH, NE, D], final_norm_g=[D],
                  k_cos=[S, 32], k_sin=[S, 32], k_ident=[128, 128], k_tri=[6, 128, 128])

    class LazyIn(dict):
        def __missing__(self, k):
            self[k] = din(k, SHAPES[k])
            return self[k]

    I = LazyIn()
    if stop_after is None:
        for k in SHAPES:
            I[k]
    yout = nc.dram_tensor("y", [S, D], F32, kind="ExternalOutput").ap()

    def scr(name, shape, dt=F32):
        kind = "ExternalOutput" if name in debug_out else "Internal"
        return nc.dram_tensor(name, list(shape), dt, kind=kind).ap()

    X = {}
    X["XR"] = scr("XR", [T, D])
    X["MODS"] = scr("MODS", [2, 128, 6 * D])
    X["QT"] = scr("QT", [8, 128, T], BF16)
    X["KT"] = scr("KT", [8, 128, T], BF16)
    X["V"] = scr("V", [8, 128, NT * 132], BF16)
    X["CBT"] = scr("CBT", [4, 128, T])
    X["CCT"] = scr("CCT", [4, 128, T])
    X["CXT"] = scr("CXT", [4, 128, T])
    X["GQT"] = scr("GQT", [2, 128, T])
    X["GKT"] = scr("GKT", [2, 128, T])
    X["GK"] = scr("GK", [T, 256])
    X["GV"] = scr("GV", [T, 512], BF16)
    X["GR"] = scr("GR", [T, 512])
    X["GAF"] = scr("GAF", [16, T])
    X["GAB"] = scr("GAB", [16, T])
    X["SIGT"] = scr("SIGT", [24, 128, T], BF16)
    X["DIFFT"] = scr("DIFFT", [8, 128, T], BF16)
    X["YCT"] = scr("YCT", [4, 128, T], BF16)
    X["YGT"] = scr("YGT", [4, 128, T], BF16)
    X["H2T"] = scr("H2T", [8, 128, T], BF16)
    X["GATES"] = scr("GATES", [T, NE])
    if "HT" in debug_out:
        X["HT"] = scr("HT", [8, 128, T], BF16)

    with ExitStack() as gs:
        ident_f = P.sb(gs, [128, 128], F32)
        ident_b = P.sb(gs, [128, 128], BF16)
        ones_f = P.sb(gs, [128, 128], F32)
        lam = P.sb(gs, [128, 4], F32)
        subg = P.sb(gs, [128, 128], F32)
        glag = P.sb(gs, [128, 128], F32)
        P.dma("sp", ident_f[:], I["k_ident"], ident_f, writes=[ident_f])
        P.cp("dve", ident_b[:], ident_f[:], [ident_f], [ident_b])
        P.memset("dve", ones_f, ones_f[:], 1.0)
        for e_ in ("act", "dve"):
            P.dly[e_] = P.sb(gs, [128, 512], F32)
            P.memset("dve", P.dly[e_], P.dly[e_][:], 0.0)
        epsc = P.sb(gs, [128, 1], F32)
        P.memset("dve", epsc, epsc[:], EPS)
        P.persist_dsems()
        P.barrier()

        for L in range(n_layers):
            last = L == n_layers - 1 and n_layers == DEPTH
            lam_init = 0.8 - 0.6 * math.exp(-0.3 * L)
            phases = [phase0, phase1_2, phase3, phase4, phase5, phase6, phase7]
            for ph in phases:
                kk = dict(ident_f=ident_f, ident_b=ident_b, ones_f=ones_f, lam=lam, subg=subg, glag=glag, epsc=epsc)
                if ph is phase7:
                    ph(P, I, X, L, last, lam_init, kk, yout)
                else:
                    ph(P, I, X, L, last, lam_init, kk)
                P.barrier()
                if stop_after == (L, ph.__name__):
                    break
            else:
                continue
            break
        P.barrier()
    nc._used_inputs = set(I.keys())
    return nc


def phase0(P, I, X, L, last, lam_init, K):
    nc = P.nc
    with ExitStack() as es:
        cs = P.sb(es, [128, 8, 2], F32)
        crep = [P.sb(es, [128, 8, 128], BF16) for _ in range(2)]
        mod = [P.sb(es, [128, 6 * D], F32) for _ in range(2)]
        grep = [P.sb(es, [128, D], F32) for _ in range(2)]
        wring = Ring([P.sb(es, [128, 8, 512], BF16) for _ in range(2)])
        pss = Ring([P.ps(es, [128, 512]) for _ in range(4)])
        dl = P.sb(es, [128, 256], F32)
        tmp = P.sb(es, [128, 256], F32)

        with nc.allow_non_contiguous_dma(reason="tiny transposed vector load"):
            P.dma("sp", cs[:, :, 0], I["c"].rearrange("(kc p) -> p kc", p=128), cs, writes=[cs])
            P.dma("sp", cs[:, :, 1], I["c_ctx"].rearrange("(kc p) -> p kc", p=128), cs, writes=[cs])
        P.act(cs[:], cs[:], AF.Silu, [cs], [cs])
        for w in range(2):
            for kc in range(8):
                P.ts("dve", crep[w][:, kc, :], K["ones_f"][:], cs[:, kc, w:w + 1], None, ALU.mult, None,
                     [cs, K["ones_f"]], [crep[w]])
            P.dma("sp", mod[w][:], I["ada_b"][L].partition_broadcast(128), mod[w], writes=[mod[w]])
        P.dma("sp", grep[0][:], I["norm1_g"][L].partition_broadcast(128), grep[0], writes=[grep[0]])
        P.dma("sp", grep[1][:], I["norm2_g"][L].partition_broadcast(128), grep[1], writes=[grep[1]])
        aw = I["ada_w"][L].rearrange("(kc p) n -> p kc n", p=128)
        for cb in range(12):
            wt = wring.next()
            P.dma("pool", wt[:], aw[:, :, cb * 512:(cb + 1) * 512], wt, writes=[wt])
            for w in range(2):
                ps = pss.next()
                for kc in range(8):
                    P.mm(ps[:], crep[w][:, kc, :], wt[:, kc, :], kc == 0, kc == 7, [crep[w], wt], [ps])
                sl = mod[w][:, cb * 512:(cb + 1) * 512]
                P.tt("dve", sl, ps[:], sl, ALU.add, [ps, mod[w]], [mod[w]])
        for w in range(2):
            for seg, g in ((1, grep[0]), (4, grep[1])):
                sl = mod[w][:, seg * D:(seg + 1) * D]
                P.stt("dve", sl, sl, 1.0, g[:], ALU.add, ALU.mult, [mod[w], g], [mod[w]])
            P.dma("sp", X["MODS"][w], mod[w][:], mod[w], reads=[mod[w]])
        lamt = K["lam"]
        P.dma("sp", dl[:], I["diff_lambda"][L].partition_broadcast(128), dl, writes=[dl])
        P.tt("dve", tmp[:, 0:64], dl[:, 0:64], dl[:, 64:128], ALU.mult, [dl], [tmp])
        P.tt("dve", tmp[:, 64:128], dl[:, 128:192], dl[:, 192:256], ALU.mult, [dl], [tmp])
        P.op("dve", lambda: nc.vector.tensor_reduce(lamt[:, 1:3], tmp[:, 0:128].rearrange("p (a b) -> p a b", a=2),
                                                    AX.X, ALU.add), [tmp], [lamt])
        P.act(lamt[:, 1:3], lamt[:, 1:3], AF.Exp, [lamt], [lamt])
        P.tt("dve", lamt[:, 0:1], lamt[:, 1:2], lamt[:, 2:3], ALU.subtract, [lamt], [lamt])
        P.ts("dve", lamt[:, 0:1], lamt[:, 0:1], float(lam_init), None, ALU.add, None, [lamt], [lamt])
        P.dma("sp", K["subg"][:], I["diff_subln_g"][L].partition_broadcast(128), K["subg"], writes=[K["subg"]])
        P.ts("dve", K["subg"][:], K["subg"][:], float(1.0 - lam_init), None, ALU.mult, None, [K["subg"]], [K["subg"]])
        P.dma("sp", K["glag"][:], I["gla_norm_g"][L].partition_broadcast(128), K["glag"], writes=[K["glag"]])


def rstd(P, st, out_ap, in_ap, n, epsc):
    P.act(out_ap, in_ap, AF.Ln, [st, epsc], [st], scale=1.0 / n, bias=epsc[:, 0:1])
    P.act(out_ap, out_ap, AF.Exp, [st], [st], scale=-0.5)


def xsrc(I, X, L, r0):
    if L > 0:
        return X["XR"][r0:r0 + 128, :]
    if r0 < C:
        return I["ctx"][r0:r0 + 128, :]
    return I["x"][r0 - C:r0 - C + 128, :]


def sumsq(P, scr, xt, st):
    P.act(scr[:], xt[:], AF.Square, [xt], [scr])
    P.op("dve", lambda: P.nc.vector.tensor_reduce(st[:, 0:1], scr[:], AX.X, ALU.add), [scr], [st])


def norm_mod(P, xt, Gt, SHt, hb, scr_b, st, epsc, eng2="pool", xo=None):
    nc = P.nc
    sumsq(P, scr_b, xt, st)
    rstd(P, st, st[:, 1:2], st[:, 0:1], D, epsc)
    xo = xt if xo is None else xo
    P.stt("dve", xo[:], xt[:], st[:, 1:2], Gt[:], ALU.mult, ALU.mult, [xt, st, Gt], [xo])
    P.tt(eng2, hb[:], xo[:], SHt[:], ALU.add, [xo, SHt], [hb])


def phase1_2(P, I, X, L, last, lam_init, K):
    nc = P.nc
    with ExitStack() as es:
        hT = P.sb(es, [128, 8, T], BF16)
        with ExitStack() as e1:
            msl = [[P.sb(e1, [128, D], F32) for _ in range(2)] for _ in range(2)]
            for w in range(2):
                for j, seg in enumerate((0, 1)):
                    P.dma("sp", msl[w][j][:], X["MODS"][w][:, seg * D:(seg + 1) * D], msl[w][j], writes=[msl[w][j]])
            xr = Ring([P.sb(e1, [128, D], F32) for _ in range(3)])
            hbr = Ring([P.sb(e1, [128, D], BF16) for _ in range(2)])
            scr_b = P.sb(e1, [128, D], F32)
            str_ = Ring([P.sb(e1, [128, 2], F32) for _ in range(2)])
            ptr = Ring([P.ps(e1, [128, 8, 128], BF16) for _ in range(2)])
            for i in range(NT):
                w = 1 if i < 2 else 0
                xt = xr.next()
                P.dma("sp", xt[:], xsrc(I, X, L, i * 128), xt, writes=[xt])
                hb = hbr.next()
                st = str_.next()
                norm_mod(P, xt, msl[w][1], msl[w][0], hb, scr_b, st, K["epsc"])
                pt = ptr.next()
                for kc in range(8):
                    P.tr(pt[:, kc, :], hb[:, kc * 128:(kc + 1) * 128], K["ident_b"][:], [hb, K["ident_b"]], [pt])
                P.cp("act", hT[:, :, i * 128:(i + 1) * 128], pt[:], [pt], [hT])
            P.barrier()
        if "HT" in X:
            P.dma("sp", X["HT"].rearrange("c p t -> p c t"), hT[:], hT, reads=[hT])
            return
        phase2(P, I, X, L, K, hT)


def phase2(P, I, X, L, K, hT):
    nc = P.nc
    wv = I["w_in"][L].rearrange("(kc p) n -> p kc n", p=128)
    with ExitStack() as es:
        wring = Ring([P.sb(es, [128, 8, 512], BF16) for _ in range(3)])
        psr = Ring([P.ps(es, [128, 512]) for _ in range(4)])
        ptr = Ring([P.ps(es, [128, 4, 128], BF16) for _ in range(2)])
        cos = P.sb(es, [128, 32, 32], F32)
        sin = P.sb(es, [128, 32, 32], F32)
        P.dma("sp", cos[:], I["k_cos"].rearrange("(t p) f -> p t f", p=128), cos, writes=[cos])
        P.dma("sp", sin[:], I["k_sin"].rearrange("(t p) f -> p t f", p=128), sin, writes=[sin])
        ra = Ring([P.sb(es, [128, 512], F32) for _ in range(2)])
        rb = Ring([P.sb(es, [128, 512], F32) for _ in range(2)])
        rob = Ring([P.sb(es, [128, 512], BF16) for _ in range(2)])
        stq = Ring([P.sb(es, [128, 4, 512], BF16) for _ in range(2)])
        stf = Ring([P.sb(es, [128, 4, 512], F32) for _ in range(2)])

        def load_w(c0, ncols):
            wt = wring.next()
            P.dma("pool", wt[:, :, 0:ncols], wv[:, :, c0:c0 + ncols], wt, writes=[wt])
            return wt

        def tok_major(wt, cw0, ncols, i):
            ps = psr.next()
            for kc in range(8):
                P.mm(ps[:, 0:ncols], hT[:, kc, i * 128:(i + 1) * 128], wt[:, kc, cw0:cw0 + ncols],
                     kc == 0, kc == 7, [hT, wt], [ps])
            return ps

        def feat_major(wt, cw0, m, t0, n):
            ps = psr.next()
            for kc in range(8):
                P.mm(ps[0:m, 0:n], wt[:, kc, cw0:cw0 + m], hT[:, kc, t0:t0 + n], kc == 0, kc == 7, [hT, wt], [ps])
            return ps

        for which, dst, c_base in (("q", X["QT"], OQ), ("k", X["KT"], OK_)):
            for half in range(2):
                wt = load_w(c_base + half * 512, 512)
                for (t0, n) in TILES:
                    sq = stq.next()
                    for s in range(n // 128):
                        i = (t0 + s * 128) // 128
                        ps = tok_major(wt, 0, 512, i)
                        ob = rob.next()
                        if i < 2:
                            P.cp("act", ob[:], ps[:], [ps], [ob])
                        else:
                            li = i - 2
                            a = ra.next()
                            b = rb.next()
                            for ax in range(2):
                                def v4(ap):
                                    return ap.rearrange("p (h r) -> p h r", r=64)[:, :, ax * 32:(ax + 1) * 32].rearrange("p h (s f) -> p h s f", s=2)
                                x4, a4, b4, o4 = v4(ps[:]), v4(a[:]), v4(b[:]), v4(ob[:])
                                cs4 = cos[:, li, ax * 16:(ax + 1) * 16].unsqueeze(1).unsqueeze(1).broadcast_to([128, 8, 2, 16])
                                sn3 = sin[:, li, ax * 16:(ax + 1) * 16].unsqueeze(1).broadcast_to([128, 8, 16])
                                P.tt("dve", a4, x4, cs4, ALU.mult, [ps, cos], [a])
                                P.tt("dve", b4[:, :, 0, :], x4[:, :, 1, :], sn3, ALU.mult, [ps, sin], [b])
                                P.tt("dve", b4[:, :, 1, :], x4[:, :, 0, :], sn3, ALU.mult, [ps, sin], [b])
                                P.tt("pool", o4[:, :, 0, :], a4[:, :, 0, :], b4[:, :, 0, :], ALU.subtract, [a, b], [ob])
                                P.tt("pool", o4[:, :, 1, :], a4[:, :, 1, :], b4[:, :, 1, :], ALU.add, [a, b], [ob])
                        pt = ptr.next()
                        for hh in range(4):
                            P.tr(pt[:, hh, :], ob[:, hh * 128:(hh + 1) * 128], K["ident_b"][:], [ob, K["ident_b"]], [pt])
                        P.cp("act", sq[:, :, s * 128:(s + 1) * 128], pt[:], [pt], [sq])
                    P.dma("sp", dst[half * 4:(half + 1) * 4, :, t0:t0 + n].rearrange("h p t -> p h t"),
                          sq[:, :, 0:n], sq, reads=[sq])
        with ExitStack() as ev_:
            vst = P.sb(ev_, [128, 4, NT, 132], BF16)
            P.memset("dve", vst, vst[:].rearrange("p h t e -> p (h t e)"), 1.0)
            for half in range(2):
                wt = load_w(OV + half * 512, 512)
                for i in range(NT):
                    ps = tok_major(wt, 0, 512, i)
                    P.cp("act", vst[:, :, i, 0:128], ps[:].rearrange("p (h e) -> p h e", h=4), [ps], [vst])
                for hh in range(4):
                    P.dma("sp", X["V"][half * 4 + hh], vst[:, hh, :, :].rearrange("p t e -> p (t e)"), vst, reads=[vst])
        for dst, c0 in ((X["CBT"], OCB), (X["CCT"], OCC), (X["CXT"], OCX)):
            wt = load_w(c0, 512)
            for (t0, n) in TILES:
                sf = stf.next()
                for ch in range(4):
                    ps = feat_major(wt, ch * 128, 128, t0, n)
                    P.cp("act", sf[:, ch, 0:n], ps[:, 0:n], [ps], [sf])
                P.dma("sp", dst[:, :, t0:t0 + n].rearrange("c p t -> p c t"), sf[:, :, 0:n], sf, reads=[sf])
        wt = load_w(OGQ, 512)
        for (t0, n) in TILES:
            sf = stf.next()
            for ch in range(4):
                ps = feat_major(wt, ch * 128, 128, t0, n)
                P.cp("act", sf[:, ch, 0:n], ps[:, 0:n], [ps], [sf])
            P.dma("sp", X["GQT"][:, :, t0:t0 + n].rearrange("c p t -> p c t"), sf[:, 0:2, 0:n], sf, reads=[sf])
            P.dma("sp", X["GKT"][:, :, t0:t0 + n].rearrange("c p t -> p c t"), sf[:, 2:4, 0:n], sf, reads=[sf])
            sf = stf.next()
            for s in range(n // 128):
                ps = tok_major(wt, 256, 256, (t0 + s * 128) // 128)
                P.cp("act", sf[:, s, 0:256], ps[:, 0:256], [ps], [sf])
            P.dma("sp", X["GK"][t0:t0 + n, :].rearrange("(s p) c -> p s c", p=128), sf[:, 0:n // 128, 0:256], sf, reads=[sf])
        wt = load_w(OGV, 512)
        for (t0, n) in TILES:
            sq = stq.next()
            for s in range(n // 128):
                ps = tok_major(wt, 0, 512, (t0 + s * 128) // 128)
                P.cp("act", sq[:, s, :], ps[:], [ps], [sq])
            P.dma("sp", X["GV"][t0:t0 + n, :].rearrange("(s p) c -> p s c", p=128), sq[:, 0:n // 128, :], sq, reads=[sq])
        wt = load_w(OGR, 512)
        for (t0, n) in TILES:
            sf = stf.next()
            for s in range(n // 128):
                ps = tok_major(wt, 0, 512, (t0 + s * 128) // 128)
                P.act(sf[:, s, :], ps[:], AF.Silu, [ps], [sf])
            P.dma("sp", X["GR"][t0:t0 + n, :].rearrange("(s p) c -> p s c", p=128), sf[:, 0:n // 128, :], sf, reads=[sf])
        wt = load_w(OGA, 32)
        for (t0, n) in TILES:
            sf = stf.next()
            ps = feat_major(wt, 0, 32, t0, n)
            P.cp("act", sf[0:32, 0, 0:n], ps[0:32, 0:n], [ps], [sf])
            P.dma("sp", X["GAF"][:, t0:t0 + n], sf[0:16, 0, 0:n], sf, reads=[sf])
            P.dma("sp", X["GAB"][:, t0:t0 + n], sf[16:32, 0, 0:n], sf, reads=[sf])
        for gblk in range(6):
            wt = load_w(OGT + gblk * 512, 512)
            for (t0, n) in TILES:
                sq = stq.next()
                for ch in range(4):
                    ps = feat_major(wt, ch * 128, 128, t0, n)
                    P.act(sq[:, ch, 0:n], ps[:, 0:n], AF.Sigmoid, [ps], [sq])
                P.dma("sp", X["SIGT"][gblk * 4:(gblk + 1) * 4, :, t0:t0 + n].rearrange("c p t -> p c t"),
                      sq[:, :, 0:n], sq, reads=[sq])


class V(Dep):
    def __init__(self, ap):
        super().__init__()
        self.ap = ap


def phase3(P, I, X, L, last, lam_init, K):
    nc = P.nc
    with ExitStack() as es:
        ktr = Ring([P.sb(es, [128, T], BF16) for _ in range(2)])
        qtr = Ring([P.sb(es, [128, T], BF16) for _ in range(2)])
        vtr = Ring([P.sb(es, [128, NT, 132], BF16) for _ in range(2)])
        pss = Ring([P.ps(es, [128, 1024]) for _ in range(2)])
        accT = P.ps(es, [128, 1536])
        ptT = Ring([P.ps(es, [128, 2, 128], BF16) for _ in range(1)])
        offs = [0, 160, 320, 512, 672, 832, 1024, 1184]
        accv = [[V(accT[:, offs[m * 4 + s]:offs[m * 4 + s] + 129]) for s in range(4)] for m in range(2)]
        ptr_ = Ring([P.sb(es, [128, 1024], BF16) for _ in range(3)])
        evr = Ring([P.sb(es, [128, 2, 132], F32) for _ in range(3)])
        t1r = Ring([P.sb(es, [128, 128], F32) for _ in range(2)])
        o_r = Ring([P.sb(es, [128, 128], F32) for _ in range(2)])
        jk = P.sb(es, [128, 128], F32)
        obr = Ring([P.sb(es, [128, 128], BF16) for _ in range(2)])
        str_ = Ring([P.sb(es, [128, 4], F32) for _ in range(3)])
        dstr = Ring([P.sb(es, [128, 512], BF16) for _ in range(2)])
        lam = K["lam"]
        qtiles = [(256 + 512 * i, 512, list(range(NT))) for i in range(8)]
        if not last:
            qtiles = [(0, 256, [0, 1])] + qtiles
        if DBG.get("p3_qtiles") is not None:
            qtiles = [qtiles[i] for i in DBG["p3_qtiles"]]
        for h in range(DBG.get("p3_heads", 8)):
            kt, qt, vt = ktr.next(), qtr.next(), vtr.next()
            P.dma("sp", kt[:], X["KT"][h], kt, writes=[kt])
            P.dma("sp", qt[:], X["QT"][h], qt, writes=[qt])
            P.dma("sp", vt[:].rearrange("p t e -> p (t e)"), X["V"][h], vt, writes=[vt])
            for (q0, n, ktl) in qtiles:
                nsub = n // 128
                def qk(kk_):
                    ps_ = pss.next()
                    for m in range(2):
                        P.mm(ps_[:, m * 512:m * 512 + n], kt[m * 64:(m + 1) * 64, kk_ * 128:(kk_ + 1) * 128],
                             qt[m * 64:(m + 1) * 64, q0:q0 + n], True, True, [kt, qt], [ps_])
                    return ps_

                ps_next = qk(ktl[0])
                for ki, kk in enumerate(ktl):
                    ps = ps_next
                    if ki + 1 < len(ktl):
                        ps_next = qk(ktl[ki + 1])
                    pt = ptr_.next()
                    if n == 512:
                        P.act(pt[:], ps[:], AF.Exp, [ps], [pt], scale=0.125)
                    else:
                        for m in range(2):
                            P.act(pt[:, m * 512:m * 512 + n], ps[:, m * 512:m * 512 + n], AF.Exp, [ps], [pt], scale=0.125)
                    if ki == 0:
                        started = set()
                    for m in range(2):
                        for s in range(nsub):
                            av = accv[m][s]
                            bank = offs[m * 4 + s] // 512
                            st_flag = ki == 0 and bank not in started
                            started.add(bank)
                            P.op("pe", lambda: nc.tensor.matmul(av.ap, pt[:, m * 512 + s * 128:m * 512 + (s + 1) * 128],
                                                                vt[:, kk, 0:129], start=st_flag, stop=(ki == len(ktl) - 1),
                                                                skip_group_check=True), [pt, vt], [av], pe_n=129)
                dst = dstr.next()
                for s in range(nsub):
                    ev = evr.next()
                    st = str_.next()
                    for m in range(2):
                        rd = [accv[m][s]] + ([accv[1][nsub - 1]] if DBG.get("h1") else [])
                        P.cp("dve", ev[:, m, 0:129], accv[m][s].ap, rd, [ev])
                    P.op("dve", lambda: nc.vector.reciprocal(st[:, 0:2], ev[:, :, 128]), [ev], [st])
                    P.tt("dve", st[:, 1:2], st[:, 1:2], lam[:, 0:1], ALU.mult, [st, lam], [st])
                    t1 = t1r.next()
                    o = o_r.next()
                    P.ts("pool", t1[:], ev[:, 1, 0:128], st[:, 1:2], None, ALU.mult, None, [ev, st], [t1])
                    P.stt("dve", o[:], ev[:, 0, 0:128], st[:, 0:1], t1[:], ALU.mult, ALU.subtract, [ev, st, t1], [o])
                    P.tt("pool", jk[:], o[:], o[:], ALU.mult, [o], [jk])
                    P.op("dve", lambda: nc.vector.tensor_reduce(st[:, 2:3], jk[:], AX.X, ALU.add), [jk], [st])
                    rstd(P, st, st[:, 2:3], st[:, 2:3], 128, K["epsc"])
                    ob = obr.next()
                    P.stt("dve", ob[:], o[:], st[:, 2:3], K["subg"][:], ALU.mult, ALU.mult, [o, st, K["subg"]], [ob])
                    pT = ptT.next()
                    P.tr(pT[:, 0, :], ob[:], K["ident_b"][:], [ob, K["ident_b"]], [pT])
                    P.cp("dve", dst[:, s * 128:(s + 1) * 128], pT[:, 0, :], [pT], [dst])
                P.dma("sp", X["DIFFT"][h, :, q0:q0 + n], dst[:, 0:n], dst, reads=[dst])


def phase4(P, I, X, L, last, lam_init, K):
    nc = P.nc
    with ExitStack() as es:
        cw = P.sb(es, [128, 4, 3], F32)
        with nc.allow_non_contiguous_dma(reason="tiny conv taps"):
            for k_ in range(3):
                P.dma("sp", cw[:, :, k_], I["conv_w"][L, k_].rearrange("(c p) -> p c", p=128), cw, writes=[cw])
        zero = P.sb(es, [128, 8], F32)
        P.memset("dve", zero, zero[:], 0.0)
        cb = P.sb(es, [128, T], F32)
        c_ = P.sb(es, [128, T], F32)
        cx = P.sb(es, [128, T], F32)
        up = P.sb(es, [128, T], F32)
        un = P.sb(es, [128, T], F32)
        y = P.sb(es, [128, T], F32)
        yb = P.sb(es, [128, T], BF16)
        for cc in range(4):
            P.dma("sp", cb[:], X["CBT"][cc], cb, writes=[cb])
            P.dma("sp", c_[:], X["CCT"][cc], c_, writes=[c_])
            P.dma("sp", cx[:], X["CXT"][cc], cx, writes=[cx])
            P.tt("dve", c_[:], c_[:], cx[:], ALU.mult, [c_, cx], [c_])
            P.dma("sp", up[:, 1:T], c_[:, 0:T - 1], up, reads=[c_], writes=[up])
            P.dma("sp", un[:, 0:T - 1], c_[:, 1:T], un, reads=[c_], writes=[un])
            for col in (0, C):
                P.dma("sp", up[:, col:col + 1], zero[:, 0:1], up, reads=[zero], writes=[up])
            for col in (C - 1, T - 1):
                P.dma("sp", un[:, col:col + 1], zero[:, 0:1], un, reads=[zero], writes=[un])
            P.ts("dve", y[:], c_[:], cw[:, cc, 1:2], None, ALU.mult, None, [c_, cw], [y])
            P.stt("dve", y[:], up[:], cw[:, cc, 0:1], y[:], ALU.mult, ALU.add, [up, cw, y], [y])
            P.stt("dve", y[:], un[:], cw[:, cc, 2:3], y[:], ALU.mult, ALU.add, [un, cw, y], [y])
            P.tt("dve", yb[:], y[:], cb[:], ALU.mult, [y, cb], [yb])
            P.dma("sp", X["YCT"][cc], yb[:], yb, reads=[yb])


def phase5(P, I, X, L, last, lam_init, K):
    nc = P.nc
    NCH = NT
    with ExitStack() as es:
        tri = P.sb(es, [128, 6, 128], F32)
        P.dma("sp", tri[:], I["k_tri"].rearrange("s m l -> m s l"), tri, writes=[tri])
        wa = P.sb(es, [16, 2, 256], F32)
        P.dma("sp", wa[:], I["gla_w_a2"][L].rearrange("d k n -> k d n"), wa, writes=[wa])
        ba = P.sb(es, [128, 512], F32)
        P.dma("sp", ba[:], I["gla_b_a"][L].partition_broadcast(128), ba, writes=[ba])
        neg16 = P.sb(es, [128, 2], F32)
        P.memset("dve", neg16, neg16[:], -1.0 / 16.0)
        Sf = P.sb(es, [128, 2, 128], F32)
        Sfb = P.sb(es, [128, 2, 128], BF16)
        Sb = P.sb(es, [128, 2, 128], F32)
        SLB = P.sb(es, [128, NCH, 2, 128], F32)
        DECB = P.sb(es, [128, NCH, 2], F32)
        SBP = P.sb(es, [128, NCH, 2, 128], BF16)
        for t_ in (Sf, Sb):
            P.memset("dve", t_, t_[:], 0.0)
        P.memset("dve", Sfb, Sfb[:], 0.0)
        psA = P.ps(es, [128, 512])
        psB = P.ps(es, [128, 512])
        psC = P.ps(es, [128, 1024])
        psO = P.ps(es, [128, 512])
        psS = P.ps(es, [128, 512])
        psT = P.ps(es, [128, 4, 128], BF16)
        gqr = Ring([P.sb(es, [128, 2, 512], F32) for _ in range(2)])
        gkr = Ring([P.sb(es, [128, 2, 512], F32) for _ in range(2)])
        gktr = Ring([P.sb(es, [128, 256], F32) for _ in range(2)])
        gvr = Ring([P.sb(es, [128, 512], BF16) for _ in range(2)])
        grr = Ring([P.sb(es, [128, 512], F32) for _ in range(2)])
        gar = [Ring([P.sb(es, [16, 512], F32) for _ in range(2)]) for _ in range(2)]
        zbr = Ring([P.sb(es, [128, 256], F32) for _ in range(2)])
        spr = Ring([P.sb(es, [128, 256], F32) for _ in range(2)])
        e1r = Ring([P.sb(es, [128, 256], F32) for _ in range(2)])
        e2r = Ring([P.sb(es, [128, 256], F32) for _ in range(2)])
        e3r = Ring([P.sb(es, [128, 256], F32) for _ in range(2)])
        decr = Ring([P.sb(es, [128, 2], F32) for _ in range(2)])
        qdr = [Ring([P.sb(es, [128, 2, 128], BF16) for _ in range(2)]) for _ in range(2)]
        kir = [Ring([P.sb(es, [128, 2, 128], BF16) for _ in range(2)]) for _ in range(2)]
        ker = Ring([P.sb(es, [128, 256], BF16) for _ in range(2)])
        atr = [Ring([P.sb(es, [128, 4, 128], BF16) for _ in range(2)]) for _ in range(2)]
        osr = Ring([P.sb(es, [128, 512], F32) for _ in range(2)])
        sqj = P.sb(es, [128, 512], F32)
        onr = Ring([P.sb(es, [128, 512], F32) for _ in range(2)])
        obr = Ring([P.sb(es, [128, 512], BF16) for _ in range(2)])
        ygr = Ring([P.sb(es, [128, 4, 512], BF16) for _ in range(2)])

        def tile_of(c):
            if c < 2:
                return 0, 256, c * 128
            j = (c - 2) // 4
            return 256 + 512 * j, 512, ((c - 2) % 4) * 128
        str_ = Ring([P.sb(es, [128, 8], F32) for _ in range(2)])
        GAsrc = (X["GAF"], X["GAB"])

        def softplus_neg(c, d, ga, off):
            P.mm(psA[:, 0:256], ga[0:16, off:off + 128], wa[0:16, d, :], True, True, [ga, wa], [psA])
            zb = zbr.next()
            P.tt("dve", zb[:], psA[:, 0:256], ba[:, d * 256:(d + 1) * 256], ALU.add, [psA, ba], [zb])
            P.act(zb[:], zb[:], AF.Exp, [zb], [zb], scale=-1.0)
            sp = spr.next()
            P.act(sp[:], zb[:], AF.Ln, [zb], [sp], bias=1.0)
            return sp

        def tot_dec(sp, dec_ap, dec_t):
            for hp in range(2):
                P.mm(psA[:, 256 + hp:257 + hp], sp[:, hp * 128:(hp + 1) * 128], neg16[:, 0:1], True, True, [sp, neg16], [psA])
            P.act(dec_ap, psA[:, 256:258], AF.Exp, [psA], [dec_t])

        def ke_of(sp, d, gkt):
            P.mm(psB[:, 256:512], tri[:, 2 + d, :], sp[:], True, True, [tri, sp], [psB])
            e3 = e3r.next()
            P.act(e3[:], psB[:, 256:512], AF.Exp, [psB], [e3])
            ke = ker.next()
            P.tt("pool", ke[:], gkt[:], e3[:], ALU.mult, [gkt, e3], [ke])
            return ke

        def sloc(ke, gv):
            for hp in range(2):
                P.mm(psS[:, hp * 256:(hp + 1) * 256], ke[:, hp * 128:(hp + 1) * 128], gv[:, hp * 256:(hp + 1) * 256],
                     True, True, [ke, gv], [psS])

        for c in range(NCH):
            gkt, gv = gktr.next(), gvr.next()
            t0_, tn_, off_ = tile_of(c)
            if off_ == 0:
                gab_t = gar[1].next()
                P.dma("sp", gab_t[:, 0:tn_], X["GAB"][:, t0_:t0_ + tn_], gab_t, writes=[gab_t])
            P.dma("sp", gkt[:], X["GK"][c * 128:(c + 1) * 128, :], gkt, writes=[gkt])
            P.dma("sp", gv[:], X["GV"][c * 128:(c + 1) * 128, :], gv, writes=[gv])
            stg = DBG.get("p5_stage", 99)
            sp = softplus_neg(c, 1, gab_t, off_)
            if stg < 2:
                continue
            tot_dec(sp, DECB[:, c, :], DECB)
            if stg < 3:
                continue
            ke = ke_of(sp, 1, gkt)
            if stg < 4:
                continue
            sloc(ke, gv)
            for hp in range(2):
                for hh in range(2):
                    P.cp("act", SLB[hh * 64:(hh + 1) * 64, c, hp, :],
                         psS[hh * 64:(hh + 1) * 64, hp * 256 + hh * 128:hp * 256 + (hh + 1) * 128], [psS], [SLB])
        if DBG.get("p5_stage", 99) < 5:
            return
        for c in [1, 0] + list(range(NCH - 1, 1, -1)):
            P.cp("pool", SBP[:, c, :, :], Sb[:], [Sb], [SBP])
            for hp in range(2):
                P.stt("dve", Sb[:, hp, :], Sb[:, hp, :], DECB[:, c, hp:hp + 1], SLB[:, c, hp, :], ALU.mult, ALU.add,
                      [Sb, DECB, SLB], [Sb])
        if DBG.get("p5_stage", 99) < 6:
            return
        stg = DBG.get("p5_stage", 99)
        for c in range(NCH):
            gkt, gv, gr = gktr.next(), gvr.next(), grr.next()
            cs = slice(c * 128, (c + 1) * 128)
            t0_, tn_, off_ = tile_of(c)
            osl = slice(off_, off_ + 128)
            if off_ == 0:
                gq_t, gk_t, yg = gqr.next(), gkr.next(), ygr.next()
                ga = [gar[0].next(), gar[1].next()]
                ts_ = slice(t0_, t0_ + tn_)
                P.dma("sp", gq_t[:, :, 0:tn_], X["GQT"][:, :, ts_].rearrange("c p t -> p c t"), gq_t, writes=[gq_t])
                P.dma("sp", gk_t[:, :, 0:tn_], X["GKT"][:, :, ts_].rearrange("c p t -> p c t"), gk_t, writes=[gk_t])
                for d in range(2):
                    P.dma("sp", ga[d][:, 0:tn_], GAsrc[d][:, ts_], ga[d], writes=[ga[d]])
            P.dma("sp", gkt[:], X["GK"][cs, :], gkt, writes=[gkt])
            P.dma("sp", gv[:], X["GV"][cs, :], gv, writes=[gv])
            P.dma("sp", gr[:], X["GR"][cs, :], gr, writes=[gr])
            qd, ki, atm = [None, None], [None, None], [None, None]
            dec = decr.next()
            ke = None
            for d in range(2):
                sp = softplus_neg(c, d, ga[d], off_)
                for hp in range(2):
                    P.mm(psB[:, hp * 128:(hp + 1) * 128], sp[:, hp * 128:(hp + 1) * 128], tri[:, d, :], True, True,
                         [sp, tri], [psB])
                e1, e2 = e1r.next(), e2r.next()
                P.act(e1[:], psB[:, 0:256], AF.Exp, [psB], [e1])
                P.act(e2[:], psB[:, 0:256], AF.Exp, [psB], [e2], scale=-1.0)
                qd[d], ki[d] = qdr[d].next(), kir[d].next()
                P.stt("dve", qd[d][:], gq_t[:, :, osl], 0.125, e1[:].rearrange("p (a b) -> p a b", a=2),
                      ALU.mult, ALU.mult, [gq_t, e1], [qd[d]])
                P.tt("pool", ki[d][:], gk_t[:, :, osl], e2[:].rearrange("p (a b) -> p a b", a=2), ALU.mult,
                     [gk_t, e2], [ki[d]])
                if stg < 7:
                    continue
                if d == 0:
                    tot_dec(sp, dec[:], dec)
                    ke = ke_of(sp, 0, gkt)
                if stg < 7.3:
                    continue
                for h in range(4):
                    hp, b0 = h // 2, (h % 2) * 64
                    co = (h % 2) * 512 + (d * 2 + hp) * 128
                    P.mm(psC[:, co:co + 128], ki[d][b0:b0 + 64, hp, :], qd[d][b0:b0 + 64, hp, :],
                         True, True, [ki[d], qd[d]], [psC])
                if stg < 7.6:
                    continue
                atm[d] = atr[d].next()
                P.tt("dve", atm[d][:].rearrange("p (hp par) l -> p hp par l", par=2),
                     psC[:].rearrange("p (par d hp l) -> p d hp par l", par=2, d=2, hp=2)[:, d],
                     tri[:, 4 + d, :].unsqueeze(1).unsqueeze(1).broadcast_to([128, 2, 2, 128]), ALU.mult, [psC, tri], [atm[d]])
            if stg < 8:
                continue
            for h in range(4):
                hp, b0 = h // 2, (h % 2) * 64
                oo = psO[:, h * 128:(h + 1) * 128]
                vv = gv[:, h * 128:(h + 1) * 128]
                P.mm(oo, atm[0][:, h, :], vv, True, False, [atm[0], gv], [psO])
                P.mm(oo, qd[0][b0:b0 + 64, hp, :], Sfb[b0:b0 + 64, hp, :], False, False, [qd[0], Sfb], [psO])
                P.mm(oo, atm[1][:, h, :], vv, False, False, [atm[1], gv], [psO])
                P.mm(oo, qd[1][b0:b0 + 64, hp, :], SBP[b0:b0 + 64, c, hp, :], False, True, [qd[1], SBP], [psO])
            sloc(ke, gv)
            for hp in range(2):
                for hh in range(2):
                    rs = slice(hh * 64, (hh + 1) * 64)
                    P.stt("dve", Sf[rs, hp, :], Sf[rs, hp, :], dec[rs, hp:hp + 1],
                          psS[rs, hp * 256 + hh * 128:hp * 256 + (hh + 1) * 128], ALU.mult, ALU.add, [Sf, dec, psS], [Sf])
            P.cp("pool", Sfb[:], Sf[:], [Sf], [Sfb])
            if stg < 9:
                continue
            osb, on, ob, st = osr.next(), onr.next(), obr.next(), str_.next()
            P.cp("act", osb[:], psO[:], [psO], [osb])
            P.tt("pool", sqj[:], osb[:], osb[:], ALU.mult, [osb], [sqj])
            P.op("dve", lambda: nc.vector.tensor_reduce(st[:, 0:4], sqj[:].rearrange("p (h e) -> p h e", h=4), AX.X, ALU.add),
                 [sqj], [st])
            rstd(P, st, st[:, 0:4], st[:, 0:4], 128, K["epsc"])
            o3 = osb[:].rearrange("p (h e) -> p h e", h=4)
            n3 = on[:].rearrange("p (h e) -> p h e", h=4)
            P.tt("dve", n3, o3, st[:, 0:4].unsqueeze(2).broadcast_to([128, 4, 128]), ALU.mult, [osb, st], [on])
            P.tt("pool", n3, n3, K["glag"][:].unsqueeze(1).broadcast_to([128, 4, 128]), ALU.mult, [on, K["glag"]], [on])
            P.tt("dve", ob[:], on[:], gr[:], ALU.mult, [on, gr], [ob])
            for h in range(4):
                P.tr(psT[:, h, :], ob[:, h * 128:(h + 1) * 128], K["ident_b"][:], [ob, K["ident_b"]], [psT])
            P.cp("act", yg[:, :, osl], psT[:], [psT], [yg])
            if off_ + 128 == tn_:
                P.dma("sp", X["YGT"][:, :, t0_:t0_ + tn_].rearrange("c p t -> p c t"), yg[:, :, 0:tn_], yg, reads=[yg])


def phase6(P, I, X, L, last, lam_init, K):
    nc = P.nc
    with ExitStack() as es:
        WD = P.sb(es, [128, 8, D], BF16)
        WC = P.sb(es, [128, 4, D], BF16)
        WG = P.sb(es, [128, 4, D], BF16)
        WO = P.sb(es, [128, 8, D], BF16)
        for wt, nm in ((WD, "diff_w_out"), (WC, "conv_w_out"), (WG, "gla_w_out"), (WO, "w_o")):
            P.dma("pool", wt[:], I[nm][L].rearrange("(kc p) n -> p kc n", p=128), wt, writes=[wt])
        RW = P.sb(es, [128, 8, NE], F32)
        P.dma("sp", RW[:], I["router_w"][L].rearrange("(kc p) e -> p kc e", p=128), RW, writes=[RW])
        RB = P.sb(es, [128, NE], F32)
        P.dma("sp", RB[:], I["router_b"][L].partition_broadcast(128), RB, writes=[RB])
        nw = 1 if last else 2
        msl = [[P.sb(es, [128, D], F32) for _ in range(3)] for _ in range(nw)]
        for w in range(nw):
            for j, seg in enumerate((2, 3, 4)):
                P.dma("sp", msl[w][j][:], X["MODS"][w][:, seg * D:(seg + 1) * D], msl[w][j], writes=[msl[w][j]])
        dTr = Ring([P.sb(es, [128, 8, 512], BF16) for _ in range(2)])
        ycr = Ring([P.sb(es, [128, 4, 512], BF16) for _ in range(2)])
        ygr = Ring([P.sb(es, [128, 4, 512], BF16) for _ in range(2)])
        sgr = Ring([P.sb(es, [128, 3, 512], BF16) for _ in range(3)])
        mgr = Ring([P.sb(es, [128, 8, 512], BF16) for _ in range(1)])
        m1r = Ring([P.sb(es, [128, 512], F32) for _ in range(2)])
        m2r = Ring([P.sb(es, [128, 512], F32) for _ in range(2)])
        m3r = Ring([P.sb(es, [128, 512], F32) for _ in range(2)])
        xr = Ring([P.sb(es, [128, D], F32) for _ in range(2)])
        xnr = Ring([P.sb(es, [128, D], F32) for _ in range(1)])
        tmr = Ring([P.sb(es, [128, 512], F32) for _ in range(2)])
        hbr = Ring([P.sb(es, [128, D], F32) for _ in range(1)])
        scr_b = P.sb(es, [128, D], F32)
        str_ = Ring([P.sb(es, [128, 2], F32) for _ in range(2)])
        h32r = Ring([P.sb(es, [128, 8, 128], F32) for _ in range(1)])
        h2st = Ring([P.sb(es, [128, 8, 512], BF16) for _ in range(1)])
        gtst = Ring([P.sb(es, [128, 4, NE], F32) for _ in range(2)])
        lgr = Ring([P.sb(es, [128, NE], F32) for _ in range(2)])
        er = Ring([P.sb(es, [128, NE], F32) for _ in range(2)])
        mkr = Ring([P.sb(es, [128, NE], F32) for _ in range(2)])
        m8r = Ring([P.sb(es, [128, 16], F32) for _ in range(2)])
        psbr = Ring([P.ps(es, [128, 512]) for _ in range(3)])
        psor = Ring([P.ps(es, [128, 512]) for _ in range(2)])
        ptr = P.ps(es, [128, 8, 128], F32)
        pslg = P.ps(es, [128, 512])
        sig4 = X["SIGT"].rearrange("(b c) p t -> c p b t", b=3)
        tiles = TILES[1:] if last else TILES
        for (t0, n) in tiles:
            w = 1 if t0 == 0 else 0
            g1, sh2, G2 = msl[w]
            dT, yc, yg = dTr.next(), ycr.next(), ygr.next()
            P.dma("sp", dT[:, :, 0:n], X["DIFFT"][:, :, t0:t0 + n].rearrange("c p t -> p c t"), dT, writes=[dT])
            P.dma("sp", yc[:, :, 0:n], X["YCT"][:, :, t0:t0 + n].rearrange("c p t -> p c t"), yc, writes=[yc])
            P.dma("sp", yg[:, :, 0:n], X["YGT"][:, :, t0:t0 + n].rearrange("c p t -> p c t"), yg, writes=[yg])
            mg = mgr.next()
            for c in range(8):
                sg = sgr.next()
                P.dma("sp", sg[:, :, 0:n], sig4[c][:, :, t0:t0 + n], sg, writes=[sg])
                cs = slice(c * 128, (c + 1) * 128)
                pd, pc, pg = psbr.next(), psbr.next(), psbr.next()
                for kc in range(8):
                    P.mm(pd[:, 0:n], WD[:, kc, cs], dT[:, kc, 0:n], kc == 0, kc == 7, [WD, dT], [pd])
                for kc in range(4):
                    P.mm(pc[:, 0:n], WC[:, kc, cs], yc[:, kc, 0:n], kc == 0, kc == 3, [WC, yc], [pc])
                for kc in range(4):
                    P.mm(pg[:, 0:n], WG[:, kc, cs], yg[:, kc, 0:n], kc == 0, kc == 3, [WG, yg], [pg])
                m1, m2, m3 = m1r.next(), m2r.next(), m3r.next()
                P.tt("dve", m1[:, 0:n], pd[:, 0:n], sg[:, 0, 0:n], ALU.mult, [pd, sg], [m1])
                P.tt("dve", m2[:, 0:n], pc[:, 0:n], sg[:, 1, 0:n], ALU.mult, [pc, sg], [m2])
                P.tt("dve", m3[:, 0:n], pg[:, 0:n], sg[:, 2, 0:n], ALU.mult, [pg, sg], [m3])
                P.tt("pool", m1[:, 0:n], m1[:, 0:n], m2[:, 0:n], ALU.add, [m1, m2], [m1])
                P.tt("pool", mg[:, c, 0:n], m1[:, 0:n], m3[:, 0:n], ALU.add, [m1, m3], [mg])
            h2s = h2st.next()
            gts = gtst.next()
            stg6 = DBG.get("p6_stage", 99)
            if stg6 < 2:
                continue
            for s in range(n // 128):
                r0 = t0 + s * 128
                xt, xn = xr.next(), xnr.next()
                P.dma("sp", xt[:], xsrc(I, X, L, r0), xt, writes=[xt])
                for hf in range(2):
                    po = psor.next()
                    hs = slice(hf * 512, (hf + 1) * 512)
                    for kc in range(8):
                        P.mm(po[:], mg[:, kc, s * 128:(s + 1) * 128], WO[:, kc, hs], kc == 0, kc == 7, [mg, WO], [po])
                    tm = tmr.next()
                    P.tt("dve", tm[:], po[:], g1[:, hs], ALU.mult, [po, g1], [tm])
                    P.tt("pool", xt[:, hs], xt[:, hs], tm[:], ALU.add, [xt, tm], [xt])
                P.dma("sp", X["XR"][r0:r0 + 128, :], xt[:], xt, reads=[xt])
                if stg6 < 3:
                    continue
                hb, st = hbr.next(), str_.next()
                norm_mod(P, xt, G2, sh2, hb, scr_b, st, K["epsc"], xo=xn)
                if stg6 < 3.3:
                    continue
                for kc in range(8):
                    P.mm(ptr[:, kc, :], hb[:, kc * 128:(kc + 1) * 128], K["ident_f"][:], True, True, [hb, K["ident_f"]], [ptr])
                if stg6 < 3.6:
                    continue
                h32 = h32r.next()
                P.cp("act", h32[:], ptr[:], [ptr], [h32])
                if stg6 < 3.8:
                    continue
                P.cp("act", h2s[:, :, s * 128:(s + 1) * 128], ptr[:], [ptr], [h2s])
                if stg6 < 4:
                    continue
                for kc in range(8):
                    P.mm(pslg[:, 0:NE], h32[:, kc, :], RW[:, kc, :], kc == 0, kc == 7, [h32, RW], [pslg])
                lg, e_, mk, m8 = lgr.next(), er.next(), mkr.next(), m8r.next()
                P.tt("dve", lg[:], pslg[:, 0:NE], RB[:], ALU.add, [pslg, RB], [lg])
                P.op("dve", lambda: nc.vector.max(m8[:, 0:8], lg[:]), [lg], [m8])
                P.ts("dve", m8[:, 8:9], m8[:, 0:1], -1.0, None, ALU.mult, None, [m8], [m8])
                P.ts("dve", mk[:], lg[:], m8[:, 3:4], None, ALU.is_ge, None, [lg, m8], [mk])
                P.act(e_[:], lg[:], AF.Exp, [lg, m8], [e_], bias=m8[:, 8:9])
                P.tt("dve", e_[:], e_[:], mk[:], ALU.mult, [e_, mk], [e_])
                P.op("dve", lambda: nc.vector.tensor_reduce(m8[:, 9:10], e_[:], AX.X, ALU.add), [e_], [m8])
                P.op("dve", lambda: nc.vector.reciprocal(m8[:, 10:11], m8[:, 9:10]), [m8], [m8])
                P.ts("dve", gts[:, s, :], e_[:], m8[:, 10:11], None, ALU.mult, None, [e_, m8], [gts])
            if stg6 < 4:
                continue
            P.dma("sp", X["H2T"][:, :, t0:t0 + n].rearrange("c p t -> p c t"), h2s[:, :, 0:n], h2s, reads=[h2s])
            P.dma("sp", X["GATES"][t0:t0 + n, :].rearrange("(s p) e -> p s e", p=128), gts[:, 0:n // 128, :], gts, reads=[gts])


def phase7(P, I, X, L, last, lam_init, K, yout):
    nc = P.nc
    with ExitStack() as es:
        nw = 1 if last else 2
        g2 = [P.sb(es, [128, D], F32) for _ in range(nw)]
        for w in range(nw):
            P.dma("sp", g2[w][:], X["MODS"][w][:, 5 * D:6 * D], g2[w], writes=[g2[w]])
        B1T = P.sb(es, [128, NE, 2, 8], F32)
        with nc.allow_non_contiguous_dma(reason="bias de-interleave"):
            for e in range(NE):
                for s_ in range(2):
                    P.dma("sp", B1T[:, e, s_, :], I["moe_b1"][L, e].rearrange("(j p s) -> s p j", p=128, s=2)[s_], B1T, writes=[B1T])
        B2 = P.sb(es, [NE, D], F32)
        P.dma("sp", B2[:], I["moe_b2"][L], B2, writes=[B2])
        FG = None
        if last:
            FG = P.sb(es, [128, D], F32)
            P.dma("sp", FG[:], I["final_norm_g"].partition_broadcast(128), FG, writes=[FG])
        acc = P.sb(es, [128, 8, D], F32)
        h2 = Ring([P.sb(es, [128, 8, 1024], BF16) for _ in range(1)])
        gtr = Ring([P.sb(es, [128, 8, NE], F32) for _ in range(1)])
        gT = Ring([P.sb(es, [NE, 128], F32) for _ in range(2)])
        w1r = Ring([P.sb(es, [128, 8, 512], BF16) for _ in range(5)])
        w2r = Ring([P.sb(es, [128, 8, 512], BF16) for _ in range(4)])
        actr = Ring([P.sb(es, [128, 8, 512], BF16) for _ in range(1)])
        glr = Ring([P.sb(es, [128, 512], F32) for _ in range(2)])
        sgr = Ring([P.sb(es, [128, 512], F32) for _ in range(2)])
        lnr = Ring([P.sb(es, [128, 512], F32) for _ in range(2)])
        xr = Ring([P.sb(es, [128, D], F32) for _ in range(2)])
        scr_b = P.sb(es, [128, D], F32)
        str_ = Ring([P.sb(es, [128, 2], F32) for _ in range(2)])
        psg = Ring([P.ps(es, [128, 512]) for _ in range(2)])
        psl = Ring([P.ps(es, [128, 512]) for _ in range(2)])
        pso = Ring([P.ps(es, [128, 512]) for _ in range(3)])
        pst = P.ps(es, [128, 512])
        if last:
            tiles = [(C + 1024 * i, 1024) for i in range(4)]
        else:
            tiles = [(0, C)] + [(C + 1024 * i, 1024) for i in range(4)]
        w1v = I["moe_w1"][L].rearrange("e (kc p) n -> e p kc n", p=128)
        w2v = I["moe_w2"][L].rearrange("e (kc p) n -> e p kc n", p=128)
        for (t0, n) in tiles:
            w = 1 if t0 == 0 else 0
            nsub = n // 128
            h2t, gt = h2.next(), gtr.next()
            P.dma("sp", h2t[:, :, 0:n], X["H2T"][:, :, t0:t0 + n].rearrange("c p t -> p c t"), h2t, writes=[h2t])
            P.dma("sp", gt[:, 0:nsub, :], X["GATES"][t0:t0 + n, :].rearrange("(s p) e -> p s e", p=128), gt, writes=[gt])
            for s in range(nsub):
                P.mm(pst[0:NE, 0:128], gt[:, s, :], K["ident_f"][:], True, True, [gt, K["ident_f"]], [pst])
                g_t = gT.next()
                P.cp("act", g_t[:], pst[0:NE, 0:128], [pst], [g_t])
                for hf in range(2):
                    po = pso.next()
                    P.mm(po[:], g_t[:], B2[:, hf * 512:(hf + 1) * 512], True, True, [g_t, B2], [po])
                    P.cp("act", acc[:, s, hf * 512:(hf + 1) * 512], po[:], [po], [acc])
            W1, W2 = {}, {}

            def load_w1(e, q):
                t_ = w1r.next()
                P.dma("pool", t_[:], w1v[e][:, :, q * 512:(q + 1) * 512], t_, writes=[t_])
                W1[(e, q)] = t_

            def load_w2(e, hf):
                t_ = w2r.next()
                P.dma("pool", t_[:], w2v[e][:, :, hf * 512:(hf + 1) * 512], t_, writes=[t_])
                W2[(e, hf)] = t_

            for q in range(4):
                load_w1(0, q)
            for hf in range(2):
                load_w2(0, hf)
            subtiles = [(j0, min(512, n - j0)) for j0 in range(0, n, 512)]
            for e in range(NE):
                for si, (j0, nj) in enumerate(subtiles):
                    pre = si == len(subtiles) - 1 and e + 1 < NE
                    at = actr.next()
                    for q in range(4):
                        w1s = W1[(e, q)]
                        for gch in range(2):
                            fc = q * 2 + gch
                            pg, pl = psg.next(), psl.next()
                            for sidx, pp in ((0, pg), (1, pl)):
                                for kc in range(8):
                                    P.mm(pp[:, 0:nj], w1s[:, kc, gch * 256 + sidx:gch * 256 + 256:2],
                                         h2t[:, kc, j0:j0 + nj], kc == 0, kc == 7, [w1s, h2t], [pp])
                            gl, sg, ln = glr.next(), sgr.next(), lnr.next()
                            P.ts("dve", gl[:, 0:nj], pg[:, 0:nj], B1T[:, e, 0, fc:fc + 1], 7.0, ALU.add, ALU.min, [pg, B1T], [gl])
                            P.act(sg[:, 0:nj], gl[:, 0:nj], AF.Sigmoid, [gl], [sg], scale=1.702)
                            P.ts("dve", ln[:, 0:nj], pl[:, 0:nj], B1T[:, e, 1, fc:fc + 1], 7.0, ALU.add, ALU.min, [pl, B1T], [ln])
                            P.ts("dve", ln[:, 0:nj], ln[:, 0:nj], -7.0, 1.0, ALU.max, ALU.add, [ln], [ln])
                            P.tt("pool", gl[:, 0:nj], gl[:, 0:nj], sg[:, 0:nj], ALU.mult, [gl, sg], [gl])
                            P.tt("pool", at[:, fc, 0:nj], gl[:, 0:nj], ln[:, 0:nj], ALU.mult, [gl, ln], [at])
                        if pre:
                            load_w1(e + 1, q)
                    if pre:
                        load_w2(e + 1, 0)
                        load_w2(e + 1, 1)
                    for s in range(nj // 128):
                        sidx = j0 // 128 + s
                        for hf in range(2):
                            po = pso.next()
                            w2s = W2[(e, hf)]
                            for fc in range(8):
                                P.mm(po[:], at[:, fc, s * 128:(s + 1) * 128], w2s[:, fc, :], fc == 0, fc == 7, [at, w2s], [po])
                            asl = acc[:, sidx, hf * 512:(hf + 1) * 512]
                            P.stt("dve", asl, po[:], gt[:, sidx, e:e + 1], asl, ALU.mult, ALU.add, [po, gt, acc], [acc])
            for s in range(nsub):
                r0 = t0 + s * 128
                xt = xr.next()
                P.dma("sp", xt[:], X["XR"][r0:r0 + 128, :], xt, writes=[xt])
                P.tt("dve", acc[:, s, :], acc[:, s, :], g2[w][:], ALU.mult, [acc, g2[w]], [acc])
                P.tt("pool", xt[:], xt[:], acc[:, s, :], ALU.add, [xt, acc], [xt])
                if not last:
                    P.dma("sp", X["XR"][r0:r0 + 128, :], xt[:], xt, reads=[xt])
                else:
                    st = str_.next()
                    sumsq(P, scr_b, xt, st)
                    rstd(P, st, st[:, 1:2], st[:, 0:1], D, K["epsc"])
                    P.stt("dve", xt[:], xt[:], st[:, 1:2], FG[:], ALU.mult, ALU.mult, [xt, st, FG], [xt])
                    P.dma("sp", yout[r0 - C:r0 - C + 128, :], xt[:], xt, reads=[xt])


def host_consts():
    inv_freq = (10000.0 ** (-np.arange(0, 32, 2, dtype=np.float32) / 32.0)).astype(np.float32)
    pos = np.arange(S)
    row = (pos // 64).astype(np.float32)
    col = (pos % 64).astype(np.float32)
    ang = np.concatenate([row[:, None] * inv_freq[None, :], col[:, None] * inv_freq[None, :]], axis=1).astype(np.float32)
    m = np.arange(128)
    tri = np.zeros((6, 128, 128), np.float32)
    tri[0] = (m[:, None] <= m[None, :]) * (-1.0 / 16.0)
    tri[1] = (m[:, None] >= m[None, :]) * (-1.0 / 16.0)
    tri[2] = (m[:, None] > m[None, :]) * (-1.0 / 16.0)
    tri[3] = (m[:, None] < m[None, :]) * (-1.0 / 16.0)
    tri[4] = (m[:, None] <= m[None, :]) * 1.0
    tri[5] = (m[:, None] >= m[None, :]) * 1.0
    return dict(k_cos=np.cos(ang).astype(np.float32), k_sin=np.sin(ang).astype(np.float32),
                k_ident=np.eye(128, dtype=np.float32), k_tri=tri)


def make_in_maps(inputs, cores, used=None):
    kc = host_consts()
    f = lambda a: np.ascontiguousarray(np.asarray(a, dtype=np.float32))
    shared = {k: f(inputs[k]) for k in ("c_ctx", "ada_w", "ada_b", "norm1_g", "norm2_g", "w_in", "diff_subln_g",
                                        "diff_w_out", "conv_w", "conv_w_out", "gla_w_a2", "gla_norm_g", "gla_w_out",
                                        "w_o", "router_w", "router_b", "moe_w1", "moe_b1", "moe_w2", "moe_b2",
                                        "final_norm_g")}
    shared["diff_lambda"] = f(inputs["diff_lambda"]).reshape(DEPTH, 256)
    shared["gla_b_a"] = f(inputs["gla_b_a"]).reshape(DEPTH, 512)
    shared.update(kc)
    maps = []
    for b in cores:
        m = dict(shared)
        m["x"] = f(inputs["x"][b])
        m["c"] = f(inputs["c"][b])
        m["ctx"] = f(inputs["ctx"][b])
        if used is not None:
            m = {k: v for k, v in m.items() if k in used}
        maps.append(m)
    return maps


def kernel(**inputs):
    nc = build()
    maps = make_in_maps(inputs, list(range(8)))
    res = run_bass_kernel_spmd(nc, maps, core_ids=list(range(8)))
    return np.stack([np.asarray(r["y"], dtype=np.float32) for r in res.results], axis=0)
```

```python
import math
from contextlib import ExitStack
import numpy as np
import concourse.bass as bass
import concourse.mybir as mybir
from concourse.bass_utils import run_bass_kernel_spmd

F32 = mybir.dt.float32
BF16 = mybir.dt.bfloat16
AF = mybir.ActivationFunctionType
ALU = mybir.AluOpType
AX = mybir.AxisListType

D = 1024
S = 4096
C = 256
T = S + C
NT = T // 128
DEPTH = 2
NE = 32
WIN = 9248
EPS = 1e-6
OQ, OK_, OV = 0, 1024, 2048
OCB, OCC, OCX = 3072, 3584, 4096
OGQ, OGK, OGV, OGR, OGA = 4608, 4864, 5120, 5632, 6144
OGT = 6176
TILES = [(0, 256)] + [(256 + 512 * i, 512) for i in range(8)]
MOE_CAP = 256


DBG = {}


class Dep:
    def __init__(self):
        self.w = None
        self.r = {}
        self.ds = None


class TL(Dep):
    def __init__(self, h):
        super().__init__()
        self.h = h

    def __getitem__(self, k):
        return self.h[k]


class DSem:
    def __init__(self, sem):
        self.sem = sem
        self.cnt = 0


class Prog:
    def __init__(self, nc, ndsem=96):
        self.nc = nc
        self.E = {"pe": nc.tensor, "act": nc.scalar, "dve": nc.vector, "pool": nc.gpsimd, "sp": nc.sync}
        self.sem = {k: nc.alloc_semaphore("s_" + k) for k in self.E}
        self.cnt = {k: 0 for k in self.E}
        self.seen = {k: {} for k in self.E}
        self.dpool = [DSem(nc.alloc_semaphore("d%d" % i)) for i in range(ndsem)]
        self.dnext = 0
        self.persist = 0
        self.nsb = 0
        self.pe_cols = [0]
        self.dly = {}
        self.nfence = 0

    def sb(self, es, shape, dt, name=None):
        self.nsb += 1
        h = es.enter_context(self.nc.sbuf_tensor("t%d" % self.nsb, list(shape), dt))
        return TL(h)

    def ps(self, es, shape, dt=F32):
        self.nsb += 1
        h = es.enter_context(self.nc.psum_tensor("p%d" % self.nsb, list(shape), dt))
        return TL(h)

    def _ds(self, t):
        if t.ds is None:
            assert self.dnext < len(self.dpool), "out of dma semaphores"
            t.ds = self.dpool[self.dnext]
            self.dnext += 1
        return t.ds

    def _wait(self, eng, ev, raw=False):
        if ev is None:
            return
        if ev[0] == "e":
            _, src, val = ev
            if src == eng and (eng == "pe" or not raw):
                return
            key = src
            sem = self.sem[src]
            if src == "pe":
                need = self.pe_cols[val] + 256
                k2 = val
                while k2 < self.cnt["pe"] and self.pe_cols[k2] < need:
                    k2 += 1
                if self.pe_cols[k2] >= need:
                    val = k2
                else:
                    val = self.cnt["pe"]
                    if self.seen[eng].get(key, 0) < val:
                        self.E[eng].wait_ge(sem, val)
                        self.seen[eng][key] = val
                    if self.seen[eng].get("pe_safe", 0) < val:
                        self._delay(eng)
                        self.seen[eng]["pe_safe"] = val
                    return
                if self.seen[eng].get("pe_safe", 0) < val:
                    self.seen[eng]["pe_safe"] = val
        else:
            ds = ev[1]
            key = id(ds)
            sem = ds.sem
            val = ds.cnt
        if self.seen[eng].get(key, 0) >= val:
            return
        self.E[eng].wait_ge(sem, val)
        self.seen[eng][key] = val

    def _delay(self, eng):
        if eng not in self.dly:
            return
        self.nfence += 1
        d = self.dly[eng]
        if eng == "act":
            self.nc.scalar.copy(d[:, 0:256], d[:, 256:512])
        else:
            self.E[eng].memset(d[:, 0:256], 0.0)

    def _deps(self, eng, reads, writes):
        for t in reads:
            self._wait(eng, t.w, raw=True)
        for t in writes:
            self._wait(eng, t.w)
            for ev in list(t.r.values()):
                self._wait(eng, ev)

    def _mark(self, ev, key, reads, writes):
        for t in reads:
            t.r[key] = ev
        for t in writes:
            t.w = ev
            t.r = {}

    def op(self, eng, ins_fn, reads=(), writes=(), pe_n=None):
        self._deps(eng, reads, writes)
        ins = ins_fn()
        self.cnt[eng] += 1
        if eng == "pe":
            if pe_n is None:
                try:
                    pe_n = int(ins.ins.outs[0].free_size()) if False else 128
                except Exception:
                    pe_n = 128
            self.pe_cols.append(self.pe_cols[-1] + pe_n)
        ins.then_inc(self.sem[eng], 1)
        self._mark(("e", eng, self.cnt[eng]), eng, reads, writes)
        return ins

    def dma(self, q, out, in_, holder, reads=(), writes=(), **kw):
        self._deps(q, reads, writes)
        ds = self._ds(holder)
        ins = self.E[q].dma_start(out=out, in_=in_, **kw)
        ins.then_inc(ds.sem, 16)
        ds.cnt += 16
        self._mark(("d", ds), id(ds), reads, writes)

    def barrier(self):
        for ds in self.dpool[: self.dnext]:
            if ds.cnt > 0:
                self._wait("sp", ("d", ds))
        for e in self.E:
            if e != "sp":
                self._wait("sp", ("e", e, self.cnt[e]))
        self.E["sp"].sem_inc(self.sem["sp"], 1)
        self.cnt["sp"] += 1
        for e in self.E:
            if e == "sp":
                continue
            for o in self.E:
                if o != e:
                    self._wait(e, ("e", o, self.cnt[o]))
        self.dnext = self.persist

    def rep(self, name):
        print("SBUF remaining after", name, self.nc.sbuf_bytes_remaining, flush=True)

    def persist_dsems(self):
        self.persist = self.dnext

    def mm(self, out, lhsT, rhs, start, stop, reads, writes):
        n = 1
        for d_ in rhs.shape[1:]:
            n *= int(d_)
        return self.op("pe", lambda: self.nc.tensor.matmul(out, lhsT, rhs, start=start, stop=stop), reads, writes, pe_n=n)

    def tr(self, out, in_, ident, reads, writes):
        return self.op("pe", lambda: self.nc.tensor.transpose(out, in_, ident), reads, writes, pe_n=64)

    def act(self, out, in_, func, reads, writes, **kw):
        return self.op("act", lambda: self.nc.scalar.activation(out=out, in_=in_, func=func, **kw), reads, writes)

    def ts(self, eng, out, in0, s1, s2, op0, op1, reads, writes):
        eng = self.cmap(eng)
        e = self.E[eng]
        if op1 is None:
            return self.op(eng, lambda: e.tensor_scalar(out, in0, s1, None, op0), reads, writes)
        return self.op(eng, lambda: e.tensor_scalar(out, in0, s1, s2, op0, op1), reads, writes)

    def tt(self, eng, out, in0, in1, op, reads, writes):
        eng = self.cmap(eng)
        e = self.E[eng]
        return self.op(eng, lambda: e.tensor_tensor(out, in0, in1, op), reads, writes)

    def stt(self, eng, out, in0, scalar, in1, op0, op1, reads, writes):
        eng = self.cmap(eng)
        e = self.E[eng]
        return self.op(eng, lambda: e.scalar_tensor_tensor(out, in0, scalar, in1, op0, op1), reads, writes)

    def cp(self, eng, out, in_, reads, writes):
        eng = self.cmap(eng)
        if eng == "act":
            return self.op("act", lambda: self.nc.scalar.copy(out, in_), reads, writes)
        e = self.E[eng]
        return self.op(eng, lambda: e.tensor_copy(out, in_), reads, writes)

    def cmap(self, eng):
        return "dve" if (eng == "pool" and not DBG.get("pool_compute", False)) else eng

    def memset(self, eng, t, ap, val):
        eng = self.cmap(eng)
        e = self.E[eng]
        return self.op(eng, lambda: e.memset(ap, val), (), (t,))


class Ring:
    def __init__(self, tiles):
        self.t = tiles
        self.i = 0

    def next(self):
        t = self.t[self.i % len(self.t)]
        self.i += 1
        return t


def build(n_layers=DEPTH, debug_out=(), stop_after=None):
    nc = bass.Bass("TRN2", target_bir_lowering=False)
    P = Prog(nc)

    def din(name, shape, dt=F32):
        return nc.dram_tensor(name, list(shape), dt, kind="ExternalInput").ap()

    SHAPES = dict(x=[S, D], c=[D], ctx=[C, D], c_ctx=[D], ada_w=[DEPTH, D, 6 * D], ada_b=[DEPTH, 6 * D],
                  norm1_g=[DEPTH, D], norm2_g=[DEPTH, D], w_in=[DEPTH, D, WIN], diff_lambda=[DEPTH, 256],
                  diff_subln_g=[DEPTH, 128], diff_w_out=[DEPTH, D, D], conv_w=[DEPTH, 3, 512],
                  conv_w_out=[DEPTH, 512, D], gla_w_a2=[DEPTH, 2, 16, 256], gla_b_a=[DEPTH, 512],
                  gla_norm_g=[DEPTH, 128], gla_w_out=[DEPTH, 512, D], w_o=[DEPTH, D, D], router_w=[DEPTH, D, NE],
                  router_b=[DEPTH, NE], moe_w1=[DEPTH, NE, D, 2 * D], moe_b1=[DEPTH, NE, 2 * D],
                  moe_w2=[DEPTH, NE, D, D], moe_b2=[DEPTH, NE, D], final_norm_g=[D],
                  k_cos=[S, 32], k_sin=[S, 32], k_ident=[128, 128], k_tri=[6, 128, 128], k_iota=[MOE_CAP])

    class LazyIn(dict):
        def __missing__(self, k):
            self[k] = din(k, SHAPES[k])
            return self[k]

    I = LazyIn()
    if stop_after is None:
        for k in SHAPES:
            I[k]
    yout = nc.dram_tensor("y", [S, D], F32, kind="ExternalOutput").ap()

    def scr(name, shape, dt=F32):
        kind = "ExternalOutput" if name in debug_out else "Internal"
        return nc.dram_tensor(name, list(shape), dt, kind=kind).ap()

    X = {}
    X["XR"] = scr("XR", [T, D])
    X["MODS"] = scr("MODS", [2, 128, 6 * D])
    X["QT"] = scr("QT", [8, 128, T], BF16)
    X["KT"] = scr("KT", [8, 128, T], BF16)
    X["V"] = scr("V", [8, 128, NT * 132], BF16)
    X["CBT"] = scr("CBT", [4, 128, T])
    X["CCT"] = scr("CCT", [4, 128, T])
    X["CXT"] = scr("CXT", [4, 128, T])
    X["GQT"] = scr("GQT", [2, 128, T])
    X["GKT"] = scr("GKT", [2, 128, T])
    X["GK"] = scr("GK", [T, 256])
    X["GV"] = scr("GV", [T, 512], BF16)
    X["GR"] = scr("GR", [T, 512])
    X["GAF"] = scr("GAF", [16, T])
    X["GAB"] = scr("GAB", [16, T])
    X["SIGT"] = scr("SIGT", [24, 128, T], BF16)
    X["DIFFT"] = scr("DIFFT", [8, 128, T], BF16)
    X["YCT"] = scr("YCT", [4, 128, T], BF16)
    X["YGT"] = scr("YGT", [4, 128, T], BF16)
    X["H2T"] = scr("H2T", [8, 128, T], BF16)
    X["GATES"] = scr("GATES", [T, NE])
    X["H2M"] = scr("H2M", [T, D], BF16)
    if "HT" in debug_out:
        X["HT"] = scr("HT", [8, 128, T], BF16)

    with ExitStack() as gs:
        ident_f = P.sb(gs, [128, 128], F32)
        ident_b = P.sb(gs, [128, 128], BF16)
        ones_f = P.sb(gs, [128, 128], F32)
        lam = P.sb(gs, [128, 4], F32)
        subg = P.sb(gs, [128, 128], F32)
        glag = P.sb(gs, [128, 128], F32)
        P.dma("sp", ident_f[:], I["k_ident"], ident_f, writes=[ident_f])
        P.cp("dve", ident_b[:], ident_f[:], [ident_f], [ident_b])
        P.memset("dve", ones_f, ones_f[:], 1.0)
        for e_ in ("act", "dve"):
            P.dly[e_] = P.sb(gs, [128, 512], F32)
            P.memset("dve", P.dly[e_], P.dly[e_][:], 0.0)
        epsc = P.sb(gs, [128, 1], F32)
        P.memset("dve", epsc, epsc[:], EPS)
        P.persist_dsems()
        P.barrier()

        for L in range(n_layers):
            last = L == n_layers - 1 and n_layers == DEPTH
            lam_init = 0.8 - 0.6 * math.exp(-0.3 * L)
            phases = [phase0, phase1_2, phase3, phase4, phase5, phase6, phase7s if DBG.get('sparse_moe') else phase7]
            for ph in phases:
                kk = dict(ident_f=ident_f, ident_b=ident_b, ones_f=ones_f, lam=lam, subg=subg, glag=glag, epsc=epsc)
                if ph in (phase7, phase7s):
                    ph(P, I, X, L, last, lam_init, kk, yout)
                else:
                    ph(P, I, X, L, last, lam_init, kk)
                P.barrier()
                if stop_after == (L, ph.__name__):
                    break
            else:
                continue
            break
        P.barrier()
    nc._used_inputs = set(I.keys())
    return nc


def phase0(P, I, X, L, last, lam_init, K):
    nc = P.nc
    with ExitStack() as es:
        cs = P.sb(es, [128, 8, 2], F32)
        crep = [P.sb(es, [128, 8, 128], BF16) for _ in range(2)]
        mod = [P.sb(es, [128, 6 * D], F32) for _ in range(2)]
        grep = [P.sb(es, [128, D], F32) for _ in range(2)]
        wring = Ring([P.sb(es, [128, 8, 512], BF16) for _ in range(2)])
        pss = Ring([P.ps(es, [128, 512]) for _ in range(4)])
        dl = P.sb(es, [128, 256], F32)
        tmp = P.sb(es, [128, 256], F32)

        with nc.allow_non_contiguous_dma(reason="tiny transposed vector load"):
            P.dma("sp", cs[:, :, 0], I["c"].rearrange("(kc p) -> p kc", p=128), cs, writes=[cs])
            P.dma("sp", cs[:, :, 1], I["c_ctx"].rearrange("(kc p) -> p kc", p=128), cs, writes=[cs])
        P.act(cs[:], cs[:], AF.Silu, [cs], [cs])
        for w in range(2):
            for kc in range(8):
                P.ts("dve", crep[w][:, kc, :], K["ones_f"][:], cs[:, kc, w:w + 1], None, ALU.mult, None,
                     [cs, K["ones_f"]], [crep[w]])
            P.dma("sp", mod[w][:], I["ada_b"][L].partition_broadcast(128), mod[w], writes=[mod[w]])
        P.dma("sp", grep[0][:], I["norm1_g"][L].partition_broadcast(128), grep[0], writes=[grep[0]])
        P.dma("sp", grep[1][:], I["norm2_g"][L].partition_broadcast(128), grep[1], writes=[grep[1]])
        aw = I["ada_w"][L].rearrange("(kc p) n -> p kc n", p=128)
        for cb in range(12):
            wt = wring.next()
            P.dma("pool", wt[:], aw[:, :, cb * 512:(cb + 1) * 512], wt, writes=[wt])
            for w in range(2):
                ps = pss.next()
                for kc in range(8):
                    P.mm(ps[:], crep[w][:, kc, :], wt[:, kc, :], kc == 0, kc == 7, [crep[w], wt], [ps])
                sl = mod[w][:, cb * 512:(cb + 1) * 512]
                P.tt("dve", sl, ps[:], sl, ALU.add, [ps, mod[w]], [mod[w]])
        for w in range(2):
            for seg, g in ((1, grep[0]), (4, grep[1])):
                sl = mod[w][:, seg * D:(seg + 1) * D]
                P.stt("dve", sl, sl, 1.0, g[:], ALU.add, ALU.mult, [mod[w], g], [mod[w]])
            P.dma("sp", X["MODS"][w], mod[w][:], mod[w], reads=[mod[w]])
        lamt = K["lam"]
        P.dma("sp", dl[:], I["diff_lambda"][L].partition_broadcast(128), dl, writes=[dl])
        P.tt("dve", tmp[:, 0:64], dl[:, 0:64], dl[:, 64:128], ALU.mult, [dl], [tmp])
        P.tt("dve", tmp[:, 64:128], dl[:, 128:192], dl[:, 192:256], ALU.mult, [dl], [tmp])
        P.op("dve", lambda: nc.vector.tensor_reduce(lamt[:, 1:3], tmp[:, 0:128].rearrange("p (a b) -> p a b", a=2),
                                                    AX.X, ALU.add), [tmp], [lamt])
        P.act(lamt[:, 1:3], lamt[:, 1:3], AF.Exp, [lamt], [lamt])
        P.tt("dve", lamt[:, 0:1], lamt[:, 1:2], lamt[:, 2:3], ALU.subtract, [lamt], [lamt])
        P.ts("dve", lamt[:, 0:1], lamt[:, 0:1], float(lam_init), None, ALU.add, None, [lamt], [lamt])
        P.dma("sp", K["subg"][:], I["diff_subln_g"][L].partition_broadcast(128), K["subg"], writes=[K["subg"]])
        P.ts("dve", K["subg"][:], K["subg"][:], float(1.0 - lam_init), None, ALU.mult, None, [K["subg"]], [K["subg"]])
        P.dma("sp", K["glag"][:], I["gla_norm_g"][L].partition_broadcast(128), K["glag"], writes=[K["glag"]])


def rstd(P, st, out_ap, in_ap, n, epsc):
    P.act(out_ap, in_ap, AF.Ln, [st, epsc], [st], scale=1.0 / n, bias=epsc[:, 0:1])
    P.act(out_ap, out_ap, AF.Exp, [st], [st], scale=-0.5)


def xsrc(I, X, L, r0):
    if L > 0:
        return X["XR"][r0:r0 + 128, :]
    if r0 < C:
        return I["ctx"][r0:r0 + 128, :]
    return I["x"][r0 - C:r0 - C + 128, :]


def sumsq(P, scr, xt, st):
    P.act(scr[:], xt[:], AF.Square, [xt], [scr])
    P.op("dve", lambda: P.nc.vector.tensor_reduce(st[:, 0:1], scr[:], AX.X, ALU.add), [scr], [st])


def norm_mod(P, xt, Gt, SHt, hb, scr_b, st, epsc, eng2="pool", xo=None):
    nc = P.nc
    sumsq(P, scr_b, xt, st)
    rstd(P, st, st[:, 1:2], st[:, 0:1], D, epsc)
    xo = xt if xo is None else xo
    P.stt("dve", xo[:], xt[:], st[:, 1:2], Gt[:], ALU.mult, ALU.mult, [xt, st, Gt], [xo])
    P.tt(eng2, hb[:], xo[:], SHt[:], ALU.add, [xo, SHt], [hb])


def phase1_2(P, I, X, L, last, lam_init, K):
    nc = P.nc
    with ExitStack() as es:
        hT = P.sb(es, [128, 8, T], BF16)
        with ExitStack() as e1:
            msl = [[P.sb(e1, [128, D], F32) for _ in range(2)] for _ in range(2)]
            for w in range(2):
                for j, seg in enumerate((0, 1)):
                    P.dma("sp", msl[w][j][:], X["MODS"][w][:, seg * D:(seg + 1) * D], msl[w][j], writes=[msl[w][j]])
            xr = Ring([P.sb(e1, [128, D], F32) for _ in range(3)])
            hbr = Ring([P.sb(e1, [128, D], BF16) for _ in range(2)])
            scr_b = P.sb(e1, [128, D], F32)
            str_ = Ring([P.sb(e1, [128, 2], F32) for _ in range(2)])
            ptr = Ring([P.ps(e1, [128, 8, 128], BF16) for _ in range(2)])
            for i in range(NT):
                w = 1 if i < 2 else 0
                xt = xr.next()
                P.dma("sp", xt[:], xsrc(I, X, L, i * 128), xt, writes=[xt])
                hb = hbr.next()
                st = str_.next()
                norm_mod(P, xt, msl[w][1], msl[w][0], hb, scr_b, st, K["epsc"])
                pt = ptr.next()
                for kc in range(8):
                    P.tr(pt[:, kc, :], hb[:, kc * 128:(kc + 1) * 128], K["ident_b"][:], [hb, K["ident_b"]], [pt])
                P.cp("act", hT[:, :, i * 128:(i + 1) * 128], pt[:], [pt], [hT])
            P.barrier()
        if "HT" in X:
            P.dma("sp", X["HT"].rearrange("c p t -> p c t"), hT[:], hT, reads=[hT])
            return
        phase2(P, I, X, L, K, hT)


def phase2(P, I, X, L, K, hT):
    nc = P.nc
    wv = I["w_in"][L].rearrange("(kc p) n -> p kc n", p=128)
    with ExitStack() as es:
        wring = Ring([P.sb(es, [128, 8, 512], BF16) for _ in range(3)])
        psr = Ring([P.ps(es, [128, 512]) for _ in range(4)])
        ptr = Ring([P.ps(es, [128, 4, 128], BF16) for _ in range(2)])
        cos = P.sb(es, [128, 32, 32], F32)
        sin = P.sb(es, [128, 32, 32], F32)
        P.dma("sp", cos[:], I["k_cos"].rearrange("(t p) f -> p t f", p=128), cos, writes=[cos])
        P.dma("sp", sin[:], I["k_sin"].rearrange("(t p) f -> p t f", p=128), sin, writes=[sin])
        ra = Ring([P.sb(es, [128, 512], F32) for _ in range(2)])
        rb = Ring([P.sb(es, [128, 512], F32) for _ in range(2)])
        rob = Ring([P.sb(es, [128, 512], BF16) for _ in range(2)])
        stq = Ring([P.sb(es, [128, 4, 512], BF16) for _ in range(2)])
        stf = Ring([P.sb(es, [128, 4, 512], F32) for _ in range(2)])

        def load_w(c0, ncols):
            wt = wring.next()
            P.dma("pool", wt[:, :, 0:ncols], wv[:, :, c0:c0 + ncols], wt, writes=[wt])
            return wt

        def tok_major(wt, cw0, ncols, i):
            ps = psr.next()
            for kc in range(8):
                P.mm(ps[:, 0:ncols], hT[:, kc, i * 128:(i + 1) * 128], wt[:, kc, cw0:cw0 + ncols],
                     kc == 0, kc == 7, [hT, wt], [ps])
            return ps

        def feat_major(wt, cw0, m, t0, n):
            ps = psr.next()
            for kc in range(8):
                P.mm(ps[0:m, 0:n], wt[:, kc, cw0:cw0 + m], hT[:, kc, t0:t0 + n], kc == 0, kc == 7, [hT, wt], [ps])
            return ps

        for which, dst, c_base in (("q", X["QT"], OQ), ("k", X["KT"], OK_)):
            for half in range(2):
                wt = load_w(c_base + half * 512, 512)
                for (t0, n) in TILES:
                    sq = stq.next()
                    for s in range(n // 128):
                        i = (t0 + s * 128) // 128
                        ps = tok_major(wt, 0, 512, i)
                        ob = rob.next()
                        if i < 2:
                            P.cp("act", ob[:], ps[:], [ps], [ob])
                        else:
                            li = i - 2
                            a = ra.next()
                            b = rb.next()
                            for ax in range(2):
                                def v4(ap):
                                    return ap.rearrange("p (h r) -> p h r", r=64)[:, :, ax * 32:(ax + 1) * 32].rearrange("p h (s f) -> p h s f", s=2)
                                x4, a4, b4, o4 = v4(ps[:]), v4(a[:]), v4(b[:]), v4(ob[:])
                                cs4 = cos[:, li, ax * 16:(ax + 1) * 16].unsqueeze(1).unsqueeze(1).broadcast_to([128, 8, 2, 16])
                                sn3 = sin[:, li, ax * 16:(ax + 1) * 16].unsqueeze(1).broadcast_to([128, 8, 16])
                                P.tt("dve", a4, x4, cs4, ALU.mult, [ps, cos], [a])
                                P.tt("dve", b4[:, :, 0, :], x4[:, :, 1, :], sn3, ALU.mult, [ps, sin], [b])
                                P.tt("dve", b4[:, :, 1, :], x4[:, :, 0, :], sn3, ALU.mult, [ps, sin], [b])
                                P.tt("pool", o4[:, :, 0, :], a4[:, :, 0, :], b4[:, :, 0, :], ALU.subtract, [a, b], [ob])
                                P.tt("pool", o4[:, :, 1, :], a4[:, :, 1, :], b4[:, :, 1, :], ALU.add, [a, b], [ob])
                        pt = ptr.next()
                        for hh in range(4):
                            P.tr(pt[:, hh, :], ob[:, hh * 128:(hh + 1) * 128], K["ident_b"][:], [ob, K["ident_b"]], [pt])
                        P.cp("act", sq[:, :, s * 128:(s + 1) * 128], pt[:], [pt], [sq])
                    P.dma("sp", dst[half * 4:(half + 1) * 4, :, t0:t0 + n].rearrange("h p t -> p h t"),
                          sq[:, :, 0:n], sq, reads=[sq])
        with ExitStack() as ev_:
            vst = P.sb(ev_, [128, 4, NT, 132], BF16)
            P.memset("dve", vst, vst[:].rearrange("p h t e -> p (h t e)"), 1.0)
            for half in range(2):
                wt = load_w(OV + half * 512, 512)
                for i in range(NT):
                    ps = tok_major(wt, 0, 512, i)
                    P.cp("act", vst[:, :, i, 0:128], ps[:].rearrange("p (h e) -> p h e", h=4), [ps], [vst])
                for hh in range(4):
                    P.dma("sp", X["V"][half * 4 + hh], vst[:, hh, :, :].rearrange("p t e -> p (t e)"), vst, reads=[vst])
        for dst, c0 in ((X["CBT"], OCB), (X["CCT"], OCC), (X["CXT"], OCX)):
            wt = load_w(c0, 512)
            for (t0, n) in TILES:
                sf = stf.next()
                for ch in range(4):
                    ps = feat_major(wt, ch * 128, 128, t0, n)
                    P.cp("act", sf[:, ch, 0:n], ps[:, 0:n], [ps], [sf])
                P.dma("sp", dst[:, :, t0:t0 + n].rearrange("c p t -> p c t"), sf[:, :, 0:n], sf, reads=[sf])
        wt = load_w(OGQ, 512)
        for (t0, n) in TILES:
            sf = stf.next()
            for ch in range(4):
                ps = feat_major(wt, ch * 128, 128, t0, n)
                P.cp("act", sf[:, ch, 0:n], ps[:, 0:n], [ps], [sf])
            P.dma("sp", X["GQT"][:, :, t0:t0 + n].rearrange("c p t -> p c t"), sf[:, 0:2, 0:n], sf, reads=[sf])
            P.dma("sp", X["GKT"][:, :, t0:t0 + n].rearrange("c p t -> p c t"), sf[:, 2:4, 0:n], sf, reads=[sf])
            sf = stf.next()
            for s in range(n // 128):
                ps = tok_major(wt, 256, 256, (t0 + s * 128) // 128)
                P.cp("act", sf[:, s, 0:256], ps[:, 0:256], [ps], [sf])
            P.dma("sp", X["GK"][t0:t0 + n, :].rearrange("(s p) c -> p s c", p=128), sf[:, 0:n // 128, 0:256], sf, reads=[sf])
        wt = load_w(OGV, 512)
        for (t0, n) in TILES:
            sq = stq.next()
            for s in range(n // 128):
                ps = tok_major(wt, 0, 512, (t0 + s * 128) // 128)
                P.cp("act", sq[:, s, :], ps[:], [ps], [sq])
            P.dma("sp", X["GV"][t0:t0 + n, :].rearrange("(s p) c -> p s c", p=128), sq[:, 0:n // 128, :], sq, reads=[sq])
        wt = load_w(OGR, 512)
        for (t0, n) in TILES:
            sf = stf.next()
            for s in range(n // 128):
                ps = tok_major(wt, 0, 512, (t0 + s * 128) // 128)
                P.act(sf[:, s, :], ps[:], AF.Silu, [ps], [sf])
            P.dma("sp", X["GR"][t0:t0 + n, :].rearrange("(s p) c -> p s c", p=128), sf[:, 0:n // 128, :], sf, reads=[sf])
        wt = load_w(OGA, 32)
        for (t0, n) in TILES:
            sf = stf.next()
            ps = feat_major(wt, 0, 32, t0, n)
            P.cp("act", sf[0:32, 0, 0:n], ps[0:32, 0:n], [ps], [sf])
            P.dma("sp", X["GAF"][:, t0:t0 + n], sf[0:16, 0, 0:n], sf, reads=[sf])
            P.dma("sp", X["GAB"][:, t0:t0 + n], sf[16:32, 0, 0:n], sf, reads=[sf])
        for gblk in range(6):
            wt = load_w(OGT + gblk * 512, 512)
            for (t0, n) in TILES:
                sq = stq.next()
                for ch in range(4):
                    ps = feat_major(wt, ch * 128, 128, t0, n)
                    P.act(sq[:, ch, 0:n], ps[:, 0:n], AF.Sigmoid, [ps], [sq])
                P.dma("sp", X["SIGT"][gblk * 4:(gblk + 1) * 4, :, t0:t0 + n].rearrange("c p t -> p c t"),
                      sq[:, :, 0:n], sq, reads=[sq])


class V(Dep):
    def __init__(self, ap):
        super().__init__()
        self.ap = ap


def phase3(P, I, X, L, last, lam_init, K):
    nc = P.nc
    with ExitStack() as es:
        ktr = Ring([P.sb(es, [128, T], BF16) for _ in range(2)])
        qtr = Ring([P.sb(es, [128, T], BF16) for _ in range(2)])
        vtr = Ring([P.sb(es, [128, NT, 132], BF16) for _ in range(2)])
        pss = Ring([P.ps(es, [128, 1024]) for _ in range(2)])
        accT = P.ps(es, [128, 1536])
        ptT = Ring([P.ps(es, [128, 2, 128], BF16) for _ in range(1)])
        offs = [0, 160, 320, 512, 672, 832, 1024, 1184]
        accv = [[V(accT[:, offs[m * 4 + s]:offs[m * 4 + s] + 129]) for s in range(4)] for m in range(2)]
        ptr_ = Ring([P.sb(es, [128, 1024], BF16) for _ in range(3)])
        evr = Ring([P.sb(es, [128, 2, 132], F32) for _ in range(3)])
        t1r = Ring([P.sb(es, [128, 128], F32) for _ in range(2)])
        o_r = Ring([P.sb(es, [128, 128], F32) for _ in range(2)])
        jk = P.sb(es, [128, 128], F32)
        obr = Ring([P.sb(es, [128, 128], BF16) for _ in range(2)])
        str_ = Ring([P.sb(es, [128, 4], F32) for _ in range(3)])
        dstr = Ring([P.sb(es, [128, 512], BF16) for _ in range(2)])
        lam = K["lam"]
        qtiles = [(256 + 512 * i, 512, list(range(NT))) for i in range(8)]
        if not last:
            qtiles = [(0, 256, [0, 1])] + qtiles
        if DBG.get("p3_qtiles") is not None:
            qtiles = [qtiles[i] for i in DBG["p3_qtiles"]]
        for h in range(DBG.get("p3_heads", 8)):
            kt, qt, vt = ktr.next(), qtr.next(), vtr.next()
            P.dma("sp", kt[:], X["KT"][h], kt, writes=[kt])
            P.dma("sp", qt[:], X["QT"][h], qt, writes=[qt])
            P.dma("sp", vt[:].rearrange("p t e -> p (t e)"), X["V"][h], vt, writes=[vt])
            for (q0, n, ktl) in qtiles:
                nsub = n // 128
                def qk(kk_):
                    ps_ = pss.next()
                    for m in range(2):
                        P.mm(ps_[:, m * 512:m * 512 + n], kt[m * 64:(m + 1) * 64, kk_ * 128:(kk_ + 1) * 128],
                             qt[m * 64:(m + 1) * 64, q0:q0 + n], True, True, [kt, qt], [ps_])
                    return ps_

                ps_next = qk(ktl[0])
                for ki, kk in enumerate(ktl):
                    ps = ps_next
                    if ki + 1 < len(ktl):
                        ps_next = qk(ktl[ki + 1])
                    pt = ptr_.next()
                    if n == 512:
                        P.act(pt[:], ps[:], AF.Exp, [ps], [pt], scale=0.125)
                    else:
                        for m in range(2):
                            P.act(pt[:, m * 512:m * 512 + n], ps[:, m * 512:m * 512 + n], AF.Exp, [ps], [pt], scale=0.125)
                    if ki == 0:
                        started = set()
                    for m in range(2):
                        for s in range(nsub):
                            av = accv[m][s]
                            bank = offs[m * 4 + s] // 512
                            st_flag = ki == 0 and bank not in started
                            started.add(bank)
                            P.op("pe", lambda: nc.tensor.matmul(av.ap, pt[:, m * 512 + s * 128:m * 512 + (s + 1) * 128],
                                                                vt[:, kk, 0:129], start=st_flag, stop=(ki == len(ktl) - 1),
                                                                skip_group_check=True), [pt, vt], [av], pe_n=129)
                dst = dstr.next()
                for s in range(nsub):
                    ev = evr.next()
                    st = str_.next()
                    for m in range(2):
                        rd = [accv[m][s]] + ([accv[1][nsub - 1]] if DBG.get("h1") else [])
                        P.cp("dve", ev[:, m, 0:129], accv[m][s].ap, rd, [ev])
                    P.op("dve", lambda: nc.vector.reciprocal(st[:, 0:2], ev[:, :, 128]), [ev], [st])
                    P.tt("dve", st[:, 1:2], st[:, 1:2], lam[:, 0:1], ALU.mult, [st, lam], [st])
                    t1 = t1r.next()
                    o = o_r.next()
                    P.ts("pool", t1[:], ev[:, 1, 0:128], st[:, 1:2], None, ALU.mult, None, [ev, st], [t1])
                    P.stt("dve", o[:], ev[:, 0, 0:128], st[:, 0:1], t1[:], ALU.mult, ALU.subtract, [ev, st, t1], [o])
                    P.tt("pool", jk[:], o[:], o[:], ALU.mult, [o], [jk])
                    P.op("dve", lambda: nc.vector.tensor_reduce(st[:, 2:3], jk[:], AX.X, ALU.add), [jk], [st])
                    rstd(P, st, st[:, 2:3], st[:, 2:3], 128, K["epsc"])
                    ob = obr.next()
                    P.stt("dve", ob[:], o[:], st[:, 2:3], K["subg"][:], ALU.mult, ALU.mult, [o, st, K["subg"]], [ob])
                    pT = ptT.next()
                    P.tr(pT[:, 0, :], ob[:], K["ident_b"][:], [ob, K["ident_b"]], [pT])
                    P.cp("dve", dst[:, s * 128:(s + 1) * 128], pT[:, 0, :], [pT], [dst])
                P.dma("sp", X["DIFFT"][h, :, q0:q0 + n], dst[:, 0:n], dst, reads=[dst])


def phase4(P, I, X, L, last, lam_init, K):
    nc = P.nc
    with ExitStack() as es:
        cw = P.sb(es, [128, 4, 3], F32)
        with nc.allow_non_contiguous_dma(reason="tiny conv taps"):
            for k_ in range(3):
                P.dma("sp", cw[:, :, k_], I["conv_w"][L, k_].rearrange("(c p) -> p c", p=128), cw, writes=[cw])
        zero = P.sb(es, [128, 8], F32)
        P.memset("dve", zero, zero[:], 0.0)
        cb = P.sb(es, [128, T], F32)
        c_ = P.sb(es, [128, T], F32)
        cx = P.sb(es, [128, T], F32)
        up = P.sb(es, [128, T], F32)
        un = P.sb(es, [128, T], F32)
        y = P.sb(es, [128, T], F32)
        yb = P.sb(es, [128, T], BF16)
        for cc in range(4):
            P.dma("sp", cb[:], X["CBT"][cc], cb, writes=[cb])
            P.dma("sp", c_[:], X["CCT"][cc], c_, writes=[c_])
            P.dma("sp", cx[:], X["CXT"][cc], cx, writes=[cx])
            P.tt("dve", c_[:], c_[:], cx[:], ALU.mult, [c_, cx], [c_])
            P.dma("sp", up[:, 1:T], c_[:, 0:T - 1], up, reads=[c_], writes=[up])
            P.dma("sp", un[:, 0:T - 1], c_[:, 1:T], un, reads=[c_], writes=[un])
            for col in (0, C):
                P.dma("sp", up[:, col:col + 1], zero[:, 0:1], up, reads=[zero], writes=[up])
            for col in (C - 1, T - 1):
                P.dma("sp", un[:, col:col + 1], zero[:, 0:1], un, reads=[zero], writes=[un])
            P.ts("dve", y[:], c_[:], cw[:, cc, 1:2], None, ALU.mult, None, [c_, cw], [y])
            P.stt("dve", y[:], up[:], cw[:, cc, 0:1], y[:], ALU.mult, ALU.add, [up, cw, y], [y])
            P.stt("dve", y[:], un[:], cw[:, cc, 2:3], y[:], ALU.mult, ALU.add, [un, cw, y], [y])
            P.tt("dve", yb[:], y[:], cb[:], ALU.mult, [y, cb], [yb])
            P.dma("sp", X["YCT"][cc], yb[:], yb, reads=[yb])


def phase5(P, I, X, L, last, lam_init, K):
    nc = P.nc
    NCH = NT
    with ExitStack() as es:
        tri = P.sb(es, [128, 6, 128], F32)
        P.dma("sp", tri[:], I["k_tri"].rearrange("s m l -> m s l"), tri, writes=[tri])
        wa = P.sb(es, [16, 2, 256], F32)
        P.dma("sp", wa[:], I["gla_w_a2"][L].rearrange("d k n -> k d n"), wa, writes=[wa])
        ba = P.sb(es, [128, 512], F32)
        P.dma("sp", ba[:], I["gla_b_a"][L].partition_broadcast(128), ba, writes=[ba])
        neg16 = P.sb(es, [128, 2], F32)
        P.memset("dve", neg16, neg16[:], -1.0 / 16.0)
        Sf = P.sb(es, [128, 2, 128], F32)
        Sfb = P.sb(es, [128, 2, 128], BF16)
        Sb = P.sb(es, [128, 2, 128], F32)
        SLB = P.sb(es, [128, NCH, 2, 128], F32)
        DECB = P.sb(es, [128, NCH, 2], F32)
        SBP = P.sb(es, [128, NCH, 2, 128], BF16)
        for t_ in (Sf, Sb):
            P.memset("dve", t_, t_[:], 0.0)
        P.memset("dve", Sfb, Sfb[:], 0.0)
        psA = P.ps(es, [128, 512])
        psB = P.ps(es, [128, 512])
        psC = P.ps(es, [128, 1024])
        psO = P.ps(es, [128, 512])
        psS = P.ps(es, [128, 512])
        psT = P.ps(es, [128, 4, 128], BF16)
        gqr = Ring([P.sb(es, [128, 2, 512], F32) for _ in range(2)])
        gkr = Ring([P.sb(es, [128, 2, 512], F32) for _ in range(2)])
        gktr = Ring([P.sb(es, [128, 256], F32) for _ in range(2)])
        gvr = Ring([P.sb(es, [128, 512], BF16) for _ in range(2)])
        grr = Ring([P.sb(es, [128, 512], F32) for _ in range(2)])
        gar = [Ring([P.sb(es, [16, 512], F32) for _ in range(2)]) for _ in range(2)]
        zbr = Ring([P.sb(es, [128, 256], F32) for _ in range(2)])
        spr = Ring([P.sb(es, [128, 256], F32) for _ in range(2)])
        e1r = Ring([P.sb(es, [128, 256], F32) for _ in range(2)])
        e2r = Ring([P.sb(es, [128, 256], F32) for _ in range(2)])
        e3r = Ring([P.sb(es, [128, 256], F32) for _ in range(2)])
        decr = Ring([P.sb(es, [128, 2], F32) for _ in range(2)])
        qdr = [Ring([P.sb(es, [128, 2, 128], BF16) for _ in range(2)]) for _ in range(2)]
        kir = [Ring([P.sb(es, [128, 2, 128], BF16) for _ in range(2)]) for _ in range(2)]
        ker = Ring([P.sb(es, [128, 256], BF16) for _ in range(2)])
        atr = [Ring([P.sb(es, [128, 4, 128], BF16) for _ in range(2)]) for _ in range(2)]
        osr = Ring([P.sb(es, [128, 512], F32) for _ in range(2)])
        sqj = P.sb(es, [128, 512], F32)
        onr = Ring([P.sb(es, [128, 512], F32) for _ in range(2)])
        obr = Ring([P.sb(es, [128, 512], BF16) for _ in range(2)])
        ygr = Ring([P.sb(es, [128, 4, 512], BF16) for _ in range(2)])

        def tile_of(c):
            if c < 2:
                return 0, 256, c * 128
            j = (c - 2) // 4
            return 256 + 512 * j, 512, ((c - 2) % 4) * 128
        str_ = Ring([P.sb(es, [128, 8], F32) for _ in range(2)])
        GAsrc = (X["GAF"], X["GAB"])

        def softplus_neg(c, d, ga, off):
            P.mm(psA[:, 0:256], ga[0:16, off:off + 128], wa[0:16, d, :], True, True, [ga, wa], [psA])
            zb = zbr.next()
            P.tt("dve", zb[:], psA[:, 0:256], ba[:, d * 256:(d + 1) * 256], ALU.add, [psA, ba], [zb])
            P.act(zb[:], zb[:], AF.Exp, [zb], [zb], scale=-1.0)
            sp = spr.next()
            P.act(sp[:], zb[:], AF.Ln, [zb], [sp], bias=1.0)
            return sp

        def tot_dec(sp, dec_ap, dec_t):
            for hp in range(2):
                P.mm(psA[:, 256 + hp:257 + hp], sp[:, hp * 128:(hp + 1) * 128], neg16[:, 0:1], True, True, [sp, neg16], [psA])
            P.act(dec_ap, psA[:, 256:258], AF.Exp, [psA], [dec_t])

        def ke_of(sp, d, gkt):
            P.mm(psB[:, 256:512], tri[:, 2 + d, :], sp[:], True, True, [tri, sp], [psB])
            e3 = e3r.next()
            P.act(e3[:], psB[:, 256:512], AF.Exp, [psB], [e3])
            ke = ker.next()
            P.tt("pool", ke[:], gkt[:], e3[:], ALU.mult, [gkt, e3], [ke])
            return ke

        def sloc(ke, gv):
            for hp in range(2):
                P.mm(psS[:, hp * 256:(hp + 1) * 256], ke[:, hp * 128:(hp + 1) * 128], gv[:, hp * 256:(hp + 1) * 256],
                     True, True, [ke, gv], [psS])

        for c in range(NCH):
            gkt, gv = gktr.next(), gvr.next()
            t0_, tn_, off_ = tile_of(c)
            if off_ == 0:
                gab_t = gar[1].next()
                P.dma("sp", gab_t[:, 0:tn_], X["GAB"][:, t0_:t0_ + tn_], gab_t, writes=[gab_t])
            P.dma("sp", gkt[:], X["GK"][c * 128:(c + 1) * 128, :], gkt, writes=[gkt])
            P.dma("sp", gv[:], X["GV"][c * 128:(c + 1) * 128, :], gv, writes=[gv])
            stg = DBG.get("p5_stage", 99)
            sp = softplus_neg(c, 1, gab_t, off_)
            if stg < 2:
                continue
            tot_dec(sp, DECB[:, c, :], DECB)
            if stg < 3:
                continue
            ke = ke_of(sp, 1, gkt)
            if stg < 4:
                continue
            sloc(ke, gv)
            for hp in range(2):
                for hh in range(2):
                    P.cp("act", SLB[hh * 64:(hh + 1) * 64, c, hp, :],
                         psS[hh * 64:(hh + 1) * 64, hp * 256 + hh * 128:hp * 256 + (hh + 1) * 128], [psS], [SLB])
        if DBG.get("p5_stage", 99) < 5:
            return
        for c in [1, 0] + list(range(NCH - 1, 1, -1)):
            P.cp("pool", SBP[:, c, :, :], Sb[:], [Sb], [SBP])
            for hp in range(2):
                P.stt("dve", Sb[:, hp, :], Sb[:, hp, :], DECB[:, c, hp:hp + 1], SLB[:, c, hp, :], ALU.mult, ALU.add,
                      [Sb, DECB, SLB], [Sb])
        if DBG.get("p5_stage", 99) < 6:
            return
        stg = DBG.get("p5_stage", 99)
        for c in range(NCH):
            gkt, gv, gr = gktr.next(), gvr.next(), grr.next()
            cs = slice(c * 128, (c + 1) * 128)
            t0_, tn_, off_ = tile_of(c)
            osl = slice(off_, off_ + 128)
            if off_ == 0:
                gq_t, gk_t, yg = gqr.next(), gkr.next(), ygr.next()
                ga = [gar[0].next(), gar[1].next()]
                ts_ = slice(t0_, t0_ + tn_)
                P.dma("sp", gq_t[:, :, 0:tn_], X["GQT"][:, :, ts_].rearrange("c p t -> p c t"), gq_t, writes=[gq_t])
                P.dma("sp", gk_t[:, :, 0:tn_], X["GKT"][:, :, ts_].rearrange("c p t -> p c t"), gk_t, writes=[gk_t])
                for d in range(2):
                    P.dma("sp", ga[d][:, 0:tn_], GAsrc[d][:, ts_], ga[d], writes=[ga[d]])
            P.dma("sp", gkt[:], X["GK"][cs, :], gkt, writes=[gkt])
            P.dma("sp", gv[:], X["GV"][cs, :], gv, writes=[gv])
            P.dma("sp", gr[:], X["GR"][cs, :], gr, writes=[gr])
            qd, ki, atm = [None, None], [None, None], [None, None]
            dec = decr.next()
            ke = None
            for d in range(2):
                sp = softplus_neg(c, d, ga[d], off_)
                for hp in range(2):
                    P.mm(psB[:, hp * 128:(hp + 1) * 128], sp[:, hp * 128:(hp + 1) * 128], tri[:, d, :], True, True,
                         [sp, tri], [psB])
                e1, e2 = e1r.next(), e2r.next()
                P.act(e1[:], psB[:, 0:256], AF.Exp, [psB], [e1])
                P.act(e2[:], psB[:, 0:256], AF.Exp, [psB], [e2], scale=-1.0)
                qd[d], ki[d] = qdr[d].next(), kir[d].next()
                P.stt("dve", qd[d][:], gq_t[:, :, osl], 0.125, e1[:].rearrange("p (a b) -> p a b", a=2),
                      ALU.mult, ALU.mult, [gq_t, e1], [qd[d]])
                P.tt("pool", ki[d][:], gk_t[:, :, osl], e2[:].rearrange("p (a b) -> p a b", a=2), ALU.mult,
                     [gk_t, e2], [ki[d]])
                if stg < 7:
                    continue
                if d == 0:
                    tot_dec(sp, dec[:], dec)
                    ke = ke_of(sp, 0, gkt)
                if stg < 7.3:
                    continue
                for h in range(4):
                    hp, b0 = h // 2, (h % 2) * 64
                    co = (h % 2) * 512 + (d * 2 + hp) * 128
                    P.mm(psC[:, co:co + 128], ki[d][b0:b0 + 64, hp, :], qd[d][b0:b0 + 64, hp, :],
                         True, True, [ki[d], qd[d]], [psC])
                if stg < 7.6:
                    continue
                atm[d] = atr[d].next()
                P.tt("dve", atm[d][:].rearrange("p (hp par) l -> p hp par l", par=2),
                     psC[:].rearrange("p (par d hp l) -> p d hp par l", par=2, d=2, hp=2)[:, d],
                     tri[:, 4 + d, :].unsqueeze(1).unsqueeze(1).broadcast_to([128, 2, 2, 128]), ALU.mult, [psC, tri], [atm[d]])
            if stg < 8:
                continue
            for h in range(4):
                hp, b0 = h // 2, (h % 2) * 64
                oo = psO[:, h * 128:(h + 1) * 128]
                vv = gv[:, h * 128:(h + 1) * 128]
                P.mm(oo, atm[0][:, h, :], vv, True, False, [atm[0], gv], [psO])
                P.mm(oo, qd[0][b0:b0 + 64, hp, :], Sfb[b0:b0 + 64, hp, :], False, False, [qd[0], Sfb], [psO])
                P.mm(oo, atm[1][:, h, :], vv, False, False, [atm[1], gv], [psO])
                P.mm(oo, qd[1][b0:b0 + 64, hp, :], SBP[b0:b0 + 64, c, hp, :], False, True, [qd[1], SBP], [psO])
            sloc(ke, gv)
            for hp in range(2):
                for hh in range(2):
                    rs = slice(hh * 64, (hh + 1) * 64)
                    P.stt("dve", Sf[rs, hp, :], Sf[rs, hp, :], dec[rs, hp:hp + 1],
                          psS[rs, hp * 256 + hh * 128:hp * 256 + (hh + 1) * 128], ALU.mult, ALU.add, [Sf, dec, psS], [Sf])
            P.cp("pool", Sfb[:], Sf[:], [Sf], [Sfb])
            if stg < 9:
                continue
            osb, on, ob, st = osr.next(), onr.next(), obr.next(), str_.next()
            P.cp("act", osb[:], psO[:], [psO], [osb])
            P.tt("pool", sqj[:], osb[:], osb[:], ALU.mult, [osb], [sqj])
            P.op("dve", lambda: nc.vector.tensor_reduce(st[:, 0:4], sqj[:].rearrange("p (h e) -> p h e", h=4), AX.X, ALU.add),
                 [sqj], [st])
            rstd(P, st, st[:, 0:4], st[:, 0:4], 128, K["epsc"])
            o3 = osb[:].rearrange("p (h e) -> p h e", h=4)
            n3 = on[:].rearrange("p (h e) -> p h e", h=4)
            P.tt("dve", n3, o3, st[:, 0:4].unsqueeze(2).broadcast_to([128, 4, 128]), ALU.mult, [osb, st], [on])
            P.tt("pool", n3, n3, K["glag"][:].unsqueeze(1).broadcast_to([128, 4, 128]), ALU.mult, [on, K["glag"]], [on])
            P.tt("dve", ob[:], on[:], gr[:], ALU.mult, [on, gr], [ob])
            for h in range(4):
                P.tr(psT[:, h, :], ob[:, h * 128:(h + 1) * 128], K["ident_b"][:], [ob, K["ident_b"]], [psT])
            P.cp("act", yg[:, :, osl], psT[:], [psT], [yg])
            if off_ + 128 == tn_:
                P.dma("sp", X["YGT"][:, :, t0_:t0_ + tn_].rearrange("c p t -> p c t"), yg[:, :, 0:tn_], yg, reads=[yg])


def phase6(P, I, X, L, last, lam_init, K):
    nc = P.nc
    with ExitStack() as es:
        WD = P.sb(es, [128, 8, D], BF16)
        WC = P.sb(es, [128, 4, D], BF16)
        WG = P.sb(es, [128, 4, D], BF16)
        WO = P.sb(es, [128, 8, D], BF16)
        for wt, nm in ((WD, "diff_w_out"), (WC, "conv_w_out"), (WG, "gla_w_out"), (WO, "w_o")):
            P.dma("pool", wt[:], I[nm][L].rearrange("(kc p) n -> p kc n", p=128), wt, writes=[wt])
        RW = P.sb(es, [128, 8, NE], F32)
        P.dma("sp", RW[:], I["router_w"][L].rearrange("(kc p) e -> p kc e", p=128), RW, writes=[RW])
        RB = P.sb(es, [128, NE], F32)
        P.dma("sp", RB[:], I["router_b"][L].partition_broadcast(128), RB, writes=[RB])
        nw = 1 if last else 2
        msl = [[P.sb(es, [128, D], F32) for _ in range(3)] for _ in range(nw)]
        for w in range(nw):
            for j, seg in enumerate((2, 3, 4)):
                P.dma("sp", msl[w][j][:], X["MODS"][w][:, seg * D:(seg + 1) * D], msl[w][j], writes=[msl[w][j]])
        dTr = Ring([P.sb(es, [128, 8, 512], BF16) for _ in range(2)])
        ycr = Ring([P.sb(es, [128, 4, 512], BF16) for _ in range(2)])
        ygr = Ring([P.sb(es, [128, 4, 512], BF16) for _ in range(2)])
        sgr = Ring([P.sb(es, [128, 3, 512], BF16) for _ in range(3)])
        mgr = Ring([P.sb(es, [128, 8, 512], BF16) for _ in range(1)])
        m1r = Ring([P.sb(es, [128, 512], F32) for _ in range(2)])
        m2r = Ring([P.sb(es, [128, 512], F32) for _ in range(2)])
        m3r = Ring([P.sb(es, [128, 512], F32) for _ in range(2)])
        xr = Ring([P.sb(es, [128, D], F32) for _ in range(2)])
        xnr = Ring([P.sb(es, [128, D], F32) for _ in range(1)])
        tmr = Ring([P.sb(es, [128, 512], F32) for _ in range(2)])
        hbr = Ring([P.sb(es, [128, D], F32) for _ in range(1)])
        hbbr = Ring([P.sb(es, [128, D], BF16) for _ in range(2)])
        scr_b = P.sb(es, [128, D], F32)
        str_ = Ring([P.sb(es, [128, 2], F32) for _ in range(2)])
        h32r = Ring([P.sb(es, [128, 8, 128], F32) for _ in range(1)])
        h2st = Ring([P.sb(es, [128, 8, 512], BF16) for _ in range(1)])
        gtst = Ring([P.sb(es, [128, 4, NE], F32) for _ in range(2)])
        lgr = Ring([P.sb(es, [128, NE], F32) for _ in range(2)])
        er = Ring([P.sb(es, [128, NE], F32) for _ in range(2)])
        mkr = Ring([P.sb(es, [128, NE], F32) for _ in range(2)])
        m8r = Ring([P.sb(es, [128, 16], F32) for _ in range(2)])
        psbr = Ring([P.ps(es, [128, 512]) for _ in range(3)])
        psor = Ring([P.ps(es, [128, 512]) for _ in range(2)])
        ptr = P.ps(es, [128, 8, 128], F32)
        pslg = P.ps(es, [128, 512])
        sig4 = X["SIGT"].rearrange("(b c) p t -> c p b t", b=3)
        tiles = TILES[1:] if last else TILES
        for (t0, n) in tiles:
            w = 1 if t0 == 0 else 0
            g1, sh2, G2 = msl[w]
            dT, yc, yg = dTr.next(), ycr.next(), ygr.next()
            P.dma("sp", dT[:, :, 0:n], X["DIFFT"][:, :, t0:t0 + n].rearrange("c p t -> p c t"), dT, writes=[dT])
            P.dma("sp", yc[:, :, 0:n], X["YCT"][:, :, t0:t0 + n].rearrange("c p t -> p c t"), yc, writes=[yc])
            P.dma("sp", yg[:, :, 0:n], X["YGT"][:, :, t0:t0 + n].rearrange("c p t -> p c t"), yg, writes=[yg])
            mg = mgr.next()
            for c in range(8):
                sg = sgr.next()
                P.dma("sp", sg[:, :, 0:n], sig4[c][:, :, t0:t0 + n], sg, writes=[sg])
                cs = slice(c * 128, (c + 1) * 128)
                pd, pc, pg = psbr.next(), psbr.next(), psbr.next()
                for kc in range(8):
                    P.mm(pd[:, 0:n], WD[:, kc, cs], dT[:, kc, 0:n], kc == 0, kc == 7, [WD, dT], [pd])
                for kc in range(4):
                    P.mm(pc[:, 0:n], WC[:, kc, cs], yc[:, kc, 0:n], kc == 0, kc == 3, [WC, yc], [pc])
                for kc in range(4):
                    P.mm(pg[:, 0:n], WG[:, kc, cs], yg[:, kc, 0:n], kc == 0, kc == 3, [WG, yg], [pg])
                m1, m2, m3 = m1r.next(), m2r.next(), m3r.next()
                P.tt("dve", m1[:, 0:n], pd[:, 0:n], sg[:, 0, 0:n], ALU.mult, [pd, sg], [m1])
                P.tt("dve", m2[:, 0:n], pc[:, 0:n], sg[:, 1, 0:n], ALU.mult, [pc, sg], [m2])
                P.tt("dve", m3[:, 0:n], pg[:, 0:n], sg[:, 2, 0:n], ALU.mult, [pg, sg], [m3])
                P.tt("pool", m1[:, 0:n], m1[:, 0:n], m2[:, 0:n], ALU.add, [m1, m2], [m1])
                P.tt("pool", mg[:, c, 0:n], m1[:, 0:n], m3[:, 0:n], ALU.add, [m1, m3], [mg])
            h2s = h2st.next()
            gts = gtst.next()
            stg6 = DBG.get("p6_stage", 99)
            if stg6 < 2:
                continue
            for s in range(n // 128):
                r0 = t0 + s * 128
                xt, xn = xr.next(), xnr.next()
                P.dma("sp", xt[:], xsrc(I, X, L, r0), xt, writes=[xt])
                for hf in range(2):
                    po = psor.next()
                    hs = slice(hf * 512, (hf + 1) * 512)
                    for kc in range(8):
                        P.mm(po[:], mg[:, kc, s * 128:(s + 1) * 128], WO[:, kc, hs], kc == 0, kc == 7, [mg, WO], [po])
                    tm = tmr.next()
                    P.tt("dve", tm[:], po[:], g1[:, hs], ALU.mult, [po, g1], [tm])
                    P.tt("pool", xt[:, hs], xt[:, hs], tm[:], ALU.add, [xt, tm], [xt])
                P.dma("sp", X["XR"][r0:r0 + 128, :], xt[:], xt, reads=[xt])
                if stg6 < 3:
                    continue
                hb, st = hbr.next(), str_.next()
                norm_mod(P, xt, G2, sh2, hb, scr_b, st, K["epsc"], xo=xn)
                hbb = hbbr.next()
                P.cp("act", hbb[:], hb[:], [hb], [hbb])
                P.dma("sp", X["H2M"][r0:r0 + 128, :], hbb[:], hbb, reads=[hbb])
                if stg6 < 3.3:
                    continue
                for kc in range(8):
                    P.mm(ptr[:, kc, :], hb[:, kc * 128:(kc + 1) * 128], K["ident_f"][:], True, True, [hb, K["ident_f"]], [ptr])
                if stg6 < 3.6:
                    continue
                h32 = h32r.next()
                P.cp("act", h32[:], ptr[:], [ptr], [h32])
                if stg6 < 3.8:
                    continue
                P.cp("act", h2s[:, :, s * 128:(s + 1) * 128], ptr[:], [ptr], [h2s])
                if stg6 < 4:
                    continue
                for kc in range(8):
                    P.mm(pslg[:, 0:NE], h32[:, kc, :], RW[:, kc, :], kc == 0, kc == 7, [h32, RW], [pslg])
                lg, e_, mk, m8 = lgr.next(), er.next(), mkr.next(), m8r.next()
                P.tt("dve", lg[:], pslg[:, 0:NE], RB[:], ALU.add, [pslg, RB], [lg])
                P.op("dve", lambda: nc.vector.max(m8[:, 0:8], lg[:]), [lg], [m8])
                P.ts("dve", m8[:, 8:9], m8[:, 0:1], -1.0, None, ALU.mult, None, [m8], [m8])
                P.ts("dve", mk[:], lg[:], m8[:, 3:4], None, ALU.is_ge, None, [lg, m8], [mk])
                P.act(e_[:], lg[:], AF.Exp, [lg, m8], [e_], bias=m8[:, 8:9])
                P.tt("dve", e_[:], e_[:], mk[:], ALU.mult, [e_, mk], [e_])
                P.op("dve", lambda: nc.vector.tensor_reduce(m8[:, 9:10], e_[:], AX.X, ALU.add), [e_], [m8])
                P.op("dve", lambda: nc.vector.reciprocal(m8[:, 10:11], m8[:, 9:10]), [m8], [m8])
                P.ts("dve", gts[:, s, :], e_[:], m8[:, 10:11], None, ALU.mult, None, [e_, m8], [gts])
            if stg6 < 4:
                continue
            P.dma("sp", X["H2T"][:, :, t0:t0 + n].rearrange("c p t -> p c t"), h2s[:, :, 0:n], h2s, reads=[h2s])
            P.dma("sp", X["GATES"][t0:t0 + n, :].rearrange("(s p) e -> p s e", p=128), gts[:, 0:n // 128, :], gts, reads=[gts])


def phase7(P, I, X, L, last, lam_init, K, yout):
    nc = P.nc
    with ExitStack() as es:
        nw = 1 if last else 2
        g2 = [P.sb(es, [128, D], F32) for _ in range(nw)]
        for w in range(nw):
            P.dma("sp", g2[w][:], X["MODS"][w][:, 5 * D:6 * D], g2[w], writes=[g2[w]])
        B1T = P.sb(es, [128, NE, 2, 8], F32)
        with nc.allow_non_contiguous_dma(reason="bias de-interleave"):
            for e in range(NE):
                for s_ in range(2):
                    P.dma("sp", B1T[:, e, s_, :], I["moe_b1"][L, e].rearrange("(j p s) -> s p j", p=128, s=2)[s_], B1T, writes=[B1T])
        B2 = P.sb(es, [NE, D], F32)
        P.dma("sp", B2[:], I["moe_b2"][L], B2, writes=[B2])
        FG = None
        if last:
            FG = P.sb(es, [128, D], F32)
            P.dma("sp", FG[:], I["final_norm_g"].partition_broadcast(128), FG, writes=[FG])
        acc = P.sb(es, [128, 8, D], F32)
        h2 = Ring([P.sb(es, [128, 8, 1024], BF16) for _ in range(1)])
        gtr = Ring([P.sb(es, [128, 8, NE], F32) for _ in range(1)])
        gT = Ring([P.sb(es, [NE, 128], F32) for _ in range(2)])
        w1r = Ring([P.sb(es, [128, 8, 512], BF16) for _ in range(5)])
        w2r = Ring([P.sb(es, [128, 8, 512], BF16) for _ in range(4)])
        actr = Ring([P.sb(es, [128, 8, 512], BF16) for _ in range(2)])
        B1S = P.sb(es, [128, NE, 8], F32)
        P.ts("dve", B1S[:], B1T[:, :, 0, :], 1.702, None, ALU.mult, None, [B1T], [B1S])
        P.ts("dve", B1T[:, :, 1, :], B1T[:, :, 1, :], 1.0, None, ALU.add, None, [B1T], [B1T])
        glr = Ring([P.sb(es, [128, 512], F32) for _ in range(2)])
        sgr = Ring([P.sb(es, [128, 512], F32) for _ in range(2)])
        lnr = Ring([P.sb(es, [128, 512], F32) for _ in range(2)])
        xr = Ring([P.sb(es, [128, D], F32) for _ in range(2)])
        scr_b = P.sb(es, [128, D], F32)
        str_ = Ring([P.sb(es, [128, 2], F32) for _ in range(2)])
        psg = Ring([P.ps(es, [128, 512]) for _ in range(2)])
        psl = Ring([P.ps(es, [128, 512]) for _ in range(2)])
        pso = Ring([P.ps(es, [128, 512]) for _ in range(3)])
        pst = P.ps(es, [128, 512])
        if last:
            tiles = [(C + 1024 * i, 1024) for i in range(4)]
        else:
            tiles = [(0, C)] + [(C + 1024 * i, 1024) for i in range(4)]
        w1v = I["moe_w1"][L].rearrange("e (kc p) n -> e p kc n", p=128)
        w2v = I["moe_w2"][L].rearrange("e (kc p) n -> e p kc n", p=128)
        for (t0, n) in tiles:
            w = 1 if t0 == 0 else 0
            nsub = n // 128
            h2t, gt = h2.next(), gtr.next()
            P.dma("sp", h2t[:, :, 0:n], X["H2T"][:, :, t0:t0 + n].rearrange("c p t -> p c t"), h2t, writes=[h2t])
            P.dma("sp", gt[:, 0:nsub, :], X["GATES"][t0:t0 + n, :].rearrange("(s p) e -> p s e", p=128), gt, writes=[gt])
            for s in range(nsub):
                P.mm(pst[0:NE, 0:128], gt[:, s, :], K["ident_f"][:], True, True, [gt, K["ident_f"]], [pst])
                g_t = gT.next()
                P.cp("act", g_t[:], pst[0:NE, 0:128], [pst], [g_t])
                for hf in range(2):
                    po = pso.next()
                    P.mm(po[:], g_t[:], B2[:, hf * 512:(hf + 1) * 512], True, True, [g_t, B2], [po])
                    P.cp("act", acc[:, s, hf * 512:(hf + 1) * 512], po[:], [po], [acc])
            W1, W2 = {}, {}

            def load_w1(e, q):
                t_ = w1r.next()
                P.dma("pool", t_[:], w1v[e][:, :, q * 512:(q + 1) * 512], t_, writes=[t_])
                W1[(e, q)] = t_

            def load_w2(e, hf):
                t_ = w2r.next()
                P.dma("pool", t_[:], w2v[e][:, :, hf * 512:(hf + 1) * 512], t_, writes=[t_])
                W2[(e, hf)] = t_

            for q in range(4):
                load_w1(0, q)
            for hf in range(2):
                load_w2(0, hf)
            subtiles = [(j0, min(512, n - j0)) for j0 in range(0, n, 512)]
            pending = [None]
            NEX = DBG.get("p7_experts", NE)
            SIGCAP = 1.0 / (1.0 + math.exp(-1.702 * 7.0))
            for e in range(NEX):
                for si, (j0, nj) in enumerate(subtiles):
                    pre = si == len(subtiles) - 1 and e + 1 < NEX
                    at = actr.next()

                    def mm1(ci):
                        q, gch = ci // 2, ci % 2
                        w1s = W1[(e, q)]
                        pg, pl = psg.next(), psl.next()
                        for sidx, pp in ((0, pg), (1, pl)):
                            for kc in range(8):
                                P.mm(pp[:, 0:nj], w1s[:, kc, gch * 256 + sidx:gch * 256 + 256:2],
                                     h2t[:, kc, j0:j0 + nj], kc == 0, kc == 7, [w1s, h2t], [pp])
                        if pre and gch == 1:
                            load_w1(e + 1, q)
                        return pg, pl

                    def post1(ci, pg, pl):
                        fc = ci
                        gl, sg, ln = glr.next(), sgr.next(), lnr.next()
                        if DBG.get("p7_old_swiglu"):
                            P.ts("dve", gl[:, 0:nj], pg[:, 0:nj], B1T[:, e, 0, fc:fc + 1], 7.0, ALU.add, ALU.min, [pg, B1T], [gl])
                            P.act(sg[:, 0:nj], gl[:, 0:nj], AF.Sigmoid, [gl], [sg], scale=1.702)
                            P.ts("dve", ln[:, 0:nj], pl[:, 0:nj], B1T[:, e, 1, fc:fc + 1], 8.0, ALU.add, ALU.min, [pl, B1T], [ln])
                            P.ts("dve", ln[:, 0:nj], ln[:, 0:nj], -6.0, None, ALU.max, None, [ln], [ln])
                            P.tt("dve", gl[:, 0:nj], gl[:, 0:nj], sg[:, 0:nj], ALU.mult, [gl, sg], [gl])
                            P.tt("dve", at[:, fc, 0:nj], gl[:, 0:nj], ln[:, 0:nj], ALU.mult, [gl, ln], [at])
                            return
                        P.ts("dve", gl[:, 0:nj], pg[:, 0:nj], B1T[:, e, 0, fc:fc + 1], 7.0, ALU.add, ALU.min, [pg, B1T], [gl])
                        P.act(sg[:, 0:nj], gl[:, 0:nj], AF.Sigmoid, [gl], [sg], scale=1.702)
                        P.ts("dve", ln[:, 0:nj], pl[:, 0:nj], B1T[:, e, 1, fc:fc + 1], 8.0, ALU.add, ALU.min, [pl, B1T], [ln])
                        if DBG.get("p7_nofuse"):
                            P.ts("dve", sg[:, 0:nj], sg[:, 0:nj], SIGCAP, None, ALU.min, None, [sg], [sg])
                            P.tt("dve", gl[:, 0:nj], sg[:, 0:nj], gl[:, 0:nj], ALU.mult, [sg, gl], [gl])
                            P.ts("dve", ln[:, 0:nj], ln[:, 0:nj], -6.0, None, ALU.max, None, [ln], [ln])
                            P.tt("dve", at[:, fc, 0:nj], ln[:, 0:nj], gl[:, 0:nj], ALU.mult, [ln, gl], [at])
                            return
                        P.stt("dve", gl[:, 0:nj], sg[:, 0:nj], SIGCAP, gl[:, 0:nj], ALU.min, ALU.mult, [sg, gl], [gl])
                        P.stt("dve", at[:, fc, 0:nj], ln[:, 0:nj], -6.0, gl[:, 0:nj], ALU.max, ALU.mult, [ln, gl], [at])

                    def make_ffn2(e_, j0_, nj_, at_):
                        def run():
                            groups = [(s_, hf_) for s_ in range(nj_ // 128) for hf_ in range(2)]

                            def mm2(g):
                                s_, hf_ = groups[g]
                                po = pso.next()
                                w2s = W2[(e_, hf_)]
                                for fc in range(8):
                                    P.mm(po[:], at_[:, fc, s_ * 128:(s_ + 1) * 128], w2s[:, fc, :], fc == 0, fc == 7, [at_, w2s], [po])
                                return po

                            def post2(g, po):
                                s_, hf_ = groups[g]
                                sidx = j0_ // 128 + s_
                                asl = acc[:, sidx, hf_ * 512:(hf_ + 1) * 512]
                                P.stt("dve", asl, po[:], gt[:, sidx, e_:e_ + 1], asl, ALU.mult, ALU.add, [po, gt, acc], [acc])

                            prev = None
                            for g in range(len(groups)):
                                cur = mm2(g)
                                if prev is not None:
                                    post2(g - 1, prev)
                                prev = cur
                            post2(len(groups) - 1, prev)
                        return run

                    prev = None
                    for ci in range(8):
                        cur = mm1(ci)
                        if prev is not None:
                            post1(ci - 1, *prev)
                        prev = cur
                        if ci == 1 and pending[0] is not None:
                            pending[0]()
                            pending[0] = None
                    post1(7, *prev)
                    if pre:
                        load_w2(e + 1, 0)
                        load_w2(e + 1, 1)
                    pending[0] = make_ffn2(e, j0, nj, at)
            if pending[0] is not None:
                pending[0]()
                pending[0] = None
            for s in range(nsub):
                r0 = t0 + s * 128
                xt = xr.next()
                P.dma("sp", xt[:], X["XR"][r0:r0 + 128, :], xt, writes=[xt])
                P.tt("dve", acc[:, s, :], acc[:, s, :], g2[w][:], ALU.mult, [acc, g2[w]], [acc])
                P.tt("pool", xt[:], xt[:], acc[:, s, :], ALU.add, [xt, acc], [xt])
                if not last:
                    P.dma("sp", X["XR"][r0:r0 + 128, :], xt[:], xt, reads=[xt])
                else:
                    st = str_.next()
                    sumsq(P, scr_b, xt, st)
                    rstd(P, st, st[:, 1:2], st[:, 0:1], D, K["epsc"])
                    P.stt("dve", xt[:], xt[:], st[:, 1:2], FG[:], ALU.mult, ALU.mult, [xt, st, FG], [xt])
                    P.dma("sp", yout[r0 - C:r0 - C + 128, :], xt[:], xt, reads=[xt])


def phase7s(P, I, X, L, last, lam_init, K, yout):
    nc = P.nc
    CAP = MOE_CAP
    with ExitStack() as es:
        nw = 1 if last else 2
        g2 = [P.sb(es, [128, D], F32) for _ in range(nw)]
        for w in range(nw):
            P.dma("sp", g2[w][:], X["MODS"][w][:, 5 * D:6 * D], g2[w], writes=[g2[w]])
        B1T = P.sb(es, [128, NE, 2, 8], F32)
        with nc.allow_non_contiguous_dma(reason="bias de-interleave"):
            for e in range(NE):
                for s_ in range(2):
                    P.dma("sp", B1T[:, e, s_, :], I["moe_b1"][L, e].rearrange("(j p s) -> s p j", p=128, s=2)[s_], B1T, writes=[B1T])
        B2 = P.sb(es, [NE, D], F32)
        P.dma("sp", B2[:], I["moe_b2"][L], B2, writes=[B2])
        FG = None
        if last:
            FG = P.sb(es, [128, D], F32)
            P.dma("sp", FG[:], I["final_norm_g"].partition_broadcast(128), FG, writes=[FG])
        trif = P.sb(es, [128, 128], F32)
        P.dma("sp", trif[:], I["k_tri"][3], trif, writes=[trif])
        trib = P.sb(es, [128, 128], BF16)
        P.ts("dve", trib[:], trif[:], -16.0, None, ALU.mult, None, [trif], [trib])
        oneb = P.sb(es, [128, 128], BF16)
        P.memset("dve", oneb, oneb[:], 1.0)
        iot = P.sb(es, [128, CAP], F32)
        P.dma("sp", iot[:], I["k_iota"].partition_broadcast(128), iot, writes=[iot])

        acc = P.sb(es, [128, 8, D], F32)
        h2m = P.sb(es, [128, 8, D], BF16)
        gt = P.sb(es, [128, 8, NE], F32)
        mkf = P.sb(es, [128, 8, NE], F32)
        mkb = P.sb(es, [128, 8, NE], BF16)
        pos = P.sb(es, [128, 8, NE], F32)
        gT = Ring([P.sb(es, [NE, 128], F32) for _ in range(2)])
        selr = Ring([P.sb(es, [128, 8, CAP], BF16) for _ in range(2)])
        selgr = Ring([P.sb(es, [128, 8, CAP], BF16) for _ in range(2)])
        xer = Ring([P.sb(es, [128, 8, CAP], BF16) for _ in range(1)])
        atr_ = Ring([P.sb(es, [128, 8, CAP], BF16) for _ in range(1)])
        yer = Ring([P.sb(es, [128, 2, D], BF16) for _ in range(1)])
        sgtr = Ring([P.sb(es, [128, 2, 8, 128], BF16) for _ in range(1)])
        w1r = Ring([P.sb(es, [128, 8, 512], BF16) for _ in range(5)])
        w2r = Ring([P.sb(es, [128, 8, 512], BF16) for _ in range(4)])
        glr = Ring([P.sb(es, [128, CAP], F32) for _ in range(2)])
        sgr = Ring([P.sb(es, [128, CAP], F32) for _ in range(2)])
        lnr = Ring([P.sb(es, [128, CAP], F32) for _ in range(2)])
        xr = Ring([P.sb(es, [128, D], F32) for _ in range(1)])
        scr_b = P.sb(es, [128, D], F32)
        str_ = Ring([P.sb(es, [128, 2], F32) for _ in range(2)])
        psa = Ring([P.ps(es, [128, 512]) for _ in range(3)])
        psb = Ring([P.ps(es, [128, 512]) for _ in range(3)])
        pstrr = Ring([P.ps(es, [128, 4, 128], BF16) for _ in range(2)])
        if last:
            tiles = [(C + 1024 * i, 1024) for i in range(4)]
        else:
            tiles = [(0, C)] + [(C + 1024 * i, 1024) for i in range(4)]
        w1v = I["moe_w1"][L].rearrange("e (kc p) n -> e p kc n", p=128)
        w2v = I["moe_w2"][L].rearrange("e (kc p) n -> e p kc n", p=128)
        for (t0, n) in tiles:
            w = 1 if t0 == 0 else 0
            nsub = n // 128
            P.dma("sp", h2m[:, 0:nsub, :], X["H2M"][t0:t0 + n, :].rearrange("(s p) d -> p s d", p=128), h2m, writes=[h2m])
            P.dma("sp", gt[:, 0:nsub, :], X["GATES"][t0:t0 + n, :].rearrange("(s p) e -> p s e", p=128), gt, writes=[gt])
            P.ts("dve", mkf[:, 0:nsub, :], gt[:, 0:nsub, :], 0.0, None, ALU.is_gt, None, [gt], [mkf])
            P.cp("dve", mkb[:, 0:nsub, :], mkf[:, 0:nsub, :], [mkf], [mkb])
            for s in range(nsub):
                pp = psb.next()
                for s2 in range(s):
                    P.mm(pp[:, 0:NE], oneb[:], mkb[:, s2, :], s2 == 0, False, [oneb, mkb], [pp])
                P.mm(pp[:, 0:NE], trib[:], mkb[:, s, :], s == 0, True, [trib, mkb], [pp])
                P.cp("act", pos[:, s, :], pp[:, 0:NE], [pp], [pos])
            for s in range(nsub):
                pq = psb.next()
                P.mm(pq[0:NE, 0:128], gt[:, s, :], K["ident_f"][:], True, True, [gt, K["ident_f"]], [pq])
                g_t = gT.next()
                P.cp("act", g_t[:], pq[0:NE, 0:128], [pq], [g_t])
                for hf in range(2):
                    po = psb.next()
                    P.mm(po[:], g_t[:], B2[:, hf * 512:(hf + 1) * 512], True, True, [g_t, B2], [po])
                    P.cp("act", acc[:, s, hf * 512:(hf + 1) * 512], po[:], [po], [acc])
            W1, W2 = {}, {}

            def load_w1(e, q):
                t_ = w1r.next()
                P.dma("pool", t_[:], w1v[e][:, :, q * 512:(q + 1) * 512], t_, writes=[t_])
                W1[(e, q)] = t_

            def load_w2(e, hf):
                t_ = w2r.next()
                P.dma("pool", t_[:], w2v[e][:, :, hf * 512:(hf + 1) * 512], t_, writes=[t_])
                W2[(e, hf)] = t_

            for q in range(4):
                load_w1(0, q)
            for hf in range(2):
                load_w2(0, hf)
            for e in range(NE):
                pre = e + 1 < NE
                sel, selg = selr.next(), selgr.next()
                for s in range(nsub):
                    P.ts("dve", sel[:, s, :], iot[:], pos[:, s, e:e + 1], mkf[:, s, e:e + 1], ALU.is_equal, ALU.mult,
                         [iot, pos, mkf], [sel])
                    P.ts("dve", selg[:, s, :], iot[:], pos[:, s, e:e + 1], gt[:, s, e:e + 1], ALU.is_equal, ALU.mult,
                         [iot, pos, gt], [selg])
                xe = xer.next()
                for dc in range(8):
                    pg = psa.next()
                    for s in range(nsub):
                        P.mm(pg[:, 0:CAP], h2m[:, s, dc * 128:(dc + 1) * 128], sel[:, s, :], s == 0, s == nsub - 1,
                             [h2m, sel], [pg])
                    P.cp("act", xe[:, dc, :], pg[:, 0:CAP], [pg], [xe])
                sgt = sgtr.next()
                for s0 in range(0, nsub, 2):
                    pstr = pstrr.next()
                    for sl in range(2):
                        for jg in range(2):
                            P.tr(pstr[:, sl * 2 + jg, :], selg[:, s0 + sl, jg * 128:(jg + 1) * 128], K["ident_b"][:],
                                 [selg, K["ident_b"]], [pstr])
                    P.cp("act", sgt[:, :, s0:s0 + 2, :], pstr[:].rearrange("p (sl jg) t -> p jg sl t", jg=2), [pstr], [sgt])
                at = atr_.next()
                for q in range(4):
                    w1s = W1[(e, q)]
                    for gch in range(2):
                        fc = q * 2 + gch
                        pg, pl = psa.next(), psa.next()
                        for sidx, pp in ((0, pg), (1, pl)):
                            for kc in range(8):
                                P.mm(pp[:, 0:CAP], w1s[:, kc, gch * 256 + sidx:gch * 256 + 256:2], xe[:, kc, :],
                                     kc == 0, kc == 7, [w1s, xe], [pp])
                        gl, sg, ln = glr.next(), sgr.next(), lnr.next()
                        P.ts("dve", gl[:], pg[:, 0:CAP], B1T[:, e, 0, fc:fc + 1], 7.0, ALU.add, ALU.min, [pg, B1T], [gl])
                        P.act(sg[:], gl[:], AF.Sigmoid, [gl], [sg], scale=1.702)
                        P.ts("dve", ln[:], pl[:, 0:CAP], B1T[:, e, 1, fc:fc + 1], 7.0, ALU.add, ALU.min, [pl, B1T], [ln])
                        P.ts("dve", ln[:], ln[:], -7.0, 1.0, ALU.max, ALU.add, [ln], [ln])
                        P.tt("dve", gl[:], gl[:], sg[:], ALU.mult, [gl, sg], [gl])
                        P.tt("dve", at[:, fc, :], gl[:], ln[:], ALU.mult, [gl, ln], [at])
                    if pre:
                        load_w1(e + 1, q)
                if pre:
                    load_w2(e + 1, 0)
                    load_w2(e + 1, 1)
                ye = yer.next()
                for jg in range(2):
                    for hf in range(2):
                        po = psb.next()
                        w2s = W2[(e, hf)]
                        for fc in range(8):
                            P.mm(po[:], at[:, fc, jg * 128:(jg + 1) * 128], w2s[:, fc, :], fc == 0, fc == 7, [at, w2s], [po])
                        P.cp("act", ye[:, jg, hf * 512:(hf + 1) * 512], po[:], [po], [ye])
                for s in range(nsub):
                    for hf in range(2):
                        po = psb.next()
                        for jg in range(2):
                            P.mm(po[:], sgt[:, jg, s, :], ye[:, jg, hf * 512:(hf + 1) * 512], jg == 0, jg == 1, [sgt, ye], [po])
                        asl = acc[:, s, hf * 512:(hf + 1) * 512]
                        P.tt("dve", asl, po[:], asl, ALU.add, [po, acc], [acc])
            for s in range(nsub):
                r0 = t0 + s * 128
                xt = xr.next()
                P.dma("sp", xt[:], X["XR"][r0:r0 + 128, :], xt, writes=[xt])
                P.tt("dve", acc[:, s, :], acc[:, s, :], g2[w][:], ALU.mult, [acc, g2[w]], [acc])
                P.tt("dve", xt[:], xt[:], acc[:, s, :], ALU.add, [xt, acc], [xt])
                if not last:
                    P.dma("sp", X["XR"][r0:r0 + 128, :], xt[:], xt, reads=[xt])
                else:
                    st = str_.next()
                    sumsq(P, scr_b, xt, st)
                    rstd(P, st, st[:, 1:2], st[:, 0:1], D, K["epsc"])
                    P.stt("dve", xt[:], xt[:], st[:, 1:2], FG[:], ALU.mult, ALU.mult, [xt, st, FG], [xt])
                    P.dma("sp", yout[r0 - C:r0 - C + 128, :], xt[:], xt, reads=[xt])


def host_consts():
    inv_freq = (10000.0 ** (-np.arange(0, 32, 2, dtype=np.float32) / 32.0)).astype(np.float32)
    pos = np.arange(S)
    row = (pos // 64).astype(np.float32)
    col = (pos % 64).astype(np.float32)
    ang = np.concatenate([row[:, None] * inv_freq[None, :], col[:, None] * inv_freq[None, :]], axis=1).astype(np.float32)
    m = np.arange(128)
    tri = np.zeros((6, 128, 128), np.float32)
    tri[0] = (m[:, None] <= m[None, :]) * (-1.0 / 16.0)
    tri[1] = (m[:, None] >= m[None, :]) * (-1.0 / 16.0)
    tri[2] = (m[:, None] > m[None, :]) * (-1.0 / 16.0)
    tri[3] = (m[:, None] < m[None, :]) * (-1.0 / 16.0)
    tri[4] = (m[:, None] <= m[None, :]) * 1.0
    tri[5] = (m[:, None] >= m[None, :]) * 1.0
    return dict(k_cos=np.cos(ang).astype(np.float32), k_sin=np.sin(ang).astype(np.float32),
                k_ident=np.eye(128, dtype=np.float32), k_tri=tri, k_iota=np.arange(MOE_CAP, dtype=np.float32))


def make_in_maps(inputs, cores, used=None):
    kc = host_consts()
    f = lambda a: np.ascontiguousarray(np.asarray(a, dtype=np.float32))
    shared = {k: f(inputs[k]) for k in ("c_ctx", "ada_w", "ada_b", "norm1_g", "norm2_g", "w_in", "diff_subln_g",
                                        "diff_w_out", "conv_w", "conv_w_out", "gla_w_a2", "gla_norm_g", "gla_w_out",
                                        "w_o", "router_w", "router_b", "moe_w1", "moe_b1", "moe_w2", "moe_b2",
                                        "final_norm_g")}
    shared["diff_lambda"] = f(inputs["diff_lambda"]).reshape(DEPTH, 256)
    shared["gla_b_a"] = f(inputs["gla_b_a"]).reshape(DEPTH, 512)
    shared.update(kc)
    maps = []
    for b in cores:
        m = dict(shared)
        m["x"] = f(inputs["x"][b])
        m["c"] = f(inputs["c"][b])
        m["ctx"] = f(inputs["ctx"][b])
        if used is not None:
            m = {k: v for k, v in m.items() if k in used}
        maps.append(m)
    return maps


def kernel(**inputs):
    nc = build()
    maps = make_in_maps(inputs, list(range(8)))
    res = run_bass_kernel_spmd(nc, maps, core_ids=list(range(8)))
    return np.stack([np.asarray(r["y"], dtype=np.float32) for r in res.results], axis=0)
```

```python
import math
from contextlib import ExitStack
import numpy as np
import concourse.bass as bass
import concourse.mybir as mybir
from concourse.bass_utils import run_bass_kernel_spmd

F32 = mybir.dt.float32
BF16 = mybir.dt.bfloat16
AF = mybir.ActivationFunctionType
ALU = mybir.AluOpType
AX = mybir.AxisListType

D = 1024
S = 4096
C = 256
T = S + C
NT = T // 128
DEPTH = 2
NE = 32
WIN = 9248
EPS = 1e-6
OQ, OK_, OV = 0, 1024, 2048
OCB, OCC, OCX = 3072, 3584, 4096
OGQ, OGK, OGV, OGR, OGA = 4608, 4864, 5120, 5632, 6144
OGT = 6176
TILES = [(0, 256)] + [(256 + 512 * i, 512) for i in range(8)]
MOE_CAP = 256


DBG = {}


class Dep:
    def __init__(self):
        self.w = None
        self.r = {}
        self.ds = None


class TL(Dep):
    def __init__(self, h):
        super().__init__()
        self.h = h

    def __getitem__(self, k):
        return self.h[k]


class DSem:
    def __init__(self, sem):
        self.sem = sem
        self.cnt = 0


class Prog:
    def __init__(self, nc, ndsem=96):
        self.nc = nc
        self.E = {"pe": nc.tensor, "act": nc.scalar, "dve": nc.vector, "pool": nc.gpsimd, "sp": nc.sync}
        self.sem = {k: nc.alloc_semaphore("s_" + k) for k in self.E}
        self.cnt = {k: 0 for k in self.E}
        self.seen = {k: {} for k in self.E}
        self.dpool = [DSem(nc.alloc_semaphore("d%d" % i)) for i in range(ndsem)]
        self.dnext = 0
        self.persist = 0
        self.nsb = 0
        self.pe_cols = [0]
        self.dly = {}
        self.nfence = 0

    def sb(self, es, shape, dt, name=None):
        self.nsb += 1
        h = es.enter_context(self.nc.sbuf_tensor("t%d" % self.nsb, list(shape), dt))
        return TL(h)

    def ps(self, es, shape, dt=F32):
        self.nsb += 1
        h = es.enter_context(self.nc.psum_tensor("p%d" % self.nsb, list(shape), dt))
        return TL(h)

    def _ds(self, t):
        if t.ds is None:
            assert self.dnext < len(self.dpool), "out of dma semaphores"
            t.ds = self.dpool[self.dnext]
            self.dnext += 1
        return t.ds

    def _wait(self, eng, ev, raw=False):
        if ev is None:
            return
        if ev[0] == "e":
            _, src, val = ev
            if src == eng and (eng == "pe" or not raw):
                return
            key = src
            sem = self.sem[src]
            if src == "pe":
                need = self.pe_cols[val] + 256
                k2 = val
                while k2 < self.cnt["pe"] and self.pe_cols[k2] < need:
                    k2 += 1
                if self.pe_cols[k2] >= need:
                    val = k2
                else:
                    val = self.cnt["pe"]
                    if self.seen[eng].get(key, 0) < val:
                        self.E[eng].wait_ge(sem, val)
                        self.seen[eng][key] = val
                    if self.seen[eng].get("pe_safe", 0) < val:
                        self._delay(eng)
                        self.seen[eng]["pe_safe"] = val
                    return
                if self.seen[eng].get("pe_safe", 0) < val:
                    self.seen[eng]["pe_safe"] = val
        else:
            ds = ev[1]
            key = id(ds)
            sem = ds.sem
            val = ds.cnt
        if self.seen[eng].get(key, 0) >= val:
            return
        self.E[eng].wait_ge(sem, val)
        self.seen[eng][key] = val

    def _delay(self, eng):
        if eng not in self.dly:
            return
        self.nfence += 1
        d = self.dly[eng]
        if eng == "act":
            self.nc.scalar.copy(d[:, 0:256], d[:, 256:512])
        else:
            self.E[eng].memset(d[:, 0:256], 0.0)

    def _deps(self, eng, reads, writes):
        for t in reads:
            self._wait(eng, t.w, raw=True)
        for t in writes:
            self._wait(eng, t.w)
            for ev in list(t.r.values()):
                self._wait(eng, ev)

    def _mark(self, ev, key, reads, writes):
        for t in reads:
            t.r[key] = ev
        for t in writes:
            t.w = ev
            t.r = {}

    def op(self, eng, ins_fn, reads=(), writes=(), pe_n=None):
        self._deps(eng, reads, writes)
        ins = ins_fn()
        self.cnt[eng] += 1
        if eng == "pe":
            if pe_n is None:
                try:
                    pe_n = int(ins.ins.outs[0].free_size()) if False else 128
                except Exception:
                    pe_n = 128
            self.pe_cols.append(self.pe_cols[-1] + pe_n)
        ins.then_inc(self.sem[eng], 1)
        self._mark(("e", eng, self.cnt[eng]), eng, reads, writes)
        return ins

    def dma(self, q, out, in_, holder, reads=(), writes=(), **kw):
        self._deps(q, reads, writes)
        ds = self._ds(holder)
        ins = self.E[q].dma_start(out=out, in_=in_, **kw)
        ins.then_inc(ds.sem, 16)
        ds.cnt += 16
        self._mark(("d", ds), id(ds), reads, writes)

    def barrier(self):
        for ds in self.dpool[: self.dnext]:
            if ds.cnt > 0:
                self._wait("sp", ("d", ds))
        for e in self.E:
            if e != "sp":
                self._wait("sp", ("e", e, self.cnt[e]))
        self.E["sp"].sem_inc(self.sem["sp"], 1)
        self.cnt["sp"] += 1
        for e in self.E:
            if e == "sp":
                continue
            for o in self.E:
                if o != e:
                    self._wait(e, ("e", o, self.cnt[o]))
        self.dnext = self.persist

    def rep(self, name):
        print("SBUF remaining after", name, self.nc.sbuf_bytes_remaining, flush=True)

    def persist_dsems(self):
        self.persist = self.dnext

    def mm(self, out, lhsT, rhs, start, stop, reads, writes):
        n = 1
        for d_ in rhs.shape[1:]:
            n *= int(d_)
        return self.op("pe", lambda: self.nc.tensor.matmul(out, lhsT, rhs, start=start, stop=stop), reads, writes, pe_n=n)

    def tr(self, out, in_, ident, reads, writes):
        return self.op("pe", lambda: self.nc.tensor.transpose(out, in_, ident), reads, writes, pe_n=64)

    def act(self, out, in_, func, reads, writes, **kw):
        return self.op("act", lambda: self.nc.scalar.activation(out=out, in_=in_, func=func, **kw), reads, writes)

    def ts(self, eng, out, in0, s1, s2, op0, op1, reads, writes):
        eng = self.cmap(eng)
        e = self.E[eng]
        if op1 is None:
            return self.op(eng, lambda: e.tensor_scalar(out, in0, s1, None, op0), reads, writes)
        return self.op(eng, lambda: e.tensor_scalar(out, in0, s1, s2, op0, op1), reads, writes)

    def tt(self, eng, out, in0, in1, op, reads, writes):
        eng = self.cmap(eng)
        e = self.E[eng]
        return self.op(eng, lambda: e.tensor_tensor(out, in0, in1, op), reads, writes)

    def stt(self, eng, out, in0, scalar, in1, op0, op1, reads, writes):
        eng = self.cmap(eng)
        e = self.E[eng]
        return self.op(eng, lambda: e.scalar_tensor_tensor(out, in0, scalar, in1, op0, op1), reads, writes)

    def cp(self, eng, out, in_, reads, writes):
        eng = self.cmap(eng)
        if eng == "act":
            return self.op("act", lambda: self.nc.scalar.copy(out, in_), reads, writes)
        e = self.E[eng]
        return self.op(eng, lambda: e.tensor_copy(out, in_), reads, writes)

    def cmap(self, eng):
        return "dve" if (eng == "pool" and not DBG.get("pool_compute", False)) else eng

    def memset(self, eng, t, ap, val):
        eng = self.cmap(eng)
        e = self.E[eng]
        return self.op(eng, lambda: e.memset(ap, val), (), (t,))


class Ring:
    def __init__(self, tiles):
        self.t = tiles
        self.i = 0

    def next(self):
        t = self.t[self.i % len(self.t)]
        self.i += 1
        return t


def build(n_layers=DEPTH, debug_out=(), stop_after=None):
    nc = bass.Bass("TRN2", target_bir_lowering=False)
    P = Prog(nc)

    def din(name, shape, dt=F32):
        return nc.dram_tensor(name, list(shape), dt, kind="ExternalInput").ap()

    SHAPES = dict(x=[S, D], c=[D], ctx=[C, D], c_ctx=[D], ada_w=[DEPTH, D, 6 * D], ada_b=[DEPTH, 6 * D],
                  norm1_g=[DEPTH, D], norm2_g=[DEPTH, D], w_in=[DEPTH, D, WIN], diff_lambda=[DEPTH, 256],
                  diff_subln_g=[DEPTH, 128], diff_w_out=[DEPTH, D, D], conv_w=[DEPTH, 3, 512],
                  conv_w_out=[DEPTH, 512, D], gla_w_a2=[DEPTH, 2, 16, 256], gla_b_a=[DEPTH, 512],
                  gla_norm_g=[DEPTH, 128], gla_w_out=[DEPTH, 512, D], w_o=[DEPTH, D, D], router_w=[DEPTH, D, NE],
                  router_b=[DEPTH, NE], moe_w1=[DEPTH, NE, D, 2 * D], moe_b1=[DEPTH, NE, 2 * D],
                  moe_w2=[DEPTH, NE, D, D], moe_b2=[DEPTH, NE, D], final_norm_g=[D],
                  k_cos=[S, 32], k_sin=[S, 32], k_ident=[128, 128], k_tri=[6, 128, 128], k_iota=[MOE_CAP])

    class LazyIn(dict):
        def __missing__(self, k):
            self[k] = din(k, SHAPES[k])
            return self[k]

    I = LazyIn()
    if stop_after is None:
        for k in SHAPES:
            I[k]
    yout = nc.dram_tensor("y", [S, D], F32, kind="ExternalOutput").ap()

    def scr(name, shape, dt=F32):
        kind = "ExternalOutput" if name in debug_out else "Internal"
        return nc.dram_tensor(name, list(shape), dt, kind=kind).ap()

    X = {}
    X["XR"] = scr("XR", [T, D])
    X["MODS"] = scr("MODS", [2, 128, 6 * D])
    X["QT"] = scr("QT", [8, 128, T], BF16)
    X["KT"] = scr("KT", [8, 128, T], BF16)
    X["V"] = scr("V", [8, 128, NT * 132], BF16)
    X["CBT"] = scr("CBT", [4, 128, T])
    X["CCT"] = scr("CCT", [4, 128, T])
    X["CXT"] = scr("CXT", [4, 128, T])
    X["GQT"] = scr("GQT", [2, 128, T])
    X["GKT"] = scr("GKT", [2, 128, T])
    X["GK"] = scr("GK", [T, 256])
    X["GV"] = scr("GV", [T, 512], BF16)
    X["GR"] = scr("GR", [T, 512])
    X["GAF"] = scr("GAF", [16, T])
    X["GAB"] = scr("GAB", [16, T])
    X["SIGT"] = scr("SIGT", [24, 128, T], BF16)
    X["DIFFT"] = scr("DIFFT", [8, 128, T], BF16)
    X["YCT"] = scr("YCT", [4, 128, T], BF16)
    X["YGT"] = scr("YGT", [4, 128, T], BF16)
    X["H2T"] = scr("H2T", [8, 128, T], BF16)
    X["GATES"] = scr("GATES", [T, NE])
    X["H2M"] = scr("H2M", [T, D], BF16)
    if "HT" in debug_out:
        X["HT"] = scr("HT", [8, 128, T], BF16)

    with ExitStack() as gs:
        ident_f = P.sb(gs, [128, 128], F32)
        ident_b = P.sb(gs, [128, 128], BF16)
        ones_f = P.sb(gs, [128, 128], F32)
        lam = P.sb(gs, [128, 4], F32)
        subg = P.sb(gs, [128, 128], F32)
        glag = P.sb(gs, [128, 128], F32)
        P.dma("sp", ident_f[:], I["k_ident"], ident_f, writes=[ident_f])
        P.cp("dve", ident_b[:], ident_f[:], [ident_f], [ident_b])
        P.memset("dve", ones_f, ones_f[:], 1.0)
        for e_ in ("act", "dve"):
            P.dly[e_] = P.sb(gs, [128, 512], F32)
            P.memset("dve", P.dly[e_], P.dly[e_][:], 0.0)
        epsc = P.sb(gs, [128, 1], F32)
        P.memset("dve", epsc, epsc[:], EPS)
        P.persist_dsems()
        P.barrier()

        for L in range(n_layers):
            last = L == n_layers - 1 and n_layers == DEPTH
            lam_init = 0.8 - 0.6 * math.exp(-0.3 * L)
            phases = [phase0, phase1_2, phase3, phase4, phase5, phase6, phase7s if DBG.get('sparse_moe') else phase7]
            for ph in phases:
                kk = dict(ident_f=ident_f, ident_b=ident_b, ones_f=ones_f, lam=lam, subg=subg, glag=glag, epsc=epsc)
                if ph in (phase7, phase7s):
                    ph(P, I, X, L, last, lam_init, kk, yout)
                else:
                    ph(P, I, X, L, last, lam_init, kk)
                P.barrier()
                if stop_after == (L, ph.__name__):
                    break
            else:
                continue
            break
        P.barrier()
    nc._used_inputs = set(I.keys())
    return nc


def phase0(P, I, X, L, last, lam_init, K):
    nc = P.nc
    with ExitStack() as es:
        cs = P.sb(es, [128, 8, 2], F32)
        crep = [P.sb(es, [128, 8, 128], BF16) for _ in range(2)]
        mod = [P.sb(es, [128, 6 * D], F32) for _ in range(2)]
        grep = [P.sb(es, [128, D], F32) for _ in range(2)]
        wring = Ring([P.sb(es, [128, 8, 512], BF16) for _ in range(2)])
        pss = Ring([P.ps(es, [128, 512]) for _ in range(4)])
        dl = P.sb(es, [128, 256], F32)
        tmp = P.sb(es, [128, 256], F32)

        with nc.allow_non_contiguous_dma(reason="tiny transposed vector load"):
            P.dma("sp", cs[:, :, 0], I["c"].rearrange("(kc p) -> p kc", p=128), cs, writes=[cs])
            P.dma("sp", cs[:, :, 1], I["c_ctx"].rearrange("(kc p) -> p kc", p=128), cs, writes=[cs])
        P.act(cs[:], cs[:], AF.Silu, [cs], [cs])
        for w in range(2):
            for kc in range(8):
                P.ts("dve", crep[w][:, kc, :], K["ones_f"][:], cs[:, kc, w:w + 1], None, ALU.mult, None,
                     [cs, K["ones_f"]], [crep[w]])
            P.dma("sp", mod[w][:], I["ada_b"][L].partition_broadcast(128), mod[w], writes=[mod[w]])
        P.dma("sp", grep[0][:], I["norm1_g"][L].partition_broadcast(128), grep[0], writes=[grep[0]])
        P.dma("sp", grep[1][:], I["norm2_g"][L].partition_broadcast(128), grep[1], writes=[grep[1]])
        aw = I["ada_w"][L].rearrange("(kc p) n -> p kc n", p=128)
        for cb in range(12):
            wt = wring.next()
            P.dma("pool", wt[:], aw[:, :, cb * 512:(cb + 1) * 512], wt, writes=[wt])
            for w in range(2):
                ps = pss.next()
                for kc in range(8):
                    P.mm(ps[:], crep[w][:, kc, :], wt[:, kc, :], kc == 0, kc == 7, [crep[w], wt], [ps])
                sl = mod[w][:, cb * 512:(cb + 1) * 512]
                P.tt("dve", sl, ps[:], sl, ALU.add, [ps, mod[w]], [mod[w]])
        for w in range(2):
            for seg, g in ((1, grep[0]), (4, grep[1])):
                sl = mod[w][:, seg * D:(seg + 1) * D]
                P.stt("dve", sl, sl, 1.0, g[:], ALU.add, ALU.mult, [mod[w], g], [mod[w]])
            P.dma("sp", X["MODS"][w], mod[w][:], mod[w], reads=[mod[w]])
        lamt = K["lam"]
        P.dma("sp", dl[:], I["diff_lambda"][L].partition_broadcast(128), dl, writes=[dl])
        P.tt("dve", tmp[:, 0:64], dl[:, 0:64], dl[:, 64:128], ALU.mult, [dl], [tmp])
        P.tt("dve", tmp[:, 64:128], dl[:, 128:192], dl[:, 192:256], ALU.mult, [dl], [tmp])
        P.op("dve", lambda: nc.vector.tensor_reduce(lamt[:, 1:3], tmp[:, 0:128].rearrange("p (a b) -> p a b", a=2),
                                                    AX.X, ALU.add), [tmp], [lamt])
        P.act(lamt[:, 1:3], lamt[:, 1:3], AF.Exp, [lamt], [lamt])
        P.tt("dve", lamt[:, 0:1], lamt[:, 1:2], lamt[:, 2:3], ALU.subtract, [lamt], [lamt])
        P.ts("dve", lamt[:, 0:1], lamt[:, 0:1], float(lam_init), None, ALU.add, None, [lamt], [lamt])
        P.dma("sp", K["subg"][:], I["diff_subln_g"][L].partition_broadcast(128), K["subg"], writes=[K["subg"]])
        P.ts("dve", K["subg"][:], K["subg"][:], float(1.0 - lam_init), None, ALU.mult, None, [K["subg"]], [K["subg"]])
        P.dma("sp", K["glag"][:], I["gla_norm_g"][L].partition_broadcast(128), K["glag"], writes=[K["glag"]])


def rstd(P, st, out_ap, in_ap, n, epsc):
    P.act(out_ap, in_ap, AF.Ln, [st, epsc], [st], scale=1.0 / n, bias=epsc[:, 0:1])
    P.act(out_ap, out_ap, AF.Exp, [st], [st], scale=-0.5)


def xsrc(I, X, L, r0):
    if L > 0:
        return X["XR"][r0:r0 + 128, :]
    if r0 < C:
        return I["ctx"][r0:r0 + 128, :]
    return I["x"][r0 - C:r0 - C + 128, :]


def sumsq(P, scr, xt, st):
    P.act(scr[:], xt[:], AF.Square, [xt], [scr])
    P.op("dve", lambda: P.nc.vector.tensor_reduce(st[:, 0:1], scr[:], AX.X, ALU.add), [scr], [st])


def norm_mod(P, xt, Gt, SHt, hb, scr_b, st, epsc, eng2="pool", xo=None):
    nc = P.nc
    sumsq(P, scr_b, xt, st)
    rstd(P, st, st[:, 1:2], st[:, 0:1], D, epsc)
    xo = xt if xo is None else xo
    P.stt("dve", xo[:], xt[:], st[:, 1:2], Gt[:], ALU.mult, ALU.mult, [xt, st, Gt], [xo])
    P.tt(eng2, hb[:], xo[:], SHt[:], ALU.add, [xo, SHt], [hb])


def phase1_2(P, I, X, L, last, lam_init, K):
    nc = P.nc
    with ExitStack() as es:
        hT = P.sb(es, [128, 8, T], BF16)
        with ExitStack() as e1:
            msl = [[P.sb(e1, [128, D], F32) for _ in range(2)] for _ in range(2)]
            for w in range(2):
                for j, seg in enumerate((0, 1)):
                    P.dma("sp", msl[w][j][:], X["MODS"][w][:, seg * D:(seg + 1) * D], msl[w][j], writes=[msl[w][j]])
            xr = Ring([P.sb(e1, [128, D], F32) for _ in range(3)])
            hbr = Ring([P.sb(e1, [128, D], BF16) for _ in range(2)])
            scr_b = P.sb(e1, [128, D], F32)
            str_ = Ring([P.sb(e1, [128, 2], F32) for _ in range(2)])
            ptr = Ring([P.ps(e1, [128, 8, 128], BF16) for _ in range(2)])
            for i in range(NT):
                w = 1 if i < 2 else 0
                xt = xr.next()
                P.dma("sp", xt[:], xsrc(I, X, L, i * 128), xt, writes=[xt])
                hb = hbr.next()
                st = str_.next()
                norm_mod(P, xt, msl[w][1], msl[w][0], hb, scr_b, st, K["epsc"])
                pt = ptr.next()
                for kc in range(8):
                    P.tr(pt[:, kc, :], hb[:, kc * 128:(kc + 1) * 128], K["ident_b"][:], [hb, K["ident_b"]], [pt])
                P.cp("act", hT[:, :, i * 128:(i + 1) * 128], pt[:], [pt], [hT])
            P.barrier()
        if "HT" in X:
            P.dma("sp", X["HT"].rearrange("c p t -> p c t"), hT[:], hT, reads=[hT])
            return
        phase2(P, I, X, L, K, hT)


def phase2(P, I, X, L, K, hT):
    nc = P.nc
    wv = I["w_in"][L].rearrange("(kc p) n -> p kc n", p=128)
    with ExitStack() as es:
        wring = Ring([P.sb(es, [128, 8, 512], BF16) for _ in range(3)])
        psr = Ring([P.ps(es, [128, 512]) for _ in range(4)])
        ptr = Ring([P.ps(es, [128, 4, 128], BF16) for _ in range(2)])
        cos = P.sb(es, [128, 32, 32], F32)
        sin = P.sb(es, [128, 32, 32], F32)
        P.dma("sp", cos[:], I["k_cos"].rearrange("(t p) f -> p t f", p=128), cos, writes=[cos])
        P.dma("sp", sin[:], I["k_sin"].rearrange("(t p) f -> p t f", p=128), sin, writes=[sin])
        ra = Ring([P.sb(es, [128, 512], F32) for _ in range(2)])
        rb = Ring([P.sb(es, [128, 512], F32) for _ in range(2)])
        rob = Ring([P.sb(es, [128, 512], BF16) for _ in range(2)])
        stq = Ring([P.sb(es, [128, 4, 512], BF16) for _ in range(2)])
        stf = Ring([P.sb(es, [128, 4, 512], F32) for _ in range(2)])

        def load_w(c0, ncols):
            wt = wring.next()
            P.dma("pool", wt[:, :, 0:ncols], wv[:, :, c0:c0 + ncols], wt, writes=[wt])
            return wt

        def tok_major(wt, cw0, ncols, i):
            ps = psr.next()
            for kc in range(8):
                P.mm(ps[:, 0:ncols], hT[:, kc, i * 128:(i + 1) * 128], wt[:, kc, cw0:cw0 + ncols],
                     kc == 0, kc == 7, [hT, wt], [ps])
            return ps

        def feat_major(wt, cw0, m, t0, n):
            ps = psr.next()
            for kc in range(8):
                P.mm(ps[0:m, 0:n], wt[:, kc, cw0:cw0 + m], hT[:, kc, t0:t0 + n], kc == 0, kc == 7, [hT, wt], [ps])
            return ps

        for which, dst, c_base in (("q", X["QT"], OQ), ("k", X["KT"], OK_)):
            for half in range(2):
                wt = load_w(c_base + half * 512, 512)
                for (t0, n) in TILES:
                    sq = stq.next()
                    for s in range(n // 128):
                        i = (t0 + s * 128) // 128
                        ps = tok_major(wt, 0, 512, i)
                        ob = rob.next()
                        if i < 2:
                            P.cp("act", ob[:], ps[:], [ps], [ob])
                        else:
                            li = i - 2
                            a = ra.next()
                            b = rb.next()
                            for ax in range(2):
                                def v4(ap):
                                    return ap.rearrange("p (h r) -> p h r", r=64)[:, :, ax * 32:(ax + 1) * 32].rearrange("p h (s f) -> p h s f", s=2)
                                x4, a4, b4, o4 = v4(ps[:]), v4(a[:]), v4(b[:]), v4(ob[:])
                                cs4 = cos[:, li, ax * 16:(ax + 1) * 16].unsqueeze(1).unsqueeze(1).broadcast_to([128, 8, 2, 16])
                                sn3 = sin[:, li, ax * 16:(ax + 1) * 16].unsqueeze(1).broadcast_to([128, 8, 16])
                                P.tt("dve", a4, x4, cs4, ALU.mult, [ps, cos], [a])
                                P.tt("dve", b4[:, :, 0, :], x4[:, :, 1, :], sn3, ALU.mult, [ps, sin], [b])
                                P.tt("dve", b4[:, :, 1, :], x4[:, :, 0, :], sn3, ALU.mult, [ps, sin], [b])
                                P.tt("pool", o4[:, :, 0, :], a4[:, :, 0, :], b4[:, :, 0, :], ALU.subtract, [a, b], [ob])
                                P.tt("pool", o4[:, :, 1, :], a4[:, :, 1, :], b4[:, :, 1, :], ALU.add, [a, b], [ob])
                        pt = ptr.next()
                        for hh in range(4):
                            P.tr(pt[:, hh, :], ob[:, hh * 128:(hh + 1) * 128], K["ident_b"][:], [ob, K["ident_b"]], [pt])
                        P.cp("act", sq[:, :, s * 128:(s + 1) * 128], pt[:], [pt], [sq])
                    P.dma("sp", dst[half * 4:(half + 1) * 4, :, t0:t0 + n].rearrange("h p t -> p h t"),
                          sq[:, :, 0:n], sq, reads=[sq])
        with ExitStack() as ev_:
            vst = P.sb(ev_, [128, 4, NT, 132], BF16)
            P.memset("dve", vst, vst[:].rearrange("p h t e -> p (h t e)"), 1.0)
            for half in range(2):
                wt = load_w(OV + half * 512, 512)
                for i in range(NT):
                    ps = tok_major(wt, 0, 512, i)
                    P.cp("act", vst[:, :, i, 0:128], ps[:].rearrange("p (h e) -> p h e", h=4), [ps], [vst])
                for hh in range(4):
                    P.dma("sp", X["V"][half * 4 + hh], vst[:, hh, :, :].rearrange("p t e -> p (t e)"), vst, reads=[vst])
        for dst, c0 in ((X["CBT"], OCB), (X["CCT"], OCC), (X["CXT"], OCX)):
            wt = load_w(c0, 512)
            for (t0, n) in TILES:
                sf = stf.next()
                for ch in range(4):
                    ps = feat_major(wt, ch * 128, 128, t0, n)
                    P.cp("act", sf[:, ch, 0:n], ps[:, 0:n], [ps], [sf])
                P.dma("sp", dst[:, :, t0:t0 + n].rearrange("c p t -> p c t"), sf[:, :, 0:n], sf, reads=[sf])
        wt = load_w(OGQ, 512)
        for (t0, n) in TILES:
            sf = stf.next()
            for ch in range(4):
                ps = feat_major(wt, ch * 128, 128, t0, n)
                P.cp("act", sf[:, ch, 0:n], ps[:, 0:n], [ps], [sf])
            P.dma("sp", X["GQT"][:, :, t0:t0 + n].rearrange("c p t -> p c t"), sf[:, 0:2, 0:n], sf, reads=[sf])
            P.dma("sp", X["GKT"][:, :, t0:t0 + n].rearrange("c p t -> p c t"), sf[:, 2:4, 0:n], sf, reads=[sf])
            sf = stf.next()
            for s in range(n // 128):
                ps = tok_major(wt, 256, 256, (t0 + s * 128) // 128)
                P.cp("act", sf[:, s, 0:256], ps[:, 0:256], [ps], [sf])
            P.dma("sp", X["GK"][t0:t0 + n, :].rearrange("(s p) c -> p s c", p=128), sf[:, 0:n // 128, 0:256], sf, reads=[sf])
        wt = load_w(OGV, 512)
        for (t0, n) in TILES:
            sq = stq.next()
            for s in range(n // 128):
                ps = tok_major(wt, 0, 512, (t0 + s * 128) // 128)
                P.cp("act", sq[:, s, :], ps[:], [ps], [sq])
            P.dma("sp", X["GV"][t0:t0 + n, :].rearrange("(s p) c -> p s c", p=128), sq[:, 0:n // 128, :], sq, reads=[sq])
        wt = load_w(OGR, 512)
        for (t0, n) in TILES:
            sf = stf.next()
            for s in range(n // 128):
                ps = tok_major(wt, 0, 512, (t0 + s * 128) // 128)
                P.act(sf[:, s, :], ps[:], AF.Silu, [ps], [sf])
            P.dma("sp", X["GR"][t0:t0 + n, :].rearrange("(s p) c -> p s c", p=128), sf[:, 0:n // 128, :], sf, reads=[sf])
        wt = load_w(OGA, 32)
        for (t0, n) in TILES:
            sf = stf.next()
            ps = feat_major(wt, 0, 32, t0, n)
            P.cp("act", sf[0:32, 0, 0:n], ps[0:32, 0:n], [ps], [sf])
            P.dma("sp", X["GAF"][:, t0:t0 + n], sf[0:16, 0, 0:n], sf, reads=[sf])
            P.dma("sp", X["GAB"][:, t0:t0 + n], sf[16:32, 0, 0:n], sf, reads=[sf])
        for gblk in range(6):
            wt = load_w(OGT + gblk * 512, 512)
            for (t0, n) in TILES:
                sq = stq.next()
                for ch in range(4):
                    ps = feat_major(wt, ch * 128, 128, t0, n)
                    P.act(sq[:, ch, 0:n], ps[:, 0:n], AF.Sigmoid, [ps], [sq])
                P.dma("sp", X["SIGT"][gblk * 4:(gblk + 1) * 4, :, t0:t0 + n].rearrange("c p t -> p c t"),
                      sq[:, :, 0:n], sq, reads=[sq])


class V(Dep):
    def __init__(self, ap):
        super().__init__()
        self.ap = ap


def phase3(P, I, X, L, last, lam_init, K):
    nc = P.nc
    with ExitStack() as es:
        ktr = Ring([P.sb(es, [128, T], BF16) for _ in range(2)])
        qtr = Ring([P.sb(es, [128, T], BF16) for _ in range(2)])
        vtr = Ring([P.sb(es, [128, NT, 132], BF16) for _ in range(2)])
        pss = Ring([P.ps(es, [128, 1024]) for _ in range(2)])
        accT = P.ps(es, [128, 1536])
        ptT = Ring([P.ps(es, [128, 2, 128], BF16) for _ in range(1)])
        offs = [0, 160, 320, 512, 672, 832, 1024, 1184]
        accv = [[V(accT[:, offs[m * 4 + s]:offs[m * 4 + s] + 129]) for s in range(4)] for m in range(2)]
        ptr_ = Ring([P.sb(es, [128, 1024], BF16) for _ in range(3)])
        evr = Ring([P.sb(es, [128, 2, 132], F32) for _ in range(8)])
        t1r = Ring([P.sb(es, [128, 128], F32) for _ in range(2)])
        o_r = Ring([P.sb(es, [128, 128], F32) for _ in range(2)])
        jk = P.sb(es, [128, 128], F32)
        obr = Ring([P.sb(es, [128, 128], BF16) for _ in range(2)])
        str_ = Ring([P.sb(es, [128, 4], F32) for _ in range(3)])
        dstr = Ring([P.sb(es, [128, 512], BF16) for _ in range(2)])
        lam = K["lam"]
        qtiles = [(256 + 512 * i, 512, list(range(NT))) for i in range(8)]
        if not last:
            qtiles = [(0, 256, [0, 1])] + qtiles
        if DBG.get("p3_qtiles") is not None:
            qtiles = [qtiles[i] for i in DBG["p3_qtiles"]]
        for h in range(DBG.get("p3_heads", 8)):
            kt, qt, vt = ktr.next(), qtr.next(), vtr.next()
            P.dma("sp", kt[:], X["KT"][h], kt, writes=[kt])
            P.dma("sp", qt[:], X["QT"][h], qt, writes=[qt])
            P.dma("sp", vt[:].rearrange("p t e -> p (t e)"), X["V"][h], vt, writes=[vt])
            for (q0, n, ktl) in qtiles:
                nsub = n // 128
                def qk(kk_):
                    ps_ = pss.next()
                    for m in range(2):
                        P.mm(ps_[:, m * 512:m * 512 + n], kt[m * 64:(m + 1) * 64, kk_ * 128:(kk_ + 1) * 128],
                             qt[m * 64:(m + 1) * 64, q0:q0 + n], True, True, [kt, qt], [ps_])
                    return ps_

                ps_next = qk(ktl[0])
                for ki, kk in enumerate(ktl):
                    ps = ps_next
                    if ki + 1 < len(ktl):
                        ps_next = qk(ktl[ki + 1])
                    pt = ptr_.next()
                    if n == 512:
                        P.act(pt[:], ps[:], AF.Exp, [ps], [pt], scale=0.125)
                    else:
                        for m in range(2):
                            P.act(pt[:, m * 512:m * 512 + n], ps[:, m * 512:m * 512 + n], AF.Exp, [ps], [pt], scale=0.125)
                    if ki == 0:
                        started = set()
                    for m in range(2):
                        for s in range(nsub):
                            av = accv[m][s]
                            bank = offs[m * 4 + s] // 512
                            st_flag = ki == 0 and bank not in started
                            started.add(bank)
                            P.op("pe", lambda: nc.tensor.matmul(av.ap, pt[:, m * 512 + s * 128:m * 512 + (s + 1) * 128],
                                                                vt[:, kk, 0:129], start=st_flag, stop=(ki == len(ktl) - 1),
                                                                skip_group_check=True), [pt, vt], [av], pe_n=129)
                dst = dstr.next()
                evs = []
                for s in range(nsub):
                    ev = evr.next()
                    for m in range(2):
                        P.cp("dve", ev[:, m, 0:129], accv[m][s].ap, [accv[m][s]], [ev])
                    evs.append(ev)
                for s in range(nsub):
                    ev = evs[s]
                    st = str_.next()
                    P.op("dve", lambda: nc.vector.reciprocal(st[:, 0:2], ev[:, :, 128]), [ev], [st])
                    P.tt("dve", st[:, 1:2], st[:, 1:2], lam[:, 0:1], ALU.mult, [st, lam], [st])
                    t1 = t1r.next()
                    o = o_r.next()
                    P.ts("pool", t1[:], ev[:, 1, 0:128], st[:, 1:2], None, ALU.mult, None, [ev, st], [t1])
                    P.stt("dve", o[:], ev[:, 0, 0:128], st[:, 0:1], t1[:], ALU.mult, ALU.subtract, [ev, st, t1], [o])
                    P.tt("pool", jk[:], o[:], o[:], ALU.mult, [o], [jk])
                    P.op("dve", lambda: nc.vector.tensor_reduce(st[:, 2:3], jk[:], AX.X, ALU.add), [jk], [st])
                    rstd(P, st, st[:, 2:3], st[:, 2:3], 128, K["epsc"])
                    ob = obr.next()
                    P.stt("dve", ob[:], o[:], st[:, 2:3], K["subg"][:], ALU.mult, ALU.mult, [o, st, K["subg"]], [ob])
                    pT = ptT.next()
                    P.tr(pT[:, 0, :], ob[:], K["ident_b"][:], [ob, K["ident_b"]], [pT])
                    P.cp("dve", dst[:, s * 128:(s + 1) * 128], pT[:, 0, :], [pT], [dst])
                P.dma("sp", X["DIFFT"][h, :, q0:q0 + n], dst[:, 0:n], dst, reads=[dst])


def phase4(P, I, X, L, last, lam_init, K):
    nc = P.nc
    with ExitStack() as es:
        cw = P.sb(es, [128, 4, 3], F32)
        with nc.allow_non_contiguous_dma(reason="tiny conv taps"):
            for k_ in range(3):
                P.dma("sp", cw[:, :, k_], I["conv_w"][L, k_].rearrange("(c p) -> p c", p=128), cw, writes=[cw])
        zero = P.sb(es, [128, 8], F32)
        P.memset("dve", zero, zero[:], 0.0)
        cb = P.sb(es, [128, T], F32)
        c_ = P.sb(es, [128, T], F32)
        cx = P.sb(es, [128, T], F32)
        up = P.sb(es, [128, T], F32)
        un = P.sb(es, [128, T], F32)
        y = P.sb(es, [128, T], F32)
        yb = P.sb(es, [128, T], BF16)
        for cc in range(4):
            P.dma("sp", cb[:], X["CBT"][cc], cb, writes=[cb])
            P.dma("sp", c_[:], X["CCT"][cc], c_, writes=[c_])
            P.dma("sp", cx[:], X["CXT"][cc], cx, writes=[cx])
            P.tt("dve", c_[:], c_[:], cx[:], ALU.mult, [c_, cx], [c_])
            P.dma("sp", up[:, 1:T], c_[:, 0:T - 1], up, reads=[c_], writes=[up])
            P.dma("sp", un[:, 0:T - 1], c_[:, 1:T], un, reads=[c_], writes=[un])
            for col in (0, C):
                P.dma("sp", up[:, col:col + 1], zero[:, 0:1], up, reads=[zero], writes=[up])
            for col in (C - 1, T - 1):
                P.dma("sp", un[:, col:col + 1], zero[:, 0:1], un, reads=[zero], writes=[un])
            P.ts("dve", y[:], c_[:], cw[:, cc, 1:2], None, ALU.mult, None, [c_, cw], [y])
            P.stt("dve", y[:], up[:], cw[:, cc, 0:1], y[:], ALU.mult, ALU.add, [up, cw, y], [y])
            P.stt("dve", y[:], un[:], cw[:, cc, 2:3], y[:], ALU.mult, ALU.add, [un, cw, y], [y])
            P.tt("dve", yb[:], y[:], cb[:], ALU.mult, [y, cb], [yb])
            P.dma("sp", X["YCT"][cc], yb[:], yb, reads=[yb])


def phase5(P, I, X, L, last, lam_init, K):
    nc = P.nc
    NCH = NT
    with ExitStack() as es:
        tri = P.sb(es, [128, 6, 128], F32)
        P.dma("sp", tri[:], I["k_tri"].rearrange("s m l -> m s l"), tri, writes=[tri])
        wa = P.sb(es, [16, 2, 256], F32)
        P.dma("sp", wa[:], I["gla_w_a2"][L].rearrange("d k n -> k d n"), wa, writes=[wa])
        ba = P.sb(es, [128, 512], F32)
        P.dma("sp", ba[:], I["gla_b_a"][L].partition_broadcast(128), ba, writes=[ba])
        neg16 = P.sb(es, [128, 2], F32)
        P.memset("dve", neg16, neg16[:], -1.0 / 16.0)
        Sf = P.sb(es, [128, 2, 128], F32)
        Sfb = P.sb(es, [128, 2, 128], BF16)
        Sb = P.sb(es, [128, 2, 128], F32)
        SLB = P.sb(es, [128, NCH, 2, 128], F32)
        DECB = P.sb(es, [128, NCH, 2], F32)
        SBP = P.sb(es, [128, NCH, 2, 128], BF16)
        for t_ in (Sf, Sb):
            P.memset("dve", t_, t_[:], 0.0)
        P.memset("dve", Sfb, Sfb[:], 0.0)
        psA = P.ps(es, [128, 512])
        psB = P.ps(es, [128, 512])
        psC = P.ps(es, [128, 1024])
        psO = P.ps(es, [128, 512])
        psS = P.ps(es, [128, 512])
        psT = P.ps(es, [128, 4, 128], BF16)
        gqr = Ring([P.sb(es, [128, 2, 512], F32) for _ in range(2)])
        gkr = Ring([P.sb(es, [128, 2, 512], F32) for _ in range(2)])
        gktr = Ring([P.sb(es, [128, 256], F32) for _ in range(2)])
        gvr = Ring([P.sb(es, [128, 512], BF16) for _ in range(2)])
        grr = Ring([P.sb(es, [128, 512], F32) for _ in range(2)])
        gar = [Ring([P.sb(es, [16, 512], F32) for _ in range(2)]) for _ in range(2)]
        zbr = Ring([P.sb(es, [128, 256], F32) for _ in range(2)])
        spr = Ring([P.sb(es, [128, 256], F32) for _ in range(2)])
        e1r = Ring([P.sb(es, [128, 256], F32) for _ in range(2)])
        e2r = Ring([P.sb(es, [128, 256], F32) for _ in range(2)])
        e3r = Ring([P.sb(es, [128, 256], F32) for _ in range(2)])
        decr = Ring([P.sb(es, [128, 2], F32) for _ in range(2)])
        qdr = [Ring([P.sb(es, [128, 2, 128], BF16) for _ in range(2)]) for _ in range(2)]
        kir = [Ring([P.sb(es, [128, 2, 128], BF16) for _ in range(2)]) for _ in range(2)]
        ker = Ring([P.sb(es, [128, 256], BF16) for _ in range(2)])
        atr = [Ring([P.sb(es, [128, 4, 128], BF16) for _ in range(2)]) for _ in range(2)]
        osr = Ring([P.sb(es, [128, 512], F32) for _ in range(2)])
        sqj = P.sb(es, [128, 512], F32)
        onr = Ring([P.sb(es, [128, 512], F32) for _ in range(2)])
        obr = Ring([P.sb(es, [128, 512], BF16) for _ in range(2)])
        ygr = Ring([P.sb(es, [128, 4, 512], BF16) for _ in range(2)])

        def tile_of(c):
            if c < 2:
                return 0, 256, c * 128
            j = (c - 2) // 4
            return 256 + 512 * j, 512, ((c - 2) % 4) * 128
        str_ = Ring([P.sb(es, [128, 8], F32) for _ in range(2)])
        GAsrc = (X["GAF"], X["GAB"])

        def softplus_neg(c, d, ga, off):
            P.mm(psA[:, 0:256], ga[0:16, off:off + 128], wa[0:16, d, :], True, True, [ga, wa], [psA])
            zb = zbr.next()
            P.tt("dve", zb[:], psA[:, 0:256], ba[:, d * 256:(d + 1) * 256], ALU.add, [psA, ba], [zb])
            P.act(zb[:], zb[:], AF.Exp, [zb], [zb], scale=-1.0)
            sp = spr.next()
            P.act(sp[:], zb[:], AF.Ln, [zb], [sp], bias=1.0)
            return sp

        def tot_dec(sp, dec_ap, dec_t):
            for hp in range(2):
                P.mm(psA[:, 256 + hp:257 + hp], sp[:, hp * 128:(hp + 1) * 128], neg16[:, 0:1], True, True, [sp, neg16], [psA])
            P.act(dec_ap, psA[:, 256:258], AF.Exp, [psA], [dec_t])

        def ke_of(sp, d, gkt):
            P.mm(psB[:, 256:512], tri[:, 2 + d, :], sp[:], True, True, [tri, sp], [psB])
            e3 = e3r.next()
            P.act(e3[:], psB[:, 256:512], AF.Exp, [psB], [e3])
            ke = ker.next()
            P.tt("pool", ke[:], gkt[:], e3[:], ALU.mult, [gkt, e3], [ke])
            return ke

        def sloc(ke, gv):
            for hp in range(2):
                P.mm(psS[:, hp * 256:(hp + 1) * 256], ke[:, hp * 128:(hp + 1) * 128], gv[:, hp * 256:(hp + 1) * 256],
                     True, True, [ke, gv], [psS])

        for c in range(NCH):
            gkt, gv = gktr.next(), gvr.next()
            t0_, tn_, off_ = tile_of(c)
            if off_ == 0:
                gab_t = gar[1].next()
                P.dma("sp", gab_t[:, 0:tn_], X["GAB"][:, t0_:t0_ + tn_], gab_t, writes=[gab_t])
            P.dma("sp", gkt[:], X["GK"][c * 128:(c + 1) * 128, :], gkt, writes=[gkt])
            P.dma("sp", gv[:], X["GV"][c * 128:(c + 1) * 128, :], gv, writes=[gv])
            stg = DBG.get("p5_stage", 99)
            sp = softplus_neg(c, 1, gab_t, off_)
            if stg < 2:
                continue
            tot_dec(sp, DECB[:, c, :], DECB)
            if stg < 3:
                continue
            ke = ke_of(sp, 1, gkt)
            if stg < 4:
                continue
            sloc(ke, gv)
            for hp in range(2):
                for hh in range(2):
                    P.cp("act", SLB[hh * 64:(hh + 1) * 64, c, hp, :],
                         psS[hh * 64:(hh + 1) * 64, hp * 256 + hh * 128:hp * 256 + (hh + 1) * 128], [psS], [SLB])
        if DBG.get("p5_stage", 99) < 5:
            return
        for c in [1, 0] + list(range(NCH - 1, 1, -1)):
            P.cp("pool", SBP[:, c, :, :], Sb[:], [Sb], [SBP])
            for hp in range(2):
                P.stt("dve", Sb[:, hp, :], Sb[:, hp, :], DECB[:, c, hp:hp + 1], SLB[:, c, hp, :], ALU.mult, ALU.add,
                      [Sb, DECB, SLB], [Sb])
        if DBG.get("p5_stage", 99) < 6:
            return
        stg = DBG.get("p5_stage", 99)
        tl = {}

        def stage_a(c):
            gkt, gv, gr = gktr.next(), gvr.next(), grr.next()
            cs = slice(c * 128, (c + 1) * 128)
            t0_, tn_, off_ = tile_of(c)
            osl = slice(off_, off_ + 128)
            if off_ == 0:
                tl["gq"], tl["gk"], tl["yg"] = gqr.next(), gkr.next(), ygr.next()
                tl["ga"] = [gar[0].next(), gar[1].next()]
                ts_ = slice(t0_, t0_ + tn_)
                P.dma("sp", tl["gq"][:, :, 0:tn_], X["GQT"][:, :, ts_].rearrange("c p t -> p c t"), tl["gq"], writes=[tl["gq"]])
                P.dma("sp", tl["gk"][:, :, 0:tn_], X["GKT"][:, :, ts_].rearrange("c p t -> p c t"), tl["gk"], writes=[tl["gk"]])
                for d in range(2):
                    P.dma("sp", tl["ga"][d][:, 0:tn_], GAsrc[d][:, ts_], tl["ga"][d], writes=[tl["ga"][d]])
            gq_t, gk_t, yg, ga = tl["gq"], tl["gk"], tl["yg"], tl["ga"]
            P.dma("sp", gkt[:], X["GK"][cs, :], gkt, writes=[gkt])
            P.dma("sp", gv[:], X["GV"][cs, :], gv, writes=[gv])
            P.dma("sp", gr[:], X["GR"][cs, :], gr, writes=[gr])
            qd, ki, atm = [None, None], [None, None], [None, None]
            dec = decr.next()
            ke = None
            for d in range(2):
                sp = softplus_neg(c, d, ga[d], off_)
                for hp in range(2):
                    P.mm(psB[:, hp * 128:(hp + 1) * 128], sp[:, hp * 128:(hp + 1) * 128], tri[:, d, :], True, True,
                         [sp, tri], [psB])
                e1, e2 = e1r.next(), e2r.next()
                P.act(e1[:], psB[:, 0:256], AF.Exp, [psB], [e1])
                P.act(e2[:], psB[:, 0:256], AF.Exp, [psB], [e2], scale=-1.0)
                qd[d], ki[d] = qdr[d].next(), kir[d].next()
                P.stt("dve", qd[d][:], gq_t[:, :, osl], 0.125, e1[:].rearrange("p (a b) -> p a b", a=2),
                      ALU.mult, ALU.mult, [gq_t, e1], [qd[d]])
                P.tt("pool", ki[d][:], gk_t[:, :, osl], e2[:].rearrange("p (a b) -> p a b", a=2), ALU.mult,
                     [gk_t, e2], [ki[d]])
                if d == 0:
                    tot_dec(sp, dec[:], dec)
                    ke = ke_of(sp, 0, gkt)
                for h in range(4):
                    hp, b0 = h // 2, (h % 2) * 64
                    co = (h % 2) * 512 + (d * 2 + hp) * 128
                    P.mm(psC[:, co:co + 128], ki[d][b0:b0 + 64, hp, :], qd[d][b0:b0 + 64, hp, :],
                         True, True, [ki[d], qd[d]], [psC])
                atm[d] = atr[d].next()
                P.tt("dve", atm[d][:].rearrange("p (hp par) l -> p hp par l", par=2),
                     psC[:].rearrange("p (par d hp l) -> p d hp par l", par=2, d=2, hp=2)[:, d],
                     tri[:, 4 + d, :].unsqueeze(1).unsqueeze(1).broadcast_to([128, 2, 2, 128]), ALU.mult, [psC, tri], [atm[d]])
            return dict(c=c, gv=gv, gr=gr, qd=qd, atm=atm, dec=dec, ke=ke, yg=yg, osl=osl, off=off_, tn=tn_, t0=t0_)

        def stage_b(a_):
            c, gv, gr, qd, atm, dec, ke, yg = a_["c"], a_["gv"], a_["gr"], a_["qd"], a_["atm"], a_["dec"], a_["ke"], a_["yg"]
            osl, off_, tn_, t0_ = a_["osl"], a_["off"], a_["tn"], a_["t0"]
            for h in range(4):
                hp, b0 = h // 2, (h % 2) * 64
                oo = psO[:, h * 128:(h + 1) * 128]
                vv = gv[:, h * 128:(h + 1) * 128]
                P.mm(oo, atm[0][:, h, :], vv, True, False, [atm[0], gv], [psO])
                P.mm(oo, qd[0][b0:b0 + 64, hp, :], Sfb[b0:b0 + 64, hp, :], False, False, [qd[0], Sfb], [psO])
                P.mm(oo, atm[1][:, h, :], vv, False, False, [atm[1], gv], [psO])
                P.mm(oo, qd[1][b0:b0 + 64, hp, :], SBP[b0:b0 + 64, c, hp, :], False, True, [qd[1], SBP], [psO])
            sloc(ke, gv)
            for hp in range(2):
                for hh in range(2):
                    rs = slice(hh * 64, (hh + 1) * 64)
                    P.stt("dve", Sf[rs, hp, :], Sf[rs, hp, :], dec[rs, hp:hp + 1],
                          psS[rs, hp * 256 + hh * 128:hp * 256 + (hh + 1) * 128], ALU.mult, ALU.add, [Sf, dec, psS], [Sf])
            P.cp("pool", Sfb[:], Sf[:], [Sf], [Sfb])
            osb, on, ob, st = osr.next(), onr.next(), obr.next(), str_.next()
            P.cp("act", osb[:], psO[:], [psO], [osb])
            P.tt("pool", sqj[:], osb[:], osb[:], ALU.mult, [osb], [sqj])
            P.op("dve", lambda: nc.vector.tensor_reduce(st[:, 0:4], sqj[:].rearrange("p (h e) -> p h e", h=4), AX.X, ALU.add),
                 [sqj], [st])
            rstd(P, st, st[:, 0:4], st[:, 0:4], 128, K["epsc"])
            o3 = osb[:].rearrange("p (h e) -> p h e", h=4)
            n3 = on[:].rearrange("p (h e) -> p h e", h=4)
            P.tt("dve", n3, o3, st[:, 0:4].unsqueeze(2).broadcast_to([128, 4, 128]), ALU.mult, [osb, st], [on])
            P.tt("pool", n3, n3, K["glag"][:].unsqueeze(1).broadcast_to([128, 4, 128]), ALU.mult, [on, K["glag"]], [on])
            P.tt("dve", ob[:], on[:], gr[:], ALU.mult, [on, gr], [ob])
            for h in range(4):
                P.tr(psT[:, h, :], ob[:, h * 128:(h + 1) * 128], K["ident_b"][:], [ob, K["ident_b"]], [psT])
            P.cp("act", yg[:, :, osl], psT[:], [psT], [yg])
            if off_ + 128 == tn_:
                P.dma("sp", X["YGT"][:, :, t0_:t0_ + tn_].rearrange("c p t -> p c t"), yg[:, :, 0:tn_], yg, reads=[yg])

        prev_a = None
        for c in range(NCH):
            cur_a = stage_a(c)
            if prev_a is not None:
                stage_b(prev_a)
            prev_a = cur_a
        stage_b(prev_a)


def phase6(P, I, X, L, last, lam_init, K):
    nc = P.nc
    with ExitStack() as es:
        WD = P.sb(es, [128, 8, D], BF16)
        WC = P.sb(es, [128, 4, D], BF16)
        WG = P.sb(es, [128, 4, D], BF16)
        WO = P.sb(es, [128, 8, D], BF16)
        for wt, nm in ((WD, "diff_w_out"), (WC, "conv_w_out"), (WG, "gla_w_out"), (WO, "w_o")):
            P.dma("pool", wt[:], I[nm][L].rearrange("(kc p) n -> p kc n", p=128), wt, writes=[wt])
        RW = P.sb(es, [128, 8, NE], F32)
        P.dma("sp", RW[:], I["router_w"][L].rearrange("(kc p) e -> p kc e", p=128), RW, writes=[RW])
        RB = P.sb(es, [128, NE], F32)
        P.dma("sp", RB[:], I["router_b"][L].partition_broadcast(128), RB, writes=[RB])
        nw = 1 if last else 2
        msl = [[P.sb(es, [128, D], F32) for _ in range(3)] for _ in range(nw)]
        for w in range(nw):
            for j, seg in enumerate((2, 3, 4)):
                P.dma("sp", msl[w][j][:], X["MODS"][w][:, seg * D:(seg + 1) * D], msl[w][j], writes=[msl[w][j]])
        dTr = Ring([P.sb(es, [128, 8, 512], BF16) for _ in range(2)])
        ycr = Ring([P.sb(es, [128, 4, 512], BF16) for _ in range(2)])
        ygr = Ring([P.sb(es, [128, 4, 512], BF16) for _ in range(2)])
        sgr = Ring([P.sb(es, [128, 3, 512], BF16) for _ in range(3)])
        mgr = Ring([P.sb(es, [128, 8, 512], BF16) for _ in range(1)])
        m1r = Ring([P.sb(es, [128, 512], F32) for _ in range(2)])
        m2r = Ring([P.sb(es, [128, 512], F32) for _ in range(2)])
        m3r = Ring([P.sb(es, [128, 512], F32) for _ in range(2)])
        xr = Ring([P.sb(es, [128, D], F32) for _ in range(2)])
        xnr = Ring([P.sb(es, [128, D], F32) for _ in range(1)])
        tmr = Ring([P.sb(es, [128, 512], F32) for _ in range(2)])
        hbr = Ring([P.sb(es, [128, D], F32) for _ in range(1)])
        hbbr = Ring([P.sb(es, [128, D], BF16) for _ in range(2)])
        scr_b = P.sb(es, [128, D], F32)
        str_ = Ring([P.sb(es, [128, 2], F32) for _ in range(2)])
        h32r = Ring([P.sb(es, [128, 8, 128], F32) for _ in range(1)])
        h2st = Ring([P.sb(es, [128, 8, 512], BF16) for _ in range(1)])
        gtst = Ring([P.sb(es, [128, 4, NE], F32) for _ in range(2)])
        lgr = Ring([P.sb(es, [128, NE], F32) for _ in range(2)])
        er = Ring([P.sb(es, [128, NE], F32) for _ in range(2)])
        mkr = Ring([P.sb(es, [128, NE], F32) for _ in range(2)])
        m8r = Ring([P.sb(es, [128, 16], F32) for _ in range(2)])
        psbr = Ring([P.ps(es, [128, 512]) for _ in range(3)])
        psor = Ring([P.ps(es, [128, 512]) for _ in range(2)])
        ptr = P.ps(es, [128, 8, 128], F32)
        pslg = P.ps(es, [128, 512])
        sig4 = X["SIGT"].rearrange("(b c) p t -> c p b t", b=3)
        tiles = TILES[1:] if last else TILES
        for (t0, n) in tiles:
            w = 1 if t0 == 0 else 0
            g1, sh2, G2 = msl[w]
            dT, yc, yg = dTr.next(), ycr.next(), ygr.next()
            P.dma("sp", dT[:, :, 0:n], X["DIFFT"][:, :, t0:t0 + n].rearrange("c p t -> p c t"), dT, writes=[dT])
            P.dma("sp", yc[:, :, 0:n], X["YCT"][:, :, t0:t0 + n].rearrange("c p t -> p c t"), yc, writes=[yc])
            P.dma("sp", yg[:, :, 0:n], X["YGT"][:, :, t0:t0 + n].rearrange("c p t -> p c t"), yg, writes=[yg])
            mg = mgr.next()
            for c in range(8):
                sg = sgr.next()
                P.dma("sp", sg[:, :, 0:n], sig4[c][:, :, t0:t0 + n], sg, writes=[sg])
                cs = slice(c * 128, (c + 1) * 128)
                pd, pc, pg = psbr.next(), psbr.next(), psbr.next()
                for kc in range(8):
                    P.mm(pd[:, 0:n], WD[:, kc, cs], dT[:, kc, 0:n], kc == 0, kc == 7, [WD, dT], [pd])
                for kc in range(4):
                    P.mm(pc[:, 0:n], WC[:, kc, cs], yc[:, kc, 0:n], kc == 0, kc == 3, [WC, yc], [pc])
                for kc in range(4):
                    P.mm(pg[:, 0:n], WG[:, kc, cs], yg[:, kc, 0:n], kc == 0, kc == 3, [WG, yg], [pg])
                m1, m2, m3 = m1r.next(), m2r.next(), m3r.next()
                P.tt("dve", m1[:, 0:n], pd[:, 0:n], sg[:, 0, 0:n], ALU.mult, [pd, sg], [m1])
                P.tt("dve", m2[:, 0:n], pc[:, 0:n], sg[:, 1, 0:n], ALU.mult, [pc, sg], [m2])
                P.tt("dve", m3[:, 0:n], pg[:, 0:n], sg[:, 2, 0:n], ALU.mult, [pg, sg], [m3])
                P.tt("pool", m1[:, 0:n], m1[:, 0:n], m2[:, 0:n], ALU.add, [m1, m2], [m1])
                P.tt("pool", mg[:, c, 0:n], m1[:, 0:n], m3[:, 0:n], ALU.add, [m1, m3], [mg])
            h2s = h2st.next()
            gts = gtst.next()
            stg6 = DBG.get("p6_stage", 99)
            if stg6 < 2:
                continue
            for s in range(n // 128):
                r0 = t0 + s * 128
                xt, xn = xr.next(), xnr.next()
                P.dma("sp", xt[:], xsrc(I, X, L, r0), xt, writes=[xt])
                for hf in range(2):
                    po = psor.next()
                    hs = slice(hf * 512, (hf + 1) * 512)
                    for kc in range(8):
                        P.mm(po[:], mg[:, kc, s * 128:(s + 1) * 128], WO[:, kc, hs], kc == 0, kc == 7, [mg, WO], [po])
                    tm = tmr.next()
                    P.tt("dve", tm[:], po[:], g1[:, hs], ALU.mult, [po, g1], [tm])
                    P.tt("pool", xt[:, hs], xt[:, hs], tm[:], ALU.add, [xt, tm], [xt])
                P.dma("sp", X["XR"][r0:r0 + 128, :], xt[:], xt, reads=[xt])
                if stg6 < 3:
                    continue
                hb, st = hbr.next(), str_.next()
                norm_mod(P, xt, G2, sh2, hb, scr_b, st, K["epsc"], xo=xn)
                hbb = hbbr.next()
                P.cp("act", hbb[:], hb[:], [hb], [hbb])
                P.dma("sp", X["H2M"][r0:r0 + 128, :], hbb[:], hbb, reads=[hbb])
                if stg6 < 3.3:
                    continue
                for kc in range(8):
                    P.mm(ptr[:, kc, :], hb[:, kc * 128:(kc + 1) * 128], K["ident_f"][:], True, True, [hb, K["ident_f"]], [ptr])
                if stg6 < 3.6:
                    continue
                h32 = h32r.next()
                P.cp("act", h32[:], ptr[:], [ptr], [h32])
                if stg6 < 3.8:
                    continue
                P.cp("act", h2s[:, :, s * 128:(s + 1) * 128], ptr[:], [ptr], [h2s])
                if stg6 < 4:
                    continue
                for kc in range(8):
                    P.mm(pslg[:, 0:NE], h32[:, kc, :], RW[:, kc, :], kc == 0, kc == 7, [h32, RW], [pslg])
                lg, e_, mk, m8 = lgr.next(), er.next(), mkr.next(), m8r.next()
                P.tt("dve", lg[:], pslg[:, 0:NE], RB[:], ALU.add, [pslg, RB], [lg])
                P.op("dve", lambda: nc.vector.max(m8[:, 0:8], lg[:]), [lg], [m8])
                P.ts("dve", m8[:, 8:9], m8[:, 0:1], -1.0, None, ALU.mult, None, [m8], [m8])
                P.ts("dve", mk[:], lg[:], m8[:, 3:4], None, ALU.is_ge, None, [lg, m8], [mk])
                P.act(e_[:], lg[:], AF.Exp, [lg, m8], [e_], bias=m8[:, 8:9])
                P.tt("dve", e_[:], e_[:], mk[:], ALU.mult, [e_, mk], [e_])
                P.op("dve", lambda: nc.vector.tensor_reduce(m8[:, 9:10], e_[:], AX.X, ALU.add), [e_], [m8])
                P.op("dve", lambda: nc.vector.reciprocal(m8[:, 10:11], m8[:, 9:10]), [m8], [m8])
                P.ts("dve", gts[:, s, :], e_[:], m8[:, 10:11], None, ALU.mult, None, [e_, m8], [gts])
            if stg6 < 4:
                continue
            P.dma("sp", X["H2T"][:, :, t0:t0 + n].rearrange("c p t -> p c t"), h2s[:, :, 0:n], h2s, reads=[h2s])
            P.dma("sp", X["GATES"][t0:t0 + n, :].rearrange("(s p) e -> p s e", p=128), gts[:, 0:n // 128, :], gts, reads=[gts])


def phase7(P, I, X, L, last, lam_init, K, yout):
    nc = P.nc
    with ExitStack() as es:
        nw = 1 if last else 2
        g2 = [P.sb(es, [128, D], F32) for _ in range(nw)]
        for w in range(nw):
            P.dma("sp", g2[w][:], X["MODS"][w][:, 5 * D:6 * D], g2[w], writes=[g2[w]])
        B1T = P.sb(es, [128, NE, 2, 8], F32)
        with nc.allow_non_contiguous_dma(reason="bias de-interleave"):
            for e in range(NE):
                for s_ in range(2):
                    P.dma("sp", B1T[:, e, s_, :], I["moe_b1"][L, e].rearrange("(j p s) -> s p j", p=128, s=2)[s_], B1T, writes=[B1T])
        B2 = P.sb(es, [NE, D], F32)
        P.dma("sp", B2[:], I["moe_b2"][L], B2, writes=[B2])
        FG = None
        if last:
            FG = P.sb(es, [128, D], F32)
            P.dma("sp", FG[:], I["final_norm_g"].partition_broadcast(128), FG, writes=[FG])
        acc = P.sb(es, [128, 8, D], F32)
        h2 = Ring([P.sb(es, [128, 8, 1024], BF16) for _ in range(1)])
        gtr = Ring([P.sb(es, [128, 8, NE], F32) for _ in range(1)])
        gT = Ring([P.sb(es, [NE, 128], F32) for _ in range(2)])
        w1r = Ring([P.sb(es, [128, 8, 512], BF16) for _ in range(5)])
        w2r = Ring([P.sb(es, [128, 8, 512], BF16) for _ in range(4)])
        actr = Ring([P.sb(es, [128, 8, 512], BF16) for _ in range(2)])
        B1S = P.sb(es, [128, NE, 8], F32)
        P.ts("dve", B1S[:], B1T[:, :, 0, :], 1.702, None, ALU.mult, None, [B1T], [B1S])
        P.ts("dve", B1T[:, :, 1, :], B1T[:, :, 1, :], 1.0, None, ALU.add, None, [B1T], [B1T])
        glr = Ring([P.sb(es, [128, 512], F32) for _ in range(2)])
        sgr = Ring([P.sb(es, [128, 512], F32) for _ in range(2)])
        lnr = Ring([P.sb(es, [128, 512], F32) for _ in range(2)])
        xr = Ring([P.sb(es, [128, D], F32) for _ in range(2)])
        scr_b = P.sb(es, [128, D], F32)
        str_ = Ring([P.sb(es, [128, 2], F32) for _ in range(2)])
        psg = Ring([P.ps(es, [128, 512]) for _ in range(2)])
        psl = Ring([P.ps(es, [128, 512]) for _ in range(2)])
        pso = Ring([P.ps(es, [128, 512]) for _ in range(3)])
        pst = P.ps(es, [128, 512])
        if last:
            tiles = [(C + 1024 * i, 1024) for i in range(4)]
        else:
            tiles = [(0, C)] + [(C + 1024 * i, 1024) for i in range(4)]
        w1v = I["moe_w1"][L].rearrange("e (kc p) n -> e p kc n", p=128)
        w2v = I["moe_w2"][L].rearrange("e (kc p) n -> e p kc n", p=128)
        for (t0, n) in tiles:
            w = 1 if t0 == 0 else 0
            nsub = n // 128
            h2t, gt = h2.next(), gtr.next()
            P.dma("sp", h2t[:, :, 0:n], X["H2T"][:, :, t0:t0 + n].rearrange("c p t -> p c t"), h2t, writes=[h2t])
            P.dma("sp", gt[:, 0:nsub, :], X["GATES"][t0:t0 + n, :].rearrange("(s p) e -> p s e", p=128), gt, writes=[gt])
            for s in range(nsub):
                P.mm(pst[0:NE, 0:128], gt[:, s, :], K["ident_f"][:], True, True, [gt, K["ident_f"]], [pst])
                g_t = gT.next()
                P.cp("act", g_t[:], pst[0:NE, 0:128], [pst], [g_t])
                for hf in range(2):
                    po = pso.next()
                    P.mm(po[:], g_t[:], B2[:, hf * 512:(hf + 1) * 512], True, True, [g_t, B2], [po])
                    P.cp("act", acc[:, s, hf * 512:(hf + 1) * 512], po[:], [po], [acc])
            W1, W2 = {}, {}

            def load_w1(e, q):
                t_ = w1r.next()
                P.dma("pool", t_[:], w1v[e][:, :, q * 512:(q + 1) * 512], t_, writes=[t_])
                W1[(e, q)] = t_

            def load_w2(e, hf):
                t_ = w2r.next()
                P.dma("pool", t_[:], w2v[e][:, :, hf * 512:(hf + 1) * 512], t_, writes=[t_])
                W2[(e, hf)] = t_

            for q in range(4):
                load_w1(0, q)
            for hf in range(2):
                load_w2(0, hf)
            subtiles = [(j0, min(512, n - j0)) for j0 in range(0, n, 512)]
            pending = [None]
            NEX = DBG.get("p7_experts", NE)
            SIGCAP = 1.0 / (1.0 + math.exp(-1.702 * 7.0))
            for e in range(NEX):
                for si, (j0, nj) in enumerate(subtiles):
                    pre = si == len(subtiles) - 1 and e + 1 < NEX
                    at = actr.next()

                    def mm1(ci):
                        q, gch = ci // 2, ci % 2
                        w1s = W1[(e, q)]
                        pg, pl = psg.next(), psl.next()
                        for sidx, pp in ((0, pg), (1, pl)):
                            for kc in range(8):
                                P.mm(pp[:, 0:nj], w1s[:, kc, gch * 256 + sidx:gch * 256 + 256:2],
                                     h2t[:, kc, j0:j0 + nj], kc == 0, kc == 7, [w1s, h2t], [pp])
                        if pre and gch == 1:
                            load_w1(e + 1, q)
                        return pg, pl

                    def post1(ci, pg, pl):
                        fc = ci
                        gl, sg, ln = glr.next(), sgr.next(), lnr.next()
                        if DBG.get("p7_old_swiglu"):
                            P.ts("dve", gl[:, 0:nj], pg[:, 0:nj], B1T[:, e, 0, fc:fc + 1], 7.0, ALU.add, ALU.min, [pg, B1T], [gl])
                            P.act(sg[:, 0:nj], gl[:, 0:nj], AF.Sigmoid, [gl], [sg], scale=1.702)
                            P.ts("dve", ln[:, 0:nj], pl[:, 0:nj], B1T[:, e, 1, fc:fc + 1], 8.0, ALU.add, ALU.min, [pl, B1T], [ln])
                            P.ts("dve", ln[:, 0:nj], ln[:, 0:nj], -6.0, None, ALU.max, None, [ln], [ln])
                            P.tt("dve", gl[:, 0:nj], gl[:, 0:nj], sg[:, 0:nj], ALU.mult, [gl, sg], [gl])
                            P.tt("dve", at[:, fc, 0:nj], gl[:, 0:nj], ln[:, 0:nj], ALU.mult, [gl, ln], [at])
                            return
                        P.ts("dve", gl[:, 0:nj], pg[:, 0:nj], B1T[:, e, 0, fc:fc + 1], 7.0, ALU.add, ALU.min, [pg, B1T], [gl])
                        P.act(sg[:, 0:nj], gl[:, 0:nj], AF.Sigmoid, [gl], [sg], scale=1.702)
                        P.ts("dve", ln[:, 0:nj], pl[:, 0:nj], B1T[:, e, 1, fc:fc + 1], 8.0, ALU.add, ALU.min, [pl, B1T], [ln])
                        if DBG.get("p7_nofuse"):
                            P.ts("dve", sg[:, 0:nj], sg[:, 0:nj], SIGCAP, None, ALU.min, None, [sg], [sg])
                            P.tt("dve", gl[:, 0:nj], sg[:, 0:nj], gl[:, 0:nj], ALU.mult, [sg, gl], [gl])
                            P.ts("dve", ln[:, 0:nj], ln[:, 0:nj], -6.0, None, ALU.max, None, [ln], [ln])
                            P.tt("dve", at[:, fc, 0:nj], ln[:, 0:nj], gl[:, 0:nj], ALU.mult, [ln, gl], [at])
                            return
                        P.stt("dve", gl[:, 0:nj], sg[:, 0:nj], SIGCAP, gl[:, 0:nj], ALU.min, ALU.mult, [sg, gl], [gl])
                        P.stt("dve", at[:, fc, 0:nj], ln[:, 0:nj], -6.0, gl[:, 0:nj], ALU.max, ALU.mult, [ln, gl], [at])

                    def make_ffn2(e_, j0_, nj_, at_):
                        def run():
                            groups = [(s_, hf_) for s_ in range(nj_ // 128) for hf_ in range(2)]

                            def mm2(g):
                                s_, hf_ = groups[g]
                                po = pso.next()
                                w2s = W2[(e_, hf_)]
                                for fc in range(8):
                                    P.mm(po[:], at_[:, fc, s_ * 128:(s_ + 1) * 128], w2s[:, fc, :], fc == 0, fc == 7, [at_, w2s], [po])
                                return po

                            def post2(g, po):
                                s_, hf_ = groups[g]
                                sidx = j0_ // 128 + s_
                                asl = acc[:, sidx, hf_ * 512:(hf_ + 1) * 512]
                                P.stt("dve", asl, po[:], gt[:, sidx, e_:e_ + 1], asl, ALU.mult, ALU.add, [po, gt, acc], [acc])

                            prev = None
                            for g in range(len(groups)):
                                cur = mm2(g)
                                if prev is not None:
                                    post2(g - 1, prev)
                                prev = cur
                            post2(len(groups) - 1, prev)
                        return run

                    prev = None
                    for ci in range(8):
                        cur = mm1(ci)
                        if prev is not None:
                            post1(ci - 1, *prev)
                        prev = cur
                        if ci == 1 and pending[0] is not None:
                            pending[0]()
                            pending[0] = None
                    post1(7, *prev)
                    if pre:
                        load_w2(e + 1, 0)
                        load_w2(e + 1, 1)
                    pending[0] = make_ffn2(e, j0, nj, at)
            if pending[0] is not None:
                pending[0]()
                pending[0] = None
            for s in range(nsub):
                r0 = t0 + s * 128
                xt = xr.next()
                P.dma("sp", xt[:], X["XR"][r0:r0 + 128, :], xt, writes=[xt])
                P.tt("dve", acc[:, s, :], acc[:, s, :], g2[w][:], ALU.mult, [acc, g2[w]], [acc])
                P.tt("pool", xt[:], xt[:], acc[:, s, :], ALU.add, [xt, acc], [xt])
                if not last:
                    P.dma("sp", X["XR"][r0:r0 + 128, :], xt[:], xt, reads=[xt])
                else:
                    st = str_.next()
                    sumsq(P, scr_b, xt, st)
                    rstd(P, st, st[:, 1:2], st[:, 0:1], D, K["epsc"])
                    P.stt("dve", xt[:], xt[:], st[:, 1:2], FG[:], ALU.mult, ALU.mult, [xt, st, FG], [xt])
                    P.dma("sp", yout[r0 - C:r0 - C + 128, :], xt[:], xt, reads=[xt])


def phase7s(P, I, X, L, last, lam_init, K, yout):
    nc = P.nc
    CAP = MOE_CAP
    with ExitStack() as es:
        nw = 1 if last else 2
        g2 = [P.sb(es, [128, D], F32) for _ in range(nw)]
        for w in range(nw):
            P.dma("sp", g2[w][:], X["MODS"][w][:, 5 * D:6 * D], g2[w], writes=[g2[w]])
        B1T = P.sb(es, [128, NE, 2, 8], F32)
        with nc.allow_non_contiguous_dma(reason="bias de-interleave"):
            for e in range(NE):
                for s_ in range(2):
                    P.dma("sp", B1T[:, e, s_, :], I["moe_b1"][L, e].rearrange("(j p s) -> s p j", p=128, s=2)[s_], B1T, writes=[B1T])
        B2 = P.sb(es, [NE, D], F32)
        P.dma("sp", B2[:], I["moe_b2"][L], B2, writes=[B2])
        FG = None
        if last:
            FG = P.sb(es, [128, D], F32)
            P.dma("sp", FG[:], I["final_norm_g"].partition_broadcast(128), FG, writes=[FG])
        trif = P.sb(es, [128, 128], F32)
        P.dma("sp", trif[:], I["k_tri"][3], trif, writes=[trif])
        trib = P.sb(es, [128, 128], BF16)
        P.ts("dve", trib[:], trif[:], -16.0, None, ALU.mult, None, [trif], [trib])
        oneb = P.sb(es, [128, 128], BF16)
        P.memset("dve", oneb, oneb[:], 1.0)
        iot = P.sb(es, [128, CAP], F32)
        P.dma("sp", iot[:], I["k_iota"].partition_broadcast(128), iot, writes=[iot])

        acc = P.sb(es, [128, 8, D], F32)
        h2m = P.sb(es, [128, 8, D], BF16)
        gt = P.sb(es, [128, 8, NE], F32)
        mkf = P.sb(es, [128, 8, NE], F32)
        mkb = P.sb(es, [128, 8, NE], BF16)
        pos = P.sb(es, [128, 8, NE], F32)
        gT = Ring([P.sb(es, [NE, 128], F32) for _ in range(2)])
        selr = Ring([P.sb(es, [128, 8, CAP], BF16) for _ in range(2)])
        selgr = Ring([P.sb(es, [128, 8, CAP], BF16) for _ in range(2)])
        xer = Ring([P.sb(es, [128, 8, CAP], BF16) for _ in range(1)])
        atr_ = Ring([P.sb(es, [128, 8, CAP], BF16) for _ in range(1)])
        yer = Ring([P.sb(es, [128, 2, D], BF16) for _ in range(1)])
        sgtr = Ring([P.sb(es, [128, 2, 8, 128], BF16) for _ in range(1)])
        w1r = Ring([P.sb(es, [128, 8, 512], BF16) for _ in range(5)])
        w2r = Ring([P.sb(es, [128, 8, 512], BF16) for _ in range(4)])
        glr = Ring([P.sb(es, [128, CAP], F32) for _ in range(2)])
        sgr = Ring([P.sb(es, [128, CAP], F32) for _ in range(2)])
        lnr = Ring([P.sb(es, [128, CAP], F32) for _ in range(2)])
        xr = Ring([P.sb(es, [128, D], F32) for _ in range(1)])
        scr_b = P.sb(es, [128, D], F32)
        str_ = Ring([P.sb(es, [128, 2], F32) for _ in range(2)])
        psa = Ring([P.ps(es, [128, 512]) for _ in range(3)])
        psb = Ring([P.ps(es, [128, 512]) for _ in range(3)])
        pstrr = Ring([P.ps(es, [128, 4, 128], BF16) for _ in range(2)])
        if last:
            tiles = [(C + 1024 * i, 1024) for i in range(4)]
        else:
            tiles = [(0, C)] + [(C + 1024 * i, 1024) for i in range(4)]
        w1v = I["moe_w1"][L].rearrange("e (kc p) n -> e p kc n", p=128)
        w2v = I["moe_w2"][L].rearrange("e (kc p) n -> e p kc n", p=128)
        for (t0, n) in tiles:
            w = 1 if t0 == 0 else 0
            nsub = n // 128
            P.dma("sp", h2m[:, 0:nsub, :], X["H2M"][t0:t0 + n, :].rearrange("(s p) d -> p s d", p=128), h2m, writes=[h2m])
            P.dma("sp", gt[:, 0:nsub, :], X["GATES"][t0:t0 + n, :].rearrange("(s p) e -> p s e", p=128), gt, writes=[gt])
            P.ts("dve", mkf[:, 0:nsub, :], gt[:, 0:nsub, :], 0.0, None, ALU.is_gt, None, [gt], [mkf])
            P.cp("dve", mkb[:, 0:nsub, :], mkf[:, 0:nsub, :], [mkf], [mkb])
            for s in range(nsub):
                pp = psb.next()
                for s2 in range(s):
                    P.mm(pp[:, 0:NE], oneb[:], mkb[:, s2, :], s2 == 0, False, [oneb, mkb], [pp])
                P.mm(pp[:, 0:NE], trib[:], mkb[:, s, :], s == 0, True, [trib, mkb], [pp])
                P.cp("act", pos[:, s, :], pp[:, 0:NE], [pp], [pos])
            for s in range(nsub):
                pq = psb.next()
                P.mm(pq[0:NE, 0:128], gt[:, s, :], K["ident_f"][:], True, True, [gt, K["ident_f"]], [pq])
                g_t = gT.next()
                P.cp("act", g_t[:], pq[0:NE, 0:128], [pq], [g_t])
                for hf in range(2):
                    po = psb.next()
                    P.mm(po[:], g_t[:], B2[:, hf * 512:(hf + 1) * 512], True, True, [g_t, B2], [po])
                    P.cp("act", acc[:, s, hf * 512:(hf + 1) * 512], po[:], [po], [acc])
            W1, W2 = {}, {}

            def load_w1(e, q):
                t_ = w1r.next()
                P.dma("pool", t_[:], w1v[e][:, :, q * 512:(q + 1) * 512], t_, writes=[t_])
                W1[(e, q)] = t_

            def load_w2(e, hf):
                t_ = w2r.next()
                P.dma("pool", t_[:], w2v[e][:, :, hf * 512:(hf + 1) * 512], t_, writes=[t_])
                W2[(e, hf)] = t_

            for q in range(4):
                load_w1(0, q)
            for hf in range(2):
                load_w2(0, hf)
            for e in range(NE):
                pre = e + 1 < NE
                sel, selg = selr.next(), selgr.next()
                for s in range(nsub):
                    P.ts("dve", sel[:, s, :], iot[:], pos[:, s, e:e + 1], mkf[:, s, e:e + 1], ALU.is_equal, ALU.mult,
                         [iot, pos, mkf], [sel])
                    P.ts("dve", selg[:, s, :], iot[:], pos[:, s, e:e + 1], gt[:, s, e:e + 1], ALU.is_equal, ALU.mult,
                         [iot, pos, gt], [selg])
                xe = xer.next()
                for dc in range(8):
                    pg = psa.next()
                    for s in range(nsub):
                        P.mm(pg[:, 0:CAP], h2m[:, s, dc * 128:(dc + 1) * 128], sel[:, s, :], s == 0, s == nsub - 1,
                             [h2m, sel], [pg])
                    P.cp("act", xe[:, dc, :], pg[:, 0:CAP], [pg], [xe])
                sgt = sgtr.next()
                for s0 in range(0, nsub, 2):
                    pstr = pstrr.next()
                    for sl in range(2):
                        for jg in range(2):
                            P.tr(pstr[:, sl * 2 + jg, :], selg[:, s0 + sl, jg * 128:(jg + 1) * 128], K["ident_b"][:],
                                 [selg, K["ident_b"]], [pstr])
                    P.cp("act", sgt[:, :, s0:s0 + 2, :], pstr[:].rearrange("p (sl jg) t -> p jg sl t", jg=2), [pstr], [sgt])
                at = atr_.next()
                for q in range(4):
                    w1s = W1[(e, q)]
                    for gch in range(2):
                        fc = q * 2 + gch
                        pg, pl = psa.next(), psa.next()
                        for sidx, pp in ((0, pg), (1, pl)):
                            for kc in range(8):
                                P.mm(pp[:, 0:CAP], w1s[:, kc, gch * 256 + sidx:gch * 256 + 256:2], xe[:, kc, :],
                                     kc == 0, kc == 7, [w1s, xe], [pp])
                        gl, sg, ln = glr.next(), sgr.next(), lnr.next()
                        P.ts("dve", gl[:], pg[:, 0:CAP], B1T[:, e, 0, fc:fc + 1], 7.0, ALU.add, ALU.min, [pg, B1T], [gl])
                        P.act(sg[:], gl[:], AF.Sigmoid, [gl], [sg], scale=1.702)
                        P.ts("dve", ln[:], pl[:, 0:CAP], B1T[:, e, 1, fc:fc + 1], 7.0, ALU.add, ALU.min, [pl, B1T], [ln])
                        P.ts("dve", ln[:], ln[:], -7.0, 1.0, ALU.max, ALU.add, [ln], [ln])
                        P.tt("dve", gl[:], gl[:], sg[:], ALU.mult, [gl, sg], [gl])
                        P.tt("dve", at[:, fc, :], gl[:], ln[:], ALU.mult, [gl, ln], [at])
                    if pre:
                        load_w1(e + 1, q)
                if pre:
                    load_w2(e + 1, 0)
                    load_w2(e + 1, 1)
                ye = yer.next()
                for jg in range(2):
                    for hf in range(2):
                        po = psb.next()
                        w2s = W2[(e, hf)]
                        for fc in range(8):
                            P.mm(po[:], at[:, fc, jg * 128:(jg + 1) * 128], w2s[:, fc, :], fc == 0, fc == 7, [at, w2s], [po])
                        P.cp("act", ye[:, jg, hf * 512:(hf + 1) * 512], po[:], [po], [ye])
                for s in range(nsub):
                    for hf in range(2):
                        po = psb.next()
                        for jg in range(2):
                            P.mm(po[:], sgt[:, jg, s, :], ye[:, jg, hf * 512:(hf + 1) * 512], jg == 0, jg == 1, [sgt, ye], [po])
                        asl = acc[:, s, hf * 512:(hf + 1) * 512]
                        P.tt("dve", asl, po[:], asl, ALU.add, [po, acc], [acc])
            for s in range(nsub):
                r0 = t0 + s * 128
                xt = xr.next()
                P.dma("sp", xt[:], X["XR"][r0:r0 + 128, :], xt, writes=[xt])
                P.tt("dve", acc[:, s, :], acc[:, s, :], g2[w][:], ALU.mult, [acc, g2[w]], [acc])
                P.tt("dve", xt[:], xt[:], acc[:, s, :], ALU.add, [xt, acc], [xt])
                if not last:
                    P.dma("sp", X["XR"][r0:r0 + 128, :], xt[:], xt, reads=[xt])
                else:
                    st = str_.next()
                    sumsq(P, scr_b, xt, st)
                    rstd(P, st, st[:, 1:2], st[:, 0:1], D, K["epsc"])
                    P.stt("dve", xt[:], xt[:], st[:, 1:2], FG[:], ALU.mult, ALU.mult, [xt, st, FG], [xt])
                    P.dma("sp", yout[r0 - C:r0 - C + 128, :], xt[:], xt, reads=[xt])


def host_consts():
    inv_freq = (10000.0 ** (-np.arange(0, 32, 2, dtype=np.float32) / 32.0)).astype(np.float32)
    pos = np.arange(S)
    row = (pos // 64).astype(np.float32)
    col = (pos % 64).astype(np.float32)
    ang = np.concatenate([row[:, None] * inv_freq[None, :], col[:, None] * inv_freq[None, :]], axis=1).astype(np.float32)
    m = np.arange(128)
    tri = np.zeros((6, 128, 128), np.float32)
    tri[0] = (m[:, None] <= m[None, :]) * (-1.0 / 16.0)
    tri[1] = (m[:, None] >= m[None, :]) * (-1.0 / 16.0)
    tri[2] = (m[:, None] > m[None, :]) * (-1.0 / 16.0)
    tri[3] = (m[:, None] < m[None, :]) * (-1.0 / 16.0)
    tri[4] = (m[:, None] <= m[None, :]) * 1.0
    tri[5] = (m[:, None] >= m[None, :]) * 1.0
    return dict(k_cos=np.cos(ang).astype(np.float32), k_sin=np.sin(ang).astype(np.float32),
                k_ident=np.eye(128, dtype=np.float32), k_tri=tri, k_iota=np.arange(MOE_CAP, dtype=np.float32))


def make_in_maps(inputs, cores, used=None):
    kc = host_consts()
    f = lambda a: np.ascontiguousarray(np.asarray(a, dtype=np.float32))
    shared = {k: f(inputs[k]) for k in ("c_ctx", "ada_w", "ada_b", "norm1_g", "norm2_g", "w_in", "diff_subln_g",
                                        "diff_w_out", "conv_w", "conv_w_out", "gla_w_a2", "gla_norm_g", "gla_w_out",
                                        "w_o", "router_w", "router_b", "moe_w1", "moe_b1", "moe_w2", "moe_b2",
                                        "final_norm_g")}
    shared["diff_lambda"] = f(inputs["diff_lambda"]).reshape(DEPTH, 256)
    shared["gla_b_a"] = f(inputs["gla_b_a"]).reshape(DEPTH, 512)
    shared.update(kc)
    maps = []
    for b in cores:
        m = dict(shared)
        m["x"] = f(inputs["x"][b])
        m["c"] = f(inputs["c"][b])
        m["ctx"] = f(inputs["ctx"][b])
        if used is not None:
            m = {k: v for k, v in m.items() if k in used}
        maps.append(m)
    return maps


def kernel(**inputs):
    nc = build()
    maps = make_in_maps(inputs, list(range(8)))
    res = run_bass_kernel_spmd(nc, maps, core_ids=list(range(8)))
    return np.stack([np.asarray(r["y"], dtype=np.float32) for r in res.results], axis=0)
```

```python
import math
from contextlib import ExitStack
import numpy as np
import concourse.bass as bass
import concourse.mybir as mybir
from concourse.bass_utils import run_bass_kernel_spmd

F32 = mybir.dt.float32
BF16 = mybir.dt.bfloat16
AF = mybir.ActivationFunctionType
ALU = mybir.AluOpType
AX = mybir.AxisListType

D = 1024
S = 4096
C = 256
T = S + C
NT = T // 128
DEPTH = 2
NE = 32
WIN = 9248
EPS = 1e-6
OQ, OK_, OV = 0, 1024, 2048
OCB, OCC, OCX = 3072, 3584, 4096
OGQ, OGK, OGV, OGR, OGA = 4608, 4864, 5120, 5632, 6144
OGT = 6176
TILES = [(0, 256)] + [(256 + 512 * i, 512) for i in range(8)]


DBG = {}


class Dep:
    def __init__(self):
        self.w = None
        self.r = {}
        self.ds = None


class TL(Dep):
    def __init__(self, h):
        super().__init__()
        self.h = h

    def __getitem__(self, k):
        return self.h[k]


class DSem:
    def __init__(self, sem):
        self.sem = sem
        self.cnt = 0


class Prog:
    def __init__(self, nc, ndsem=96):
        self.nc = nc
        self.E = {"pe": nc.tensor, "act": nc.scalar, "dve": nc.vector, "pool": nc.gpsimd, "sp": nc.sync}
        self.sem = {k: nc.alloc_semaphore("s_" + k) for k in self.E}
        self.cnt = {k: 0 for k in self.E}
        self.seen = {k: {} for k in self.E}
        self.dpool = [DSem(nc.alloc_semaphore("d%d" % i)) for i in range(ndsem)]
        self.dnext = 0
        self.persist = 0
        self.nsb = 0
        self.pe_cols = [0]
        self.dly = {}
        self.nfence = 0

    def sb(self, es, shape, dt, name=None):
        self.nsb += 1
        h = es.enter_context(self.nc.sbuf_tensor("t%d" % self.nsb, list(shape), dt))
        return TL(h)

    def ps(self, es, shape, dt=F32):
        self.nsb += 1
        h = es.enter_context(self.nc.psum_tensor("p%d" % self.nsb, list(shape), dt))
        return TL(h)

    def _ds(self, t):
        if t.ds is None:
            assert self.dnext < len(self.dpool), "out of dma semaphores"
            t.ds = self.dpool[self.dnext]
            self.dnext += 1
        return t.ds

    def _wait(self, eng, ev, raw=False):
        if ev is None:
            return
        if ev[0] == "e":
            _, src, val = ev
            if src == eng and (eng == "pe" or not raw):
                return
            key = src
            sem = self.sem[src]
            if src == "pe":
                need = self.pe_cols[val] + 256
                k2 = val
                while k2 < self.cnt["pe"] and self.pe_cols[k2] < need:
                    k2 += 1
                if self.pe_cols[k2] >= need:
                    val = k2
                else:
                    val = self.cnt["pe"]
                    if self.seen[eng].get(key, 0) < val:
                        self.E[eng].wait_ge(sem, val)
                        self.seen[eng][key] = val
                    if self.seen[eng].get("pe_safe", 0) < val:
                        self._delay(eng)
                        self.seen[eng]["pe_safe"] = val
                    return
                if self.seen[eng].get("pe_safe", 0) < val:
                    self.seen[eng]["pe_safe"] = val
        else:
            ds = ev[1]
            key = id(ds)
            sem = ds.sem
            val = ds.cnt
        if self.seen[eng].get(key, 0) >= val:
            return
        self.E[eng].wait_ge(sem, val)
        self.seen[eng][key] = val

    def _delay(self, eng):
        if eng not in self.dly:
            return
        self.nfence += 1
        d = self.dly[eng]
        if eng == "act":
            self.nc.scalar.copy(d[:, 0:256], d[:, 256:512])
        else:
            self.E[eng].memset(d[:, 0:256], 0.0)

    def _deps(self, eng, reads, writes):
        for t in reads:
            self._wait(eng, t.w, raw=True)
        for t in writes:
            self._wait(eng, t.w)
            for ev in list(t.r.values()):
                self._wait(eng, ev)

    def _mark(self, ev, key, reads, writes):
        for t in reads:
            t.r[key] = ev
        for t in writes:
            t.w = ev
            t.r = {}

    def op(self, eng, ins_fn, reads=(), writes=(), pe_n=None):
        self._deps(eng, reads, writes)
        ins = ins_fn()
        self.cnt[eng] += 1
        if eng == "pe":
            if pe_n is None:
                try:
                    pe_n = int(ins.ins.outs[0].free_size()) if False else 128
                except Exception:
                    pe_n = 128
            self.pe_cols.append(self.pe_cols[-1] + pe_n)
        ins.then_inc(self.sem[eng], 1)
        self._mark(("e", eng, self.cnt[eng]), eng, reads, writes)
        return ins

    def dma(self, q, out, in_, holder, reads=(), writes=(), **kw):
        self._deps(q, reads, writes)
        ds = self._ds(holder)
        ins = self.E[q].dma_start(out=out, in_=in_, **kw)
        ins.then_inc(ds.sem, 16)
        ds.cnt += 16
        self._mark(("d", ds), id(ds), reads, writes)

    def barrier(self):
        for ds in self.dpool[: self.dnext]:
            if ds.cnt > 0:
                self._wait("sp", ("d", ds))
        for e in self.E:
            if e != "sp":
                self._wait("sp", ("e", e, self.cnt[e]))
        self.E["sp"].sem_inc(self.sem["sp"], 1)
        self.cnt["sp"] += 1
        for e in self.E:
            if e == "sp":
                continue
            for o in self.E:
                if o != e:
                    self._wait(e, ("e", o, self.cnt[o]))
        self.dnext = self.persist

    def rep(self, name):
        print("SBUF remaining after", name, self.nc.sbuf_bytes_remaining, flush=True)

    def persist_dsems(self):
        self.persist = self.dnext

    def mm(self, out, lhsT, rhs, start, stop, reads, writes):
        n = 1
        for d_ in rhs.shape[1:]:
            n *= int(d_)
        return self.op("pe", lambda: self.nc.tensor.matmul(out, lhsT, rhs, start=start, stop=stop), reads, writes, pe_n=n)

    def tr(self, out, in_, ident, reads, writes):
        return self.op("pe", lambda: self.nc.tensor.transpose(out, in_, ident), reads, writes, pe_n=64)

    def act(self, out, in_, func, reads, writes, **kw):
        return self.op("act", lambda: self.nc.scalar.activation(out=out, in_=in_, func=func, **kw), reads, writes)

    def ts(self, eng, out, in0, s1, s2, op0, op1, reads, writes):
        eng = self.cmap(eng)
        e = self.E[eng]
        if op1 is None:
            return self.op(eng, lambda: e.tensor_scalar(out, in0, s1, None, op0), reads, writes)
        return self.op(eng, lambda: e.tensor_scalar(out, in0, s1, s2, op0, op1), reads, writes)

    def tt(self, eng, out, in0, in1, op, reads, writes):
        eng = self.cmap(eng)
        e = self.E[eng]
        return self.op(eng, lambda: e.tensor_tensor(out, in0, in1, op), reads, writes)

    def stt(self, eng, out, in0, scalar, in1, op0, op1, reads, writes):
        eng = self.cmap(eng)
        e = self.E[eng]
        return self.op(eng, lambda: e.scalar_tensor_tensor(out, in0, scalar, in1, op0, op1), reads, writes)

    def cp(self, eng, out, in_, reads, writes):
        eng = self.cmap(eng)
        if eng == "act":
            return self.op("act", lambda: self.nc.scalar.copy(out, in_), reads, writes)
        e = self.E[eng]
        return self.op(eng, lambda: e.tensor_copy(out, in_), reads, writes)

    def cmap(self, eng):
        return "dve" if (eng == "pool" and not DBG.get("pool_compute", False)) else eng

    def memset(self, eng, t, ap, val):
        eng = self.cmap(eng)
        e = self.E[eng]
        return self.op(eng, lambda: e.memset(ap, val), (), (t,))


class Ring:
    def __init__(self, tiles):
        self.t = tiles
        self.i = 0

    def next(self):
        t = self.t[self.i % len(self.t)]
        self.i += 1
        return t


def build(n_layers=DEPTH, debug_out=(), stop_after=None):
    nc = bass.Bass("TRN2", target_bir_lowering=False)
    P = Prog(nc)

    def din(name, shape, dt=F32):
        return nc.dram_tensor(name, list(shape), dt, kind="ExternalInput").ap()

    SHAPES = dict(x=[S, D], c=[D], ctx=[C, D], c_ctx=[D], ada_w=[DEPTH, D, 6 * D], ada_b=[DEPTH, 6 * D],
                  norm1_g=[DEPTH, D], norm2_g=[DEPTH, D], w_in=[DEPTH, D, WIN], diff_lambda=[DEPTH, 256],
                  diff_subln_g=[DEPTH, 128], diff_w_out=[DEPTH, D, D], conv_w=[DEPTH, 3, 512],
                  conv_w_out=[DEPTH, 512, D], gla_w_a2=[DEPTH, 2, 16, 256], gla_b_a=[DEPTH, 512],
                  gla_norm_g=[DEPTH, 128], gla_w_out=[DEPTH, 512, D], w_o=[DEPTH, D, D], router_w=[DEPTH, D, NE],
                  router_b=[DEPTH, NE], moe_w1=[DEPTH, NE, D, 2 * D], moe_b1=[DEPTH, NE, 2 * D],
                  moe_w2=[DEPTH, NE, D, D], moe_b2=[DEPTH, NE, D], final_norm_g=[D],
                  k_cos=[S, 32], k_sin=[S, 32], k_ident=[128, 128], k_tri=[6, 128, 128])

    class LazyIn(dict):
        def __missing__(self, k):
            self[k] = din(k, SHAPES[k])
            return self[k]

    I = LazyIn()
    if stop_after is None:
        for k in SHAPES:
            I[k]
    yout = nc.dram_tensor("y", [S, D], F32, kind="ExternalOutput").ap()

    def scr(name, shape, dt=F32):
        kind = "ExternalOutput" if name in debug_out else "Internal"
        return nc.dram_tensor(name, list(shape), dt, kind=kind).ap()

    X = {}
    X["XR"] = scr("XR", [T, D])
    X["MODS"] = scr("MODS", [2, 128, 6 * D])
    X["QT"] = scr("QT", [8, 128, T], BF16)
    X["KT"] = scr("KT", [8, 128, T], BF16)
    X["V"] = scr("V", [8, 128, NT * 132], BF16)
    X["CBT"] = scr("CBT", [4, 128, T])
    X["CCT"] = scr("CCT", [4, 128, T])
    X["CXT"] = scr("CXT", [4, 128, T])
    X["GQT"] = scr("GQT", [2, 128, T])
    X["GKT"] = scr("GKT", [2, 128, T])
    X["GK"] = scr("GK", [T, 256])
    X["GV"] = scr("GV", [T, 512], BF16)
    X["GR"] = scr("GR", [T, 512])
    X["GAF"] = scr("GAF", [16, T])
    X["GAB"] = scr("GAB", [16, T])
    X["SIGT"] = scr("SIGT", [24, 128, T], BF16)
    X["DIFFT"] = scr("DIFFT", [8, 128, T], BF16)
    X["YCT"] = scr("YCT", [4, 128, T], BF16)
    X["YGT"] = scr("YGT", [4, 128, T], BF16)
    X["H2T"] = scr("H2T", [8, 128, T], BF16)
    X["GATES"] = scr("GATES", [T, NE])
    X["H2M"] = scr("H2M", [T, D], BF16)
    if "HT" in debug_out:
        X["HT"] = scr("HT", [8, 128, T], BF16)

    with ExitStack() as gs:
        ident_f = P.sb(gs, [128, 128], F32)
        ident_b = P.sb(gs, [128, 128], BF16)
        ones_f = P.sb(gs, [128, 128], F32)
        lam = P.sb(gs, [128, 4], F32)
        subg = P.sb(gs, [128, 128], F32)
        glag = P.sb(gs, [128, 128], F32)
        P.dma("sp", ident_f[:], I["k_ident"], ident_f, writes=[ident_f])
        P.cp("dve", ident_b[:], ident_f[:], [ident_f], [ident_b])
        P.memset("dve", ones_f, ones_f[:], 1.0)
        for e_ in ("act", "dve"):
            P.dly[e_] = P.sb(gs, [128, 512], F32)
            P.memset("dve", P.dly[e_], P.dly[e_][:], 0.0)
        epsc = P.sb(gs, [128, 1], F32)
        P.memset("dve", epsc, epsc[:], EPS)
        P.persist_dsems()
        P.barrier()

        for L in range(n_layers):
            last = L == n_layers - 1 and n_layers == DEPTH
            lam_init = 0.8 - 0.6 * math.exp(-0.3 * L)
            phases = [phase0, phase1_2, phase3, phase4, phase5, phase6, phase7]
            for ph in phases:
                kk = dict(ident_f=ident_f, ident_b=ident_b, ones_f=ones_f, lam=lam, subg=subg, glag=glag, epsc=epsc)
                if ph is phase7:
                    ph(P, I, X, L, last, lam_init, kk, yout)
                else:
                    ph(P, I, X, L, last, lam_init, kk)
                P.barrier()
                if stop_after == (L, ph.__name__):
                    break
            else:
                continue
            break
        P.barrier()
    nc._used_inputs = set(I.keys())
    return nc


def phase0(P, I, X, L, last, lam_init, K):
    nc = P.nc
    with ExitStack() as es:
        cs = P.sb(es, [128, 8, 2], F32)
        crep = [P.sb(es, [128, 8, 128], BF16) for _ in range(2)]
        mod = [P.sb(es, [128, 6 * D], F32) for _ in range(2)]
        grep = [P.sb(es, [128, D], F32) for _ in range(2)]
        wring = Ring([P.sb(es, [128, 8, 512], BF16) for _ in range(2)])
        pss = Ring([P.ps(es, [128, 512]) for _ in range(4)])
        dl = P.sb(es, [128, 256], F32)
        tmp = P.sb(es, [128, 256], F32)

        with nc.allow_non_contiguous_dma(reason="tiny transposed vector load"):
            P.dma("sp", cs[:, :, 0], I["c"].rearrange("(kc p) -> p kc", p=128), cs, writes=[cs])
            P.dma("sp", cs[:, :, 1], I["c_ctx"].rearrange("(kc p) -> p kc", p=128), cs, writes=[cs])
        P.act(cs[:], cs[:], AF.Silu, [cs], [cs])
        for w in range(2):
            for kc in range(8):
                P.ts("dve", crep[w][:, kc, :], K["ones_f"][:], cs[:, kc, w:w + 1], None, ALU.mult, None,
                     [cs, K["ones_f"]], [crep[w]])
            P.dma("sp", mod[w][:], I["ada_b"][L].partition_broadcast(128), mod[w], writes=[mod[w]])
        P.dma("sp", grep[0][:], I["norm1_g"][L].partition_broadcast(128), grep[0], writes=[grep[0]])
        P.dma("sp", grep[1][:], I["norm2_g"][L].partition_broadcast(128), grep[1], writes=[grep[1]])
        aw = I["ada_w"][L].rearrange("(kc p) n -> p kc n", p=128)
        for cb in range(12):
            wt = wring.next()
            P.dma("pool", wt[:], aw[:, :, cb * 512:(cb + 1) * 512], wt, writes=[wt])
            for w in range(2):
                ps = pss.next()
                for kc in range(8):
                    P.mm(ps[:], crep[w][:, kc, :], wt[:, kc, :], kc == 0, kc == 7, [crep[w], wt], [ps])
                sl = mod[w][:, cb * 512:(cb + 1) * 512]
                P.tt("dve", sl, ps[:], sl, ALU.add, [ps, mod[w]], [mod[w]])
        for w in range(2):
            for seg, g in ((1, grep[0]), (4, grep[1])):
                sl = mod[w][:, seg * D:(seg + 1) * D]
                P.stt("dve", sl, sl, 1.0, g[:], ALU.add, ALU.mult, [mod[w], g], [mod[w]])
            P.dma("sp", X["MODS"][w], mod[w][:], mod[w], reads=[mod[w]])
        lamt = K["lam"]
        P.dma("sp", dl[:], I["diff_lambda"][L].partition_broadcast(128), dl, writes=[dl])
        P.tt("dve", tmp[:, 0:64], dl[:, 0:64], dl[:, 64:128], ALU.mult, [dl], [tmp])
        P.tt("dve", tmp[:, 64:128], dl[:, 128:192], dl[:, 192:256], ALU.mult, [dl], [tmp])
        P.op("dve", lambda: nc.vector.tensor_reduce(lamt[:, 1:3], tmp[:, 0:128].rearrange("p (a b) -> p a b", a=2),
                                                    AX.X, ALU.add), [tmp], [lamt])
        P.act(lamt[:, 1:3], lamt[:, 1:3], AF.Exp, [lamt], [lamt])
        P.tt("dve", lamt[:, 0:1], lamt[:, 1:2], lamt[:, 2:3], ALU.subtract, [lamt], [lamt])
        P.ts("dve", lamt[:, 0:1], lamt[:, 0:1], float(lam_init), None, ALU.add, None, [lamt], [lamt])
        P.dma("sp", K["subg"][:], I["diff_subln_g"][L].partition_broadcast(128), K["subg"], writes=[K["subg"]])
        P.ts("dve", K["subg"][:], K["subg"][:], float(1.0 - lam_init), None, ALU.mult, None, [K["subg"]], [K["subg"]])
        P.dma("sp", K["glag"][:], I["gla_norm_g"][L].partition_broadcast(128), K["glag"], writes=[K["glag"]])


def rstd(P, st, out_ap, in_ap, n, epsc):
    P.act(out_ap, in_ap, AF.Ln, [st, epsc], [st], scale=1.0 / n, bias=epsc[:, 0:1])
    P.act(out_ap, out_ap, AF.Exp, [st], [st], scale=-0.5)


def xsrc(I, X, L, r0):
    if L > 0:
        return X["XR"][r0:r0 + 128, :]
    if r0 < C:
        return I["ctx"][r0:r0 + 128, :]
    return I["x"][r0 - C:r0 - C + 128, :]


def sumsq(P, scr, xt, st):
    P.act(scr[:], xt[:], AF.Square, [xt], [scr])
    P.op("dve", lambda: P.nc.vector.tensor_reduce(st[:, 0:1], scr[:], AX.X, ALU.add), [scr], [st])


def norm_mod(P, xt, Gt, SHt, hb, scr_b, st, epsc, eng2="pool", xo=None):
    nc = P.nc
    sumsq(P, scr_b, xt, st)
    rstd(P, st, st[:, 1:2], st[:, 0:1], D, epsc)
    xo = xt if xo is None else xo
    P.stt("dve", xo[:], xt[:], st[:, 1:2], Gt[:], ALU.mult, ALU.mult, [xt, st, Gt], [xo])
    P.tt(eng2, hb[:], xo[:], SHt[:], ALU.add, [xo, SHt], [hb])


def phase1_2(P, I, X, L, last, lam_init, K):
    nc = P.nc
    with ExitStack() as es:
        hT = P.sb(es, [128, 8, T], BF16)
        with ExitStack() as e1:
            msl = [[P.sb(e1, [128, D], F32) for _ in range(2)] for _ in range(2)]
            for w in range(2):
                for j, seg in enumerate((0, 1)):
                    P.dma("sp", msl[w][j][:], X["MODS"][w][:, seg * D:(seg + 1) * D], msl[w][j], writes=[msl[w][j]])
            xr = Ring([P.sb(e1, [128, D], F32) for _ in range(3)])
            hbr = Ring([P.sb(e1, [128, D], BF16) for _ in range(2)])
            scr_b = P.sb(e1, [128, D], F32)
            str_ = Ring([P.sb(e1, [128, 2], F32) for _ in range(2)])
            ptr = Ring([P.ps(e1, [128, 8, 128], BF16) for _ in range(2)])
            for i in range(NT):
                w = 1 if i < 2 else 0
                xt = xr.next()
                P.dma("sp", xt[:], xsrc(I, X, L, i * 128), xt, writes=[xt])
                hb = hbr.next()
                st = str_.next()
                norm_mod(P, xt, msl[w][1], msl[w][0], hb, scr_b, st, K["epsc"])
                pt = ptr.next()
                for kc in range(8):
                    P.tr(pt[:, kc, :], hb[:, kc * 128:(kc + 1) * 128], K["ident_b"][:], [hb, K["ident_b"]], [pt])
                P.cp("act", hT[:, :, i * 128:(i + 1) * 128], pt[:], [pt], [hT])
            P.barrier()
        if "HT" in X:
            P.dma("sp", X["HT"].rearrange("c p t -> p c t"), hT[:], hT, reads=[hT])
            return
        phase2(P, I, X, L, K, hT)


def phase2(P, I, X, L, K, hT):
    nc = P.nc
    wv = I["w_in"][L].rearrange("(kc p) n -> p kc n", p=128)
    with ExitStack() as es:
        wring = Ring([P.sb(es, [128, 8, 512], BF16) for _ in range(3)])
        psr = Ring([P.ps(es, [128, 512]) for _ in range(4)])
        ptr = Ring([P.ps(es, [128, 4, 128], BF16) for _ in range(2)])
        cos = P.sb(es, [128, 32, 32], F32)
        sin = P.sb(es, [128, 32, 32], F32)
        P.dma("sp", cos[:], I["k_cos"].rearrange("(t p) f -> p t f", p=128), cos, writes=[cos])
        P.dma("sp", sin[:], I["k_sin"].rearrange("(t p) f -> p t f", p=128), sin, writes=[sin])
        ra = Ring([P.sb(es, [128, 512], F32) for _ in range(2)])
        rb = Ring([P.sb(es, [128, 512], F32) for _ in range(2)])
        rob = Ring([P.sb(es, [128, 512], BF16) for _ in range(2)])
        stq = Ring([P.sb(es, [128, 4, 512], BF16) for _ in range(2)])
        stf = Ring([P.sb(es, [128, 4, 512], F32) for _ in range(2)])

        def load_w(c0, ncols):
            wt = wring.next()
            P.dma("pool", wt[:, :, 0:ncols], wv[:, :, c0:c0 + ncols], wt, writes=[wt])
            return wt

        def tok_major(wt, cw0, ncols, i):
            ps = psr.next()
            for kc in range(8):
                P.mm(ps[:, 0:ncols], hT[:, kc, i * 128:(i + 1) * 128], wt[:, kc, cw0:cw0 + ncols],
                     kc == 0, kc == 7, [hT, wt], [ps])
            return ps

        def feat_major(wt, cw0, m, t0, n):
            ps = psr.next()
            for kc in range(8):
                P.mm(ps[0:m, 0:n], wt[:, kc, cw0:cw0 + m], hT[:, kc, t0:t0 + n], kc == 0, kc == 7, [hT, wt], [ps])
            return ps

        for which, dst, c_base in (("q", X["QT"], OQ), ("k", X["KT"], OK_)):
            for half in range(2):
                wt = load_w(c_base + half * 512, 512)
                for (t0, n) in TILES:
                    sq = stq.next()
                    for s in range(n // 128):
                        i = (t0 + s * 128) // 128
                        ps = tok_major(wt, 0, 512, i)
                        ob = rob.next()
                        if i < 2:
                            P.cp("act", ob[:], ps[:], [ps], [ob])
                        else:
                            li = i - 2
                            a = ra.next()
                            b = rb.next()
                            for ax in range(2):
                                def v4(ap):
                                    return ap.rearrange("p (h r) -> p h r", r=64)[:, :, ax * 32:(ax + 1) * 32].rearrange("p h (s f) -> p h s f", s=2)
                                x4, a4, b4, o4 = v4(ps[:]), v4(a[:]), v4(b[:]), v4(ob[:])
                                cs4 = cos[:, li, ax * 16:(ax + 1) * 16].unsqueeze(1).unsqueeze(1).broadcast_to([128, 8, 2, 16])
                                sn3 = sin[:, li, ax * 16:(ax + 1) * 16].unsqueeze(1).broadcast_to([128, 8, 16])
                                P.tt("dve", a4, x4, cs4, ALU.mult, [ps, cos], [a])
                                P.tt("dve", b4[:, :, 0, :], x4[:, :, 1, :], sn3, ALU.mult, [ps, sin], [b])
                                P.tt("dve", b4[:, :, 1, :], x4[:, :, 0, :], sn3, ALU.mult, [ps, sin], [b])
                                P.tt("pool", o4[:, :, 0, :], a4[:, :, 0, :], b4[:, :, 0, :], ALU.subtract, [a, b], [ob])
                                P.tt("pool", o4[:, :, 1, :], a4[:, :, 1, :], b4[:, :, 1, :], ALU.add, [a, b], [ob])
                        pt = ptr.next()
                        for hh in range(4):
                            P.tr(pt[:, hh, :], ob[:, hh * 128:(hh + 1) * 128], K["ident_b"][:], [ob, K["ident_b"]], [pt])
                        P.cp("act", sq[:, :, s * 128:(s + 1) * 128], pt[:], [pt], [sq])
                    P.dma("sp", dst[half * 4:(half + 1) * 4, :, t0:t0 + n].rearrange("h p t -> p h t"),
                          sq[:, :, 0:n], sq, reads=[sq])
        with ExitStack() as ev_:
            vst = P.sb(ev_, [128, 4, NT, 132], BF16)
            P.memset("dve", vst, vst[:].rearrange("p h t e -> p (h t e)"), 1.0)
            for half in range(2):
                wt = load_w(OV + half * 512, 512)
                for i in range(NT):
                    ps = tok_major(wt, 0, 512, i)
                    P.cp("act", vst[:, :, i, 0:128], ps[:].rearrange("p (h e) -> p h e", h=4), [ps], [vst])
                for hh in range(4):
                    P.dma("sp", X["V"][half * 4 + hh], vst[:, hh, :, :].rearrange("p t e -> p (t e)"), vst, reads=[vst])
        for dst, c0 in ((X["CBT"], OCB), (X["CCT"], OCC), (X["CXT"], OCX)):
            wt = load_w(c0, 512)
            for (t0, n) in TILES:
                sf = stf.next()
                for ch in range(4):
                    ps = feat_major(wt, ch * 128, 128, t0, n)
                    P.cp("act", sf[:, ch, 0:n], ps[:, 0:n], [ps], [sf])
                P.dma("sp", dst[:, :, t0:t0 + n].rearrange("c p t -> p c t"), sf[:, :, 0:n], sf, reads=[sf])
        wt = load_w(OGQ, 512)
        for (t0, n) in TILES:
            sf = stf.next()
            for ch in range(4):
                ps = feat_major(wt, ch * 128, 128, t0, n)
                P.cp("act", sf[:, ch, 0:n], ps[:, 0:n], [ps], [sf])
            P.dma("sp", X["GQT"][:, :, t0:t0 + n].rearrange("c p t -> p c t"), sf[:, 0:2, 0:n], sf, reads=[sf])
            P.dma("sp", X["GKT"][:, :, t0:t0 + n].rearrange("c p t -> p c t"), sf[:, 2:4, 0:n], sf, reads=[sf])
            sf = stf.next()
            for s in range(n // 128):
                ps = tok_major(wt, 256, 256, (t0 + s * 128) // 128)
                P.cp("act", sf[:, s, 0:256], ps[:, 0:256], [ps], [sf])
            P.dma("sp", X["GK"][t0:t0 + n, :].rearrange("(s p) c -> p s c", p=128), sf[:, 0:n // 128, 0:256], sf, reads=[sf])
        wt = load_w(OGV, 512)
        for (t0, n) in TILES:
            sq = stq.next()
            for s in range(n // 128):
                ps = tok_major(wt, 0, 512, (t0 + s * 128) // 128)
                P.cp("act", sq[:, s, :], ps[:], [ps], [sq])
            P.dma("sp", X["GV"][t0:t0 + n, :].rearrange("(s p) c -> p s c", p=128), sq[:, 0:n // 128, :], sq, reads=[sq])
        wt = load_w(OGR, 512)
        for (t0, n) in TILES:
            sf = stf.next()
            for s in range(n // 128):
                ps = tok_major(wt, 0, 512, (t0 + s * 128) // 128)
                P.act(sf[:, s, :], ps[:], AF.Silu, [ps], [sf])
            P.dma("sp", X["GR"][t0:t0 + n, :].rearrange("(s p) c -> p s c", p=128), sf[:, 0:n // 128, :], sf, reads=[sf])
        wt = load_w(OGA, 32)
        for (t0, n) in TILES:
            sf = stf.next()
            ps = feat_major(wt, 0, 32, t0, n)
            P.cp("act", sf[0:32, 0, 0:n], ps[0:32, 0:n], [ps], [sf])
            P.dma("sp", X["GAF"][:, t0:t0 + n], sf[0:16, 0, 0:n], sf, reads=[sf])
            P.dma("sp", X["GAB"][:, t0:t0 + n], sf[16:32, 0, 0:n], sf, reads=[sf])
        for gblk in range(6):
            wt = load_w(OGT + gblk * 512, 512)
            for (t0, n) in TILES:
                sq = stq.next()
                for ch in range(4):
                    ps = feat_major(wt, ch * 128, 128, t0, n)
                    P.act(sq[:, ch, 0:n], ps[:, 0:n], AF.Sigmoid, [ps], [sq])
                P.dma("sp", X["SIGT"][gblk * 4:(gblk + 1) * 4, :, t0:t0 + n].rearrange("c p t -> p c t"),
                      sq[:, :, 0:n], sq, reads=[sq])


class V(Dep):
    def __init__(self, ap):
        super().__init__()
        self.ap = ap


def phase3(P, I, X, L, last, lam_init, K):
    nc = P.nc
    with ExitStack() as es:
        ktr = Ring([P.sb(es, [128, T], BF16) for _ in range(2)])
        qtr = Ring([P.sb(es, [128, T], BF16) for _ in range(2)])
        vtr = Ring([P.sb(es, [128, NT, 132], BF16) for _ in range(2)])
        pss = Ring([P.ps(es, [128, 1024]) for _ in range(2)])
        accT = P.ps(es, [128, 1536])
        ptT = Ring([P.ps(es, [128, 2, 128], BF16) for _ in range(1)])
        offs = [0, 160, 320, 512, 672, 832, 1024, 1184]
        accv = [[V(accT[:, offs[m * 4 + s]:offs[m * 4 + s] + 129]) for s in range(4)] for m in range(2)]
        ptr_ = Ring([P.sb(es, [128, 1024], BF16) for _ in range(3)])
        evr = Ring([P.sb(es, [128, 2, 132], F32) for _ in range(8)])
        t1r = Ring([P.sb(es, [128, 128], F32) for _ in range(2)])
        o_r = Ring([P.sb(es, [128, 128], F32) for _ in range(2)])
        jk = P.sb(es, [128, 128], F32)
        obr = Ring([P.sb(es, [128, 128], BF16) for _ in range(2)])
        str_ = Ring([P.sb(es, [128, 4], F32) for _ in range(3)])
        dstr = Ring([P.sb(es, [128, 512], BF16) for _ in range(2)])
        lam = K["lam"]
        qtiles = [(256 + 512 * i, 512, list(range(NT))) for i in range(8)]
        if not last:
            qtiles = [(0, 256, [0, 1])] + qtiles
        if DBG.get("p3_qtiles") is not None:
            qtiles = [qtiles[i] for i in DBG["p3_qtiles"]]
        NH = DBG.get("p3_heads", 8)
        hbuf = {}

        def load_head(h_):
            kt_, qt_, vt_ = ktr.next(), qtr.next(), vtr.next()
            P.dma("sp", kt_[:], X["KT"][h_], kt_, writes=[kt_])
            P.dma("sp", qt_[:], X["QT"][h_], qt_, writes=[qt_])
            P.dma("sp", vt_[:].rearrange("p t e -> p (t e)"), X["V"][h_], vt_, writes=[vt_])
            hbuf[h_] = (kt_, qt_, vt_)

        load_head(0)
        for h in range(NH):
            if h + 1 < NH:
                load_head(h + 1)
            kt, qt, vt = hbuf.pop(h)
            for (q0, n, ktl) in qtiles:
                nsub = n // 128
                def qk(kk_):
                    ps_ = pss.next()
                    for m in range(2):
                        P.mm(ps_[:, m * 512:m * 512 + n], kt[m * 64:(m + 1) * 64, kk_ * 128:(kk_ + 1) * 128],
                             qt[m * 64:(m + 1) * 64, q0:q0 + n], True, True, [kt, qt], [ps_])
                    return ps_

                ps_next = qk(ktl[0])
                for ki, kk in enumerate(ktl):
                    ps = ps_next
                    if ki + 1 < len(ktl):
                        ps_next = qk(ktl[ki + 1])
                    pt = ptr_.next()
                    if n == 512:
                        P.act(pt[:], ps[:], AF.Exp, [ps], [pt], scale=0.125)
                    else:
                        for m in range(2):
                            P.act(pt[:, m * 512:m * 512 + n], ps[:, m * 512:m * 512 + n], AF.Exp, [ps], [pt], scale=0.125)
                    if ki == 0:
                        started = set()
                    for m in range(2):
                        for s in range(nsub):
                            av = accv[m][s]
                            bank = offs[m * 4 + s] // 512
                            st_flag = ki == 0 and bank not in started
                            started.add(bank)
                            P.op("pe", lambda: nc.tensor.matmul(av.ap, pt[:, m * 512 + s * 128:m * 512 + (s + 1) * 128],
                                                                vt[:, kk, 0:129], start=st_flag, stop=(ki == len(ktl) - 1),
                                                                skip_group_check=True), [pt, vt], [av], pe_n=129)
                dst = dstr.next()
                evs = []
                for s in range(nsub):
                    ev = evr.next()
                    for m in range(2):
                        P.cp("dve", ev[:, m, 0:129], accv[m][s].ap, [accv[m][s]], [ev])
                    evs.append(ev)
                for s in range(nsub):
                    ev = evs[s]
                    st = str_.next()
                    P.op("dve", lambda: nc.vector.reciprocal(st[:, 0:2], ev[:, :, 128]), [ev], [st])
                    P.tt("dve", st[:, 1:2], st[:, 1:2], lam[:, 0:1], ALU.mult, [st, lam], [st])
                    t1 = t1r.next()
                    o = o_r.next()
                    P.ts("pool", t1[:], ev[:, 1, 0:128], st[:, 1:2], None, ALU.mult, None, [ev, st], [t1])
                    P.stt("dve", o[:], ev[:, 0, 0:128], st[:, 0:1], t1[:], ALU.mult, ALU.subtract, [ev, st, t1], [o])
                    P.tt("pool", jk[:], o[:], o[:], ALU.mult, [o], [jk])
                    P.op("dve", lambda: nc.vector.tensor_reduce(st[:, 2:3], jk[:], AX.X, ALU.add), [jk], [st])
                    rstd(P, st, st[:, 2:3], st[:, 2:3], 128, K["epsc"])
                    ob = obr.next()
                    P.stt("dve", ob[:], o[:], st[:, 2:3], K["subg"][:], ALU.mult, ALU.mult, [o, st, K["subg"]], [ob])
                    pT = ptT.next()
                    P.tr(pT[:, 0, :], ob[:], K["ident_b"][:], [ob, K["ident_b"]], [pT])
                    P.cp("dve", dst[:, s * 128:(s + 1) * 128], pT[:, 0, :], [pT], [dst])
                P.dma("sp", X["DIFFT"][h, :, q0:q0 + n], dst[:, 0:n], dst, reads=[dst])


def phase4(P, I, X, L, last, lam_init, K):
    nc = P.nc
    with ExitStack() as es:
        cw = P.sb(es, [128, 4, 3], F32)
        with nc.allow_non_contiguous_dma(reason="tiny conv taps"):
            for k_ in range(3):
                P.dma("sp", cw[:, :, k_], I["conv_w"][L, k_].rearrange("(c p) -> p c", p=128), cw, writes=[cw])
        zero = P.sb(es, [128, 8], F32)
        P.memset("dve", zero, zero[:], 0.0)
        cb = P.sb(es, [128, T], F32)
        c_ = P.sb(es, [128, T], F32)
        cx = P.sb(es, [128, T], F32)
        up = P.sb(es, [128, T], F32)
        un = P.sb(es, [128, T], F32)
        y = P.sb(es, [128, T], F32)
        yb = P.sb(es, [128, T], BF16)
        for cc in range(4):
            P.dma("sp", cb[:], X["CBT"][cc], cb, writes=[cb])
            P.dma("sp", c_[:], X["CCT"][cc], c_, writes=[c_])
            P.dma("sp", cx[:], X["CXT"][cc], cx, writes=[cx])
            P.tt("dve", c_[:], c_[:], cx[:], ALU.mult, [c_, cx], [c_])
            P.dma("sp", up[:, 1:T], c_[:, 0:T - 1], up, reads=[c_], writes=[up])
            P.dma("sp", un[:, 0:T - 1], c_[:, 1:T], un, reads=[c_], writes=[un])
            for col in (0, C):
                P.dma("sp", up[:, col:col + 1], zero[:, 0:1], up, reads=[zero], writes=[up])
            for col in (C - 1, T - 1):
                P.dma("sp", un[:, col:col + 1], zero[:, 0:1], un, reads=[zero], writes=[un])
            P.ts("dve", y[:], c_[:], cw[:, cc, 1:2], None, ALU.mult, None, [c_, cw], [y])
            P.stt("dve", y[:], up[:], cw[:, cc, 0:1], y[:], ALU.mult, ALU.add, [up, cw, y], [y])
            P.stt("dve", y[:], un[:], cw[:, cc, 2:3], y[:], ALU.mult, ALU.add, [un, cw, y], [y])
            P.tt("dve", yb[:], y[:], cb[:], ALU.mult, [y, cb], [yb])
            P.dma("sp", X["YCT"][cc], yb[:], yb, reads=[yb])


def phase5(P, I, X, L, last, lam_init, K):
    nc = P.nc
    NCH = NT
    with ExitStack() as es:
        tri = P.sb(es, [128, 6, 128], F32)
        P.dma("sp", tri[:], I["k_tri"].rearrange("s m l -> m s l"), tri, writes=[tri])
        wa = P.sb(es, [16, 2, 256], F32)
        P.dma("sp", wa[:], I["gla_w_a2"][L].rearrange("d k n -> k d n"), wa, writes=[wa])
        ba = P.sb(es, [128, 512], F32)
        P.dma("sp", ba[:], I["gla_b_a"][L].partition_broadcast(128), ba, writes=[ba])
        neg16 = P.sb(es, [128, 2], F32)
        P.memset("dve", neg16, neg16[:], -1.0 / 16.0)
        Sf = P.sb(es, [128, 2, 128], F32)
        Sfb = P.sb(es, [128, 2, 128], BF16)
        Sb = P.sb(es, [128, 2, 128], F32)
        SLB = P.sb(es, [128, NCH, 2, 128], F32)
        DECB = P.sb(es, [128, NCH, 2], F32)
        SBP = P.sb(es, [128, NCH, 2, 128], BF16)
        for t_ in (Sf, Sb):
            P.memset("dve", t_, t_[:], 0.0)
        P.memset("dve", Sfb, Sfb[:], 0.0)
        psA = P.ps(es, [128, 512])
        psB = P.ps(es, [128, 512])
        psC = P.ps(es, [128, 1024])
        psO = P.ps(es, [128, 512])
        psS = P.ps(es, [128, 512])
        psT = P.ps(es, [128, 4, 128], BF16)
        gqr = Ring([P.sb(es, [128, 2, 512], F32) for _ in range(2)])
        gkr = Ring([P.sb(es, [128, 2, 512], F32) for _ in range(2)])
        gktr = Ring([P.sb(es, [128, 256], F32) for _ in range(2)])
        gvr = Ring([P.sb(es, [128, 512], BF16) for _ in range(2)])
        grr = Ring([P.sb(es, [128, 512], F32) for _ in range(2)])
        gar = [Ring([P.sb(es, [16, 512], F32) for _ in range(2)]) for _ in range(2)]
        zbr = Ring([P.sb(es, [128, 256], F32) for _ in range(2)])
        spr = Ring([P.sb(es, [128, 256], F32) for _ in range(2)])
        e1r = Ring([P.sb(es, [128, 256], F32) for _ in range(2)])
        e2r = Ring([P.sb(es, [128, 256], F32) for _ in range(2)])
        e3r = Ring([P.sb(es, [128, 256], F32) for _ in range(2)])
        decr = Ring([P.sb(es, [128, 2], F32) for _ in range(2)])
        qdr = [Ring([P.sb(es, [128, 2, 128], BF16) for _ in range(2)]) for _ in range(2)]
        kir = [Ring([P.sb(es, [128, 2, 128], BF16) for _ in range(2)]) for _ in range(2)]
        ker = Ring([P.sb(es, [128, 256], BF16) for _ in range(2)])
        atr = [Ring([P.sb(es, [128, 4, 128], BF16) for _ in range(2)]) for _ in range(2)]
        osr = Ring([P.sb(es, [128, 512], F32) for _ in range(2)])
        sqj = P.sb(es, [128, 512], F32)
        onr = Ring([P.sb(es, [128, 512], F32) for _ in range(2)])
        obr = Ring([P.sb(es, [128, 512], BF16) for _ in range(2)])
        ygr = Ring([P.sb(es, [128, 4, 512], BF16) for _ in range(2)])

        def tile_of(c):
            if c < 2:
                return 0, 256, c * 128
            j = (c - 2) // 4
            return 256 + 512 * j, 512, ((c - 2) % 4) * 128
        str_ = Ring([P.sb(es, [128, 8], F32) for _ in range(2)])
        GAsrc = (X["GAF"], X["GAB"])

        def softplus_neg(c, d, ga, off):
            P.mm(psA[:, 0:256], ga[0:16, off:off + 128], wa[0:16, d, :], True, True, [ga, wa], [psA])
            zb = zbr.next()
            P.tt("dve", zb[:], psA[:, 0:256], ba[:, d * 256:(d + 1) * 256], ALU.add, [psA, ba], [zb])
            P.act(zb[:], zb[:], AF.Exp, [zb], [zb], scale=-1.0)
            sp = spr.next()
            P.act(sp[:], zb[:], AF.Ln, [zb], [sp], bias=1.0)
            return sp

        def tot_dec(sp, dec_ap, dec_t):
            for hp in range(2):
                P.mm(psA[:, 256 + hp:257 + hp], sp[:, hp * 128:(hp + 1) * 128], neg16[:, 0:1], True, True, [sp, neg16], [psA])
            P.act(dec_ap, psA[:, 256:258], AF.Exp, [psA], [dec_t])

        def ke_of(sp, d, gkt):
            P.mm(psB[:, 256:512], tri[:, 2 + d, :], sp[:], True, True, [tri, sp], [psB])
            e3 = e3r.next()
            P.act(e3[:], psB[:, 256:512], AF.Exp, [psB], [e3])
            ke = ker.next()
            P.tt("pool", ke[:], gkt[:], e3[:], ALU.mult, [gkt, e3], [ke])
            return ke

        def sloc(ke, gv):
            for hp in range(2):
                P.mm(psS[:, hp * 256:(hp + 1) * 256], ke[:, hp * 128:(hp + 1) * 128], gv[:, hp * 256:(hp + 1) * 256],
                     True, True, [ke, gv], [psS])

        for c in range(NCH):
            gkt, gv = gktr.next(), gvr.next()
            t0_, tn_, off_ = tile_of(c)
            if off_ == 0:
                gab_t = gar[1].next()
                P.dma("sp", gab_t[:, 0:tn_], X["GAB"][:, t0_:t0_ + tn_], gab_t, writes=[gab_t])
            P.dma("sp", gkt[:], X["GK"][c * 128:(c + 1) * 128, :], gkt, writes=[gkt])
            P.dma("sp", gv[:], X["GV"][c * 128:(c + 1) * 128, :], gv, writes=[gv])
            stg = DBG.get("p5_stage", 99)
            sp = softplus_neg(c, 1, gab_t, off_)
            if stg < 2:
                continue
            tot_dec(sp, DECB[:, c, :], DECB)
            if stg < 3:
                continue
            ke = ke_of(sp, 1, gkt)
            if stg < 4:
                continue
            sloc(ke, gv)
            for hp in range(2):
                for hh in range(2):
                    P.cp("act", SLB[hh * 64:(hh + 1) * 64, c, hp, :],
                         psS[hh * 64:(hh + 1) * 64, hp * 256 + hh * 128:hp * 256 + (hh + 1) * 128], [psS], [SLB])
        if DBG.get("p5_stage", 99) < 5:
            return
        for c in [1, 0] + list(range(NCH - 1, 1, -1)):
            P.cp("pool", SBP[:, c, :, :], Sb[:], [Sb], [SBP])
            for hp in range(2):
                P.stt("dve", Sb[:, hp, :], Sb[:, hp, :], DECB[:, c, hp:hp + 1], SLB[:, c, hp, :], ALU.mult, ALU.add,
                      [Sb, DECB, SLB], [Sb])
        if DBG.get("p5_stage", 99) < 6:
            return
        stg = DBG.get("p5_stage", 99)
        tl = {}

        def stage_a(c):
            gkt, gv, gr = gktr.next(), gvr.next(), grr.next()
            cs = slice(c * 128, (c + 1) * 128)
            t0_, tn_, off_ = tile_of(c)
            osl = slice(off_, off_ + 128)
            if off_ == 0:
                tl["gq"], tl["gk"], tl["yg"] = gqr.next(), gkr.next(), ygr.next()
                tl["ga"] = [gar[0].next(), gar[1].next()]
                ts_ = slice(t0_, t0_ + tn_)
                P.dma("sp", tl["gq"][:, :, 0:tn_], X["GQT"][:, :, ts_].rearrange("c p t -> p c t"), tl["gq"], writes=[tl["gq"]])
                P.dma("sp", tl["gk"][:, :, 0:tn_], X["GKT"][:, :, ts_].rearrange("c p t -> p c t"), tl["gk"], writes=[tl["gk"]])
                for d in range(2):
                    P.dma("sp", tl["ga"][d][:, 0:tn_], GAsrc[d][:, ts_], tl["ga"][d], writes=[tl["ga"][d]])
            gq_t, gk_t, yg, ga = tl["gq"], tl["gk"], tl["yg"], tl["ga"]
            P.dma("sp", gkt[:], X["GK"][cs, :], gkt, writes=[gkt])
            P.dma("sp", gv[:], X["GV"][cs, :], gv, writes=[gv])
            P.dma("sp", gr[:], X["GR"][cs, :], gr, writes=[gr])
            qd, ki, atm = [None, None], [None, None], [None, None]
            dec = decr.next()
            ke = None
            for d in range(2):
                sp = softplus_neg(c, d, ga[d], off_)
                for hp in range(2):
                    P.mm(psB[:, hp * 128:(hp + 1) * 128], sp[:, hp * 128:(hp + 1) * 128], tri[:, d, :], True, True,
                         [sp, tri], [psB])
                e1, e2 = e1r.next(), e2r.next()
                P.act(e1[:], psB[:, 0:256], AF.Exp, [psB], [e1])
                P.act(e2[:], psB[:, 0:256], AF.Exp, [psB], [e2], scale=-1.0)
                qd[d], ki[d] = qdr[d].next(), kir[d].next()
                P.stt("dve", qd[d][:], gq_t[:, :, osl], 0.125, e1[:].rearrange("p (a b) -> p a b", a=2),
                      ALU.mult, ALU.mult, [gq_t, e1], [qd[d]])
                P.tt("pool", ki[d][:], gk_t[:, :, osl], e2[:].rearrange("p (a b) -> p a b", a=2), ALU.mult,
                     [gk_t, e2], [ki[d]])
                if d == 0:
                    tot_dec(sp, dec[:], dec)
                    ke = ke_of(sp, 0, gkt)
                for h in range(4):
                    hp, b0 = h // 2, (h % 2) * 64
                    co = (h % 2) * 512 + (d * 2 + hp) * 128
                    P.mm(psC[:, co:co + 128], ki[d][b0:b0 + 64, hp, :], qd[d][b0:b0 + 64, hp, :],
                         True, True, [ki[d], qd[d]], [psC])
                atm[d] = atr[d].next()
                P.tt("dve", atm[d][:].rearrange("p (hp par) l -> p hp par l", par=2),
                     psC[:].rearrange("p (par d hp l) -> p d hp par l", par=2, d=2, hp=2)[:, d],
                     tri[:, 4 + d, :].unsqueeze(1).unsqueeze(1).broadcast_to([128, 2, 2, 128]), ALU.mult, [psC, tri], [atm[d]])
            return dict(c=c, gv=gv, gr=gr, qd=qd, atm=atm, dec=dec, ke=ke, yg=yg, osl=osl, off=off_, tn=tn_, t0=t0_)

        def stage_b(a_):
            c, gv, gr, qd, atm, dec, ke, yg = a_["c"], a_["gv"], a_["gr"], a_["qd"], a_["atm"], a_["dec"], a_["ke"], a_["yg"]
            osl, off_, tn_, t0_ = a_["osl"], a_["off"], a_["tn"], a_["t0"]
            for h in range(4):
                hp, b0 = h // 2, (h % 2) * 64
                oo = psO[:, h * 128:(h + 1) * 128]
                vv = gv[:, h * 128:(h + 1) * 128]
                P.mm(oo, atm[0][:, h, :], vv, True, False, [atm[0], gv], [psO])
                P.mm(oo, qd[0][b0:b0 + 64, hp, :], Sfb[b0:b0 + 64, hp, :], False, False, [qd[0], Sfb], [psO])
                P.mm(oo, atm[1][:, h, :], vv, False, False, [atm[1], gv], [psO])
                P.mm(oo, qd[1][b0:b0 + 64, hp, :], SBP[b0:b0 + 64, c, hp, :], False, True, [qd[1], SBP], [psO])
            sloc(ke, gv)
            for hp in range(2):
                for hh in range(2):
                    rs = slice(hh * 64, (hh + 1) * 64)
                    P.stt("dve", Sf[rs, hp, :], Sf[rs, hp, :], dec[rs, hp:hp + 1],
                          psS[rs, hp * 256 + hh * 128:hp * 256 + (hh + 1) * 128], ALU.mult, ALU.add, [Sf, dec, psS], [Sf])
            P.cp("pool", Sfb[:], Sf[:], [Sf], [Sfb])
            osb, on, ob, st = osr.next(), onr.next(), obr.next(), str_.next()
            P.cp("act", osb[:], psO[:], [psO], [osb])
            P.tt("pool", sqj[:], osb[:], osb[:], ALU.mult, [osb], [sqj])
            P.op("dve", lambda: nc.vector.tensor_reduce(st[:, 0:4], sqj[:].rearrange("p (h e) -> p h e", h=4), AX.X, ALU.add),
                 [sqj], [st])
            rstd(P, st, st[:, 0:4], st[:, 0:4], 128, K["epsc"])
            o3 = osb[:].rearrange("p (h e) -> p h e", h=4)
            n3 = on[:].rearrange("p (h e) -> p h e", h=4)
            P.tt("dve", n3, o3, st[:, 0:4].unsqueeze(2).broadcast_to([128, 4, 128]), ALU.mult, [osb, st], [on])
            P.tt("pool", n3, n3, K["glag"][:].unsqueeze(1).broadcast_to([128, 4, 128]), ALU.mult, [on, K["glag"]], [on])
            P.tt("dve", ob[:], on[:], gr[:], ALU.mult, [on, gr], [ob])
            for h in range(4):
                P.tr(psT[:, h, :], ob[:, h * 128:(h + 1) * 128], K["ident_b"][:], [ob, K["ident_b"]], [psT])
            P.cp("act", yg[:, :, osl], psT[:], [psT], [yg])
            if off_ + 128 == tn_:
                P.dma("sp", X["YGT"][:, :, t0_:t0_ + tn_].rearrange("c p t -> p c t"), yg[:, :, 0:tn_], yg, reads=[yg])

        prev_a = None
        for c in range(NCH):
            cur_a = stage_a(c)
            if prev_a is not None:
                stage_b(prev_a)
            prev_a = cur_a
        stage_b(prev_a)


def phase6(P, I, X, L, last, lam_init, K):
    nc = P.nc
    with ExitStack() as es:
        WD = P.sb(es, [128, 8, D], BF16)
        WC = P.sb(es, [128, 4, D], BF16)
        WG = P.sb(es, [128, 4, D], BF16)
        WO = P.sb(es, [128, 8, D], BF16)
        for wt, nm in ((WD, "diff_w_out"), (WC, "conv_w_out"), (WG, "gla_w_out"), (WO, "w_o")):
            P.dma("pool", wt[:], I[nm][L].rearrange("(kc p) n -> p kc n", p=128), wt, writes=[wt])
        RW = P.sb(es, [128, 8, NE], F32)
        P.dma("sp", RW[:], I["router_w"][L].rearrange("(kc p) e -> p kc e", p=128), RW, writes=[RW])
        RB = P.sb(es, [128, NE], F32)
        P.dma("sp", RB[:], I["router_b"][L].partition_broadcast(128), RB, writes=[RB])
        nw = 1 if last else 2
        msl = [[P.sb(es, [128, D], F32) for _ in range(3)] for _ in range(nw)]
        for w in range(nw):
            for j, seg in enumerate((2, 3, 4)):
                P.dma("sp", msl[w][j][:], X["MODS"][w][:, seg * D:(seg + 1) * D], msl[w][j], writes=[msl[w][j]])
        dTr = Ring([P.sb(es, [128, 8, 512], BF16) for _ in range(2)])
        ycr = Ring([P.sb(es, [128, 4, 512], BF16) for _ in range(2)])
        ygr = Ring([P.sb(es, [128, 4, 512], BF16) for _ in range(2)])
        sgr = Ring([P.sb(es, [128, 3, 512], BF16) for _ in range(3)])
        mgr = Ring([P.sb(es, [128, 8, 512], BF16) for _ in range(1)])
        m1r = Ring([P.sb(es, [128, 512], F32) for _ in range(2)])
        m2r = Ring([P.sb(es, [128, 512], F32) for _ in range(2)])
        m3r = Ring([P.sb(es, [128, 512], F32) for _ in range(2)])
        xr = Ring([P.sb(es, [128, D], F32) for _ in range(2)])
        xnr = Ring([P.sb(es, [128, D], F32) for _ in range(1)])
        tmr = Ring([P.sb(es, [128, 512], F32) for _ in range(2)])
        hbr = Ring([P.sb(es, [128, D], F32) for _ in range(1)])
        hbbr = Ring([P.sb(es, [128, D], BF16) for _ in range(2)])
        scr_b = P.sb(es, [128, D], F32)
        str_ = Ring([P.sb(es, [128, 2], F32) for _ in range(2)])
        h32r = Ring([P.sb(es, [128, 8, 128], F32) for _ in range(1)])
        h2st = Ring([P.sb(es, [128, 8, 512], BF16) for _ in range(1)])
        gtst = Ring([P.sb(es, [128, 4, NE], F32) for _ in range(2)])
        lgr = Ring([P.sb(es, [128, NE], F32) for _ in range(2)])
        er = Ring([P.sb(es, [128, NE], F32) for _ in range(2)])
        mkr = Ring([P.sb(es, [128, NE], F32) for _ in range(2)])
        m8r = Ring([P.sb(es, [128, 16], F32) for _ in range(2)])
        psbr = Ring([P.ps(es, [128, 512]) for _ in range(3)])
        psor = Ring([P.ps(es, [128, 512]) for _ in range(2)])
        ptr = P.ps(es, [128, 8, 128], F32)
        pslg = P.ps(es, [128, 512])
        sig4 = X["SIGT"].rearrange("(b c) p t -> c p b t", b=3)
        tiles = TILES[1:] if last else TILES
        for (t0, n) in tiles:
            w = 1 if t0 == 0 else 0
            g1, sh2, G2 = msl[w]
            dT, yc, yg = dTr.next(), ycr.next(), ygr.next()
            P.dma("sp", dT[:, :, 0:n], X["DIFFT"][:, :, t0:t0 + n].rearrange("c p t -> p c t"), dT, writes=[dT])
            P.dma("sp", yc[:, :, 0:n], X["YCT"][:, :, t0:t0 + n].rearrange("c p t -> p c t"), yc, writes=[yc])
            P.dma("sp", yg[:, :, 0:n], X["YGT"][:, :, t0:t0 + n].rearrange("c p t -> p c t"), yg, writes=[yg])
            mg = mgr.next()
            for c in range(8):
                sg = sgr.next()
                P.dma("sp", sg[:, :, 0:n], sig4[c][:, :, t0:t0 + n], sg, writes=[sg])
                cs = slice(c * 128, (c + 1) * 128)
                pd, pc, pg = psbr.next(), psbr.next(), psbr.next()
                for kc in range(8):
                    P.mm(pd[:, 0:n], WD[:, kc, cs], dT[:, kc, 0:n], kc == 0, kc == 7, [WD, dT], [pd])
                for kc in range(4):
                    P.mm(pc[:, 0:n], WC[:, kc, cs], yc[:, kc, 0:n], kc == 0, kc == 3, [WC, yc], [pc])
                for kc in range(4):
                    P.mm(pg[:, 0:n], WG[:, kc, cs], yg[:, kc, 0:n], kc == 0, kc == 3, [WG, yg], [pg])
                m1, m2, m3 = m1r.next(), m2r.next(), m3r.next()
                P.tt("dve", m1[:, 0:n], pd[:, 0:n], sg[:, 0, 0:n], ALU.mult, [pd, sg], [m1])
                P.tt("dve", m2[:, 0:n], pc[:, 0:n], sg[:, 1, 0:n], ALU.mult, [pc, sg], [m2])
                P.tt("dve", m3[:, 0:n], pg[:, 0:n], sg[:, 2, 0:n], ALU.mult, [pg, sg], [m3])
                P.tt("pool", m1[:, 0:n], m1[:, 0:n], m2[:, 0:n], ALU.add, [m1, m2], [m1])
                P.tt("pool", mg[:, c, 0:n], m1[:, 0:n], m3[:, 0:n], ALU.add, [m1, m3], [mg])
            h2s = h2st.next()
            gts = gtst.next()
            stg6 = DBG.get("p6_stage", 99)
            if stg6 < 2:
                continue
            for s in range(n // 128):
                r0 = t0 + s * 128
                xt, xn = xr.next(), xnr.next()
                P.dma("sp", xt[:], xsrc(I, X, L, r0), xt, writes=[xt])
                for hf in range(2):
                    po = psor.next()
                    hs = slice(hf * 512, (hf + 1) * 512)
                    for kc in range(8):
                        P.mm(po[:], mg[:, kc, s * 128:(s + 1) * 128], WO[:, kc, hs], kc == 0, kc == 7, [mg, WO], [po])
                    tm = tmr.next()
                    P.tt("dve", tm[:], po[:], g1[:, hs], ALU.mult, [po, g1], [tm])
                    P.tt("pool", xt[:, hs], xt[:, hs], tm[:], ALU.add, [xt, tm], [xt])
                P.dma("sp", X["XR"][r0:r0 + 128, :], xt[:], xt, reads=[xt])
                if stg6 < 3:
                    continue
                hb, st = hbr.next(), str_.next()
                norm_mod(P, xt, G2, sh2, hb, scr_b, st, K["epsc"], xo=xn)
                hbb = hbbr.next()
                P.cp("act", hbb[:], hb[:], [hb], [hbb])
                P.dma("sp", X["H2M"][r0:r0 + 128, :], hbb[:], hbb, reads=[hbb])
                if stg6 < 3.3:
                    continue
                for kc in range(8):
                    P.mm(ptr[:, kc, :], hb[:, kc * 128:(kc + 1) * 128], K["ident_f"][:], True, True, [hb, K["ident_f"]], [ptr])
                if stg6 < 3.6:
                    continue
                h32 = h32r.next()
                P.cp("act", h32[:], ptr[:], [ptr], [h32])
                if stg6 < 3.8:
                    continue
                P.cp("act", h2s[:, :, s * 128:(s + 1) * 128], ptr[:], [ptr], [h2s])
                if stg6 < 4:
                    continue
                for kc in range(8):
                    P.mm(pslg[:, 0:NE], h32[:, kc, :], RW[:, kc, :], kc == 0, kc == 7, [h32, RW], [pslg])
                lg, e_, mk, m8 = lgr.next(), er.next(), mkr.next(), m8r.next()
                P.tt("dve", lg[:], pslg[:, 0:NE], RB[:], ALU.add, [pslg, RB], [lg])
                P.op("dve", lambda: nc.vector.max(m8[:, 0:8], lg[:]), [lg], [m8])
                P.ts("dve", m8[:, 8:9], m8[:, 0:1], -1.0, None, ALU.mult, None, [m8], [m8])
                P.ts("dve", mk[:], lg[:], m8[:, 3:4], None, ALU.is_ge, None, [lg, m8], [mk])
                P.act(e_[:], lg[:], AF.Exp, [lg, m8], [e_], bias=m8[:, 8:9])
                P.tt("dve", e_[:], e_[:], mk[:], ALU.mult, [e_, mk], [e_])
                P.op("dve", lambda: nc.vector.tensor_reduce(m8[:, 9:10], e_[:], AX.X, ALU.add), [e_], [m8])
                P.op("dve", lambda: nc.vector.reciprocal(m8[:, 10:11], m8[:, 9:10]), [m8], [m8])
                P.ts("dve", gts[:, s, :], e_[:], m8[:, 10:11], None, ALU.mult, None, [e_, m8], [gts])
            if stg6 < 4:
                continue
            P.dma("sp", X["H2T"][:, :, t0:t0 + n].rearrange("c p t -> p c t"), h2s[:, :, 0:n], h2s, reads=[h2s])
            P.dma("sp", X["GATES"][t0:t0 + n, :].rearrange("(s p) e -> p s e", p=128), gts[:, 0:n // 128, :], gts, reads=[gts])


def phase7(P, I, X, L, last, lam_init, K, yout):
    nc = P.nc
    with ExitStack() as es:
        nw = 1 if last else 2
        g2 = [P.sb(es, [128, D], F32) for _ in range(nw)]
        for w in range(nw):
            P.dma("sp", g2[w][:], X["MODS"][w][:, 5 * D:6 * D], g2[w], writes=[g2[w]])
        B1T = P.sb(es, [128, NE, 2, 8], F32)
        with nc.allow_non_contiguous_dma(reason="bias de-interleave"):
            for e in range(NE):
                for s_ in range(2):
                    P.dma("sp", B1T[:, e, s_, :], I["moe_b1"][L, e].rearrange("(j p s) -> s p j", p=128, s=2)[s_], B1T, writes=[B1T])
        B2 = P.sb(es, [NE, D], F32)
        P.dma("sp", B2[:], I["moe_b2"][L], B2, writes=[B2])
        FG = None
        if last:
            FG = P.sb(es, [128, D], F32)
            P.dma("sp", FG[:], I["final_norm_g"].partition_broadcast(128), FG, writes=[FG])
        acc = P.sb(es, [128, 8, D], F32)
        h2 = Ring([P.sb(es, [128, 8, 1024], BF16) for _ in range(1)])
        gtr = Ring([P.sb(es, [128, 8, NE], F32) for _ in range(1)])
        gT = Ring([P.sb(es, [NE, 128], F32) for _ in range(2)])
        w1r = Ring([P.sb(es, [128, 8, 512], BF16) for _ in range(5)])
        w2r = Ring([P.sb(es, [128, 8, 512], BF16) for _ in range(4)])
        actr = Ring([P.sb(es, [128, 8, 512], BF16) for _ in range(2)])
        B1S = P.sb(es, [128, NE, 8], F32)
        P.ts("dve", B1S[:], B1T[:, :, 0, :], 1.702, None, ALU.mult, None, [B1T], [B1S])
        P.ts("dve", B1T[:, :, 1, :], B1T[:, :, 1, :], 1.0, None, ALU.add, None, [B1T], [B1T])
        glr = Ring([P.sb(es, [128, 512], F32) for _ in range(2)])
        sgr = Ring([P.sb(es, [128, 512], F32) for _ in range(2)])
        lnr = Ring([P.sb(es, [128, 512], F32) for _ in range(2)])
        xr = Ring([P.sb(es, [128, D], F32) for _ in range(2)])
        scr_b = P.sb(es, [128, D], F32)
        str_ = Ring([P.sb(es, [128, 2], F32) for _ in range(2)])
        psg = Ring([P.ps(es, [128, 512]) for _ in range(2)])
        psl = Ring([P.ps(es, [128, 512]) for _ in range(2)])
        pso = Ring([P.ps(es, [128, 512]) for _ in range(3)])
        pst = P.ps(es, [128, 512])
        if last:
            tiles = [(C + 1024 * i, 1024) for i in range(4)]
        else:
            tiles = [(0, C)] + [(C + 1024 * i, 1024) for i in range(4)]
        w1v = I["moe_w1"][L].rearrange("e (kc p) n -> e p kc n", p=128)
        w2v = I["moe_w2"][L].rearrange("e (kc p) n -> e p kc n", p=128)
        for (t0, n) in tiles:
            w = 1 if t0 == 0 else 0
            nsub = n // 128
            h2t, gt = h2.next(), gtr.next()
            P.dma("sp", h2t[:, :, 0:n], X["H2T"][:, :, t0:t0 + n].rearrange("c p t -> p c t"), h2t, writes=[h2t])
            P.dma("sp", gt[:, 0:nsub, :], X["GATES"][t0:t0 + n, :].rearrange("(s p) e -> p s e", p=128), gt, writes=[gt])
            for s in range(nsub):
                P.mm(pst[0:NE, 0:128], gt[:, s, :], K["ident_f"][:], True, True, [gt, K["ident_f"]], [pst])
                g_t = gT.next()
                P.cp("act", g_t[:], pst[0:NE, 0:128], [pst], [g_t])
                for hf in range(2):
                    po = pso.next()
                    P.mm(po[:], g_t[:], B2[:, hf * 512:(hf + 1) * 512], True, True, [g_t, B2], [po])
                    P.cp("act", acc[:, s, hf * 512:(hf + 1) * 512], po[:], [po], [acc])
            W1, W2 = {}, {}

            def load_w1(e, q):
                t_ = w1r.next()
                P.dma("pool", t_[:], w1v[e][:, :, q * 512:(q + 1) * 512], t_, writes=[t_])
                W1[(e, q)] = t_

            def load_w2(e, hf):
                t_ = w2r.next()
                P.dma("pool", t_[:], w2v[e][:, :, hf * 512:(hf + 1) * 512], t_, writes=[t_])
                W2[(e, hf)] = t_

            for q in range(4):
                load_w1(0, q)
            for hf in range(2):
                load_w2(0, hf)
            subtiles = [(j0, min(512, n - j0)) for j0 in range(0, n, 512)]
            pending = [None]
            NEX = DBG.get("p7_experts", NE)
            SIGCAP = 1.0 / (1.0 + math.exp(-1.702 * 7.0))
            for e in range(NEX):
                for si, (j0, nj) in enumerate(subtiles):
                    pre = si == len(subtiles) - 1 and e + 1 < NEX
                    at = actr.next()

                    def mm1(ci):
                        q, gch = ci // 2, ci % 2
                        w1s = W1[(e, q)]
                        pg, pl = psg.next(), psl.next()
                        for sidx, pp in ((0, pg), (1, pl)):
                            for kc in range(8):
                                P.mm(pp[:, 0:nj], w1s[:, kc, gch * 256 + sidx:gch * 256 + 256:2],
                                     h2t[:, kc, j0:j0 + nj], kc == 0, kc == 7, [w1s, h2t], [pp])
                        if pre and gch == 1:
                            load_w1(e + 1, q)
                        return pg, pl

                    def post1(ci, pg, pl):
                        fc = ci
                        gl, sg, ln = glr.next(), sgr.next(), lnr.next()
                        if DBG.get("p7_old_swiglu"):
                            P.ts("dve", gl[:, 0:nj], pg[:, 0:nj], B1T[:, e, 0, fc:fc + 1], 7.0, ALU.add, ALU.min, [pg, B1T], [gl])
                            P.act(sg[:, 0:nj], gl[:, 0:nj], AF.Sigmoid, [gl], [sg], scale=1.702)
                            P.ts("dve", ln[:, 0:nj], pl[:, 0:nj], B1T[:, e, 1, fc:fc + 1], 8.0, ALU.add, ALU.min, [pl, B1T], [ln])
                            P.ts("dve", ln[:, 0:nj], ln[:, 0:nj], -6.0, None, ALU.max, None, [ln], [ln])
                            P.tt("dve", gl[:, 0:nj], gl[:, 0:nj], sg[:, 0:nj], ALU.mult, [gl, sg], [gl])
                            P.tt("dve", at[:, fc, 0:nj], gl[:, 0:nj], ln[:, 0:nj], ALU.mult, [gl, ln], [at])
                            return
                        P.ts("dve", gl[:, 0:nj], pg[:, 0:nj], B1T[:, e, 0, fc:fc + 1], 7.0, ALU.add, ALU.min, [pg, B1T], [gl])
                        P.act(sg[:, 0:nj], gl[:, 0:nj], AF.Sigmoid, [gl], [sg], scale=1.702)
                        P.ts("dve", ln[:, 0:nj], pl[:, 0:nj], B1T[:, e, 1, fc:fc + 1], 8.0, ALU.add, ALU.min, [pl, B1T], [ln])
                        if DBG.get("p7_nofuse"):
                            P.ts("dve", sg[:, 0:nj], sg[:, 0:nj], SIGCAP, None, ALU.min, None, [sg], [sg])
                            P.tt("dve", gl[:, 0:nj], sg[:, 0:nj], gl[:, 0:nj], ALU.mult, [sg, gl], [gl])
                            P.ts("dve", ln[:, 0:nj], ln[:, 0:nj], -6.0, None, ALU.max, None, [ln], [ln])
                            P.tt("dve", at[:, fc, 0:nj], ln[:, 0:nj], gl[:, 0:nj], ALU.mult, [ln, gl], [at])
                            return
                        P.stt("dve", gl[:, 0:nj], sg[:, 0:nj], SIGCAP, gl[:, 0:nj], ALU.min, ALU.mult, [sg, gl], [gl])
                        P.stt("dve", at[:, fc, 0:nj], ln[:, 0:nj], -6.0, gl[:, 0:nj], ALU.max, ALU.mult, [ln, gl], [at])

                    def make_ffn2(e_, j0_, nj_, at_):
                        def run():
                            groups = [(s_, hf_) for s_ in range(nj_ // 128) for hf_ in range(2)]

                            def mm2(g):
                                s_, hf_ = groups[g]
                                po = pso.next()
                                w2s = W2[(e_, hf_)]
                                for fc in range(8):
                                    P.mm(po[:], at_[:, fc, s_ * 128:(s_ + 1) * 128], w2s[:, fc, :], fc == 0, fc == 7, [at_, w2s], [po])
                                return po

                            def post2(g, po):
                                s_, hf_ = groups[g]
                                sidx = j0_ // 128 + s_
                                asl = acc[:, sidx, hf_ * 512:(hf_ + 1) * 512]
                                P.stt("dve", asl, po[:], gt[:, sidx, e_:e_ + 1], asl, ALU.mult, ALU.add, [po, gt, acc], [acc])

                            prev = None
                            for g in range(len(groups)):
                                cur = mm2(g)
                                if prev is not None:
                                    post2(g - 1, prev)
                                prev = cur
                            post2(len(groups) - 1, prev)
                        return run

                    prev = None
                    for ci in range(8):
                        cur = mm1(ci)
                        if prev is not None:
                            post1(ci - 1, *prev)
                        prev = cur
                        if ci == 1 and pending[0] is not None:
                            pending[0]()
                            pending[0] = None
                    post1(7, *prev)
                    if pre:
                        load_w2(e + 1, 0)
                        load_w2(e + 1, 1)
                    pending[0] = make_ffn2(e, j0, nj, at)
            if pending[0] is not None:
                pending[0]()
                pending[0] = None
            for s in range(nsub):
                r0 = t0 + s * 128
                xt = xr.next()
                P.dma("sp", xt[:], X["XR"][r0:r0 + 128, :], xt, writes=[xt])
                P.tt("dve", acc[:, s, :], acc[:, s, :], g2[w][:], ALU.mult, [acc, g2[w]], [acc])
                P.tt("pool", xt[:], xt[:], acc[:, s, :], ALU.add, [xt, acc], [xt])
                if not last:
                    P.dma("sp", X["XR"][r0:r0 + 128, :], xt[:], xt, reads=[xt])
                else:
                    st = str_.next()
                    sumsq(P, scr_b, xt, st)
                    rstd(P, st, st[:, 1:2], st[:, 0:1], D, K["epsc"])
                    P.stt("dve", xt[:], xt[:], st[:, 1:2], FG[:], ALU.mult, ALU.mult, [xt, st, FG], [xt])
                    P.dma("sp", yout[r0 - C:r0 - C + 128, :], xt[:], xt, reads=[xt])


def host_consts():
    inv_freq = (10000.0 ** (-np.arange(0, 32, 2, dtype=np.float32) / 32.0)).astype(np.float32)
    pos = np.arange(S)
    row = (pos // 64).astype(np.float32)
    col = (pos % 64).astype(np.float32)
    ang = np.concatenate([row[:, None] * inv_freq[None, :], col[:, None] * inv_freq[None, :]], axis=1).astype(np.float32)
    m = np.arange(128)
    tri = np.zeros((6, 128, 128), np.float32)
    tri[0] = (m[:, None] <= m[None, :]) * (-1.0 / 16.0)
    tri[1] = (m[:, None] >= m[None, :]) * (-1.0 / 16.0)
    tri[2] = (m[:, None] > m[None, :]) * (-1.0 / 16.0)
    tri[3] = (m[:, None] < m[None, :]) * (-1.0 / 16.0)
    tri[4] = (m[:, None] <= m[None, :]) * 1.0
    tri[5] = (m[:, None] >= m[None, :]) * 1.0
    return dict(k_cos=np.cos(ang).astype(np.float32), k_sin=np.sin(ang).astype(np.float32),
                k_ident=np.eye(128, dtype=np.float32), k_tri=tri)


def make_in_maps(inputs, cores, used=None):
    kc = host_consts()
    f = lambda a: np.ascontiguousarray(np.asarray(a, dtype=np.float32))
    shared = {k: f(inputs[k]) for k in ("c_ctx", "ada_w", "ada_b", "norm1_g", "norm2_g", "w_in", "diff_subln_g",
                                        "diff_w_out", "conv_w", "conv_w_out", "gla_w_a2", "gla_norm_g", "gla_w_out",
                                        "w_o", "router_w", "router_b", "moe_w1", "moe_b1", "moe_w2", "moe_b2",
                                        "final_norm_g")}
    shared["diff_lambda"] = f(inputs["diff_lambda"]).reshape(DEPTH, 256)
    shared["gla_b_a"] = f(inputs["gla_b_a"]).reshape(DEPTH, 512)
    shared.update(kc)
    maps = []
    for b in cores:
        m = dict(shared)
        m["x"] = f(inputs["x"][b])
        m["c"] = f(inputs["c"][b])
        m["ctx"] = f(inputs["ctx"][b])
        if used is not None:
            m = {k: v for k, v in m.items() if k in used}
        maps.append(m)
    return maps


def kernel(**inputs):
    nc = build()
    maps = make_in_maps(inputs, list(range(8)))
    res = run_bass_kernel_spmd(nc, maps, core_ids=list(range(8)))
    return np.stack([np.asarray(r["y"], dtype=np.float32) for r in res.results], axis=0)
```
